# Optimizing a Trainium2 kernel written in Bass

```python
import math
import jax, jax.numpy as jnp
from jax import lax
import numpy as np

D_MODEL = 1024
BATCH = 4
SEQ = 4096
DEPTH = 1

CTX_LEN = 256
GRID_W = 64
F_GROUPS = 4
F_GROUP_DIM = 128
F_DIM = F_GROUPS * F_GROUP_DIM
NQK_HEADS = 4
NV_HEADS = 8
DK = 128
DV = 128
QK_DIM = NQK_HEADS * DK
V_DIM = NV_HEADS * DV
QKV_DIM = 2 * QK_DIM + V_DIM
CONV_K = 3
CHUNK = 64
BETA_OFF = QKV_DIM
A_OFF = BETA_OFF + 2 * NV_HEADS
GDN_IN_COLS = A_OFF + 2 * NV_HEADS
Z_OFF = GDN_IN_COLS
F_OFF = Z_OFF + V_DIM
GA_OFF = F_OFF + F_DIM
GB_OFF = GA_OFF + D_MODEL
IN_COLS = GB_OFF + D_MODEL
N_EXPERTS = 32
TOP_K = 4
D_EXPERT = D_MODEL
SWIGLU_ALPHA = 1.702
SWIGLU_LIMIT = 7.0
MOE_BLOCK = 128
EPS = 1e-6

kernel_name = 'hybrid_fnet_gdn_moe_dit_block'


def rmsnorm(x, g):
    xf = x.astype(jnp.float32)
    y = xf * lax.rsqrt(jnp.mean(xf * xf, axis=-1, keepdims=True) + EPS)
    return (y * g.astype(jnp.float32)).astype(x.dtype)


def modulate(h, shift, scale):
    return h * (1.0 + scale) + shift


def l2norm(t):
    t = t.astype(jnp.float32)
    return t * lax.rsqrt(jnp.sum(t * t, axis=-1, keepdims=True) + EPS)


def short_conv_grid(t, w, rows):
    B, L, C = t.shape
    y = lax.conv_general_dilated(t.reshape(B, rows, GRID_W, C), w[:, :, None, :], (1, 1), 'SAME',
                                 dimension_numbers=('NHWC', 'HWIO', 'NHWC'), feature_group_count=C)
    return y.reshape(B, L, C)


def short_conv_seq(t, w):
    C = t.shape[-1]
    return lax.conv_general_dilated(t, w[CONV_K // 2][:, None, :], (1,), 'SAME',
                                    dimension_numbers=('NWC', 'WIO', 'NWC'), feature_group_count=C)


def gdn_inputs(p, conv_w, rows):
    B, L, _ = p.shape
    qkv = p[..., :QKV_DIM]
    qkv = jax.nn.silu(short_conv_grid(qkv, conv_w, rows) if rows is not None else short_conv_seq(qkv, conv_w))
    rep = NV_HEADS // NQK_HEADS
    q = jnp.repeat(l2norm(qkv[..., :QK_DIM].reshape(B, L, NQK_HEADS, DK)) * DK ** -0.5, rep, axis=2)
    k = jnp.repeat(l2norm(qkv[..., QK_DIM:2 * QK_DIM].reshape(B, L, NQK_HEADS, DK)), rep, axis=2)
    v = qkv[..., 2 * QK_DIM:].reshape(B, L, NV_HEADS, DV)
    beta = jax.nn.sigmoid(p[..., BETA_OFF:A_OFF].astype(jnp.float32)).reshape(B, L, 2, NV_HEADS)
    a_raw = p[..., A_OFF:GDN_IN_COLS].astype(jnp.float32).reshape(B, L, 2, NV_HEADS)
    return q, k, v, a_raw, beta


def chunk_gated_delta(q, k, v, log_g, beta, s0):
    B, L, H, _ = q.shape
    n = L // CHUNK
    f32 = jnp.float32

    def blocks(t):
        t = t.astype(f32).reshape((B, n, CHUNK, H) + t.shape[3:])
        return jnp.moveaxis(t, 3, 1)

    qc, kc, vc, bc = blocks(q), blocks(k), blocks(v), blocks(beta)
    gc = jnp.cumsum(blocks(log_g), axis=-1)
    incl = jnp.tril(jnp.ones((CHUNK, CHUNK), bool))
    strict = jnp.tril(jnp.ones((CHUNK, CHUNK), bool), -1)
    decay = jnp.exp(jnp.where(incl, gc[..., :, None] - gc[..., None, :], -jnp.inf))
    kk = jnp.einsum('bhnid,bhnjd->bhnij', kc, kc)
    lower = jnp.where(strict, bc[..., :, None] * kk * decay, 0.0)
    rhs = jnp.concatenate([bc[..., None] * vc, (bc * jnp.exp(gc))[..., None] * kc], axis=-1)
    sol = lax.linalg.triangular_solve(jnp.eye(CHUNK, dtype=f32) + lower, rhs,
                                      left_side=True, lower=True, unit_diagonal=True)
    u_new, w = sol[..., :DV], sol[..., DV:]
    qk = jnp.einsum('bhnid,bhnjd->bhnij', qc, kc) * decay
    q_dec = qc * jnp.exp(gc)[..., None]
    g_end = gc[..., -1]
    k_dec = kc * jnp.exp(g_end[..., None] - gc)[..., None]

    def step(S, xs):
        u_c, w_c, qk_c, qd_c, kd_c, ge_c = xs
        u = u_c - jnp.einsum('bhck,bhkv->bhcv', w_c, S)
        o = jnp.einsum('bhck,bhkv->bhcv', qd_c, S) + jnp.einsum('bhij,bhjv->bhiv', qk_c, u)
        S = jnp.exp(ge_c)[..., None, None] * S + jnp.einsum('bhck,bhcv->bhkv', kd_c, u)
        return S, o

    xs = tuple(jnp.moveaxis(t, 2, 0) for t in (u_new, w, qk, q_dec, k_dec, g_end))
    s_fin, o = lax.scan(step, s0.astype(f32), xs)
    o = jnp.moveaxis(jnp.moveaxis(o, 0, 2), 1, 3).reshape(B, L, H, DV)
    return o, s_fin


def gdn_bidir(q, k, v, a_raw, beta, a_log, dt_bias, s0_f, s0_b):
    log_g = -jnp.exp(a_log.astype(jnp.float32)) * jax.nn.softplus(a_raw + dt_bias.astype(jnp.float32))
    o_f, s_f = chunk_gated_delta(q, k, v, log_g[:, :, 0], beta[:, :, 0], s0_f)
    flip = lambda t: jnp.flip(t, axis=1)
    o_b, s_b = chunk_gated_delta(flip(q), flip(k), flip(v), flip(log_g[:, :, 1]), flip(beta[:, :, 1]), s0_b)
    return o_f + flip(o_b), s_f, s_b


def fourier_mix(f):
    B, L, _ = f.shape
    fg = f.astype(jnp.float32).reshape(B, L, F_GROUPS, F_GROUP_DIM)
    return jnp.fft.fftn(fg, axes=(1, 3), norm='ortho').real.reshape(B, L, F_DIM).astype(f.dtype)


def merge_branches(p, o, gdn_norm_g, w_fourier_out, w_gdn_out, w_merge_out):
    B, L, _ = p.shape
    z = p[..., Z_OFF:F_OFF].astype(jnp.float32).reshape(B, L, NV_HEADS, DV)
    yb = o * lax.rsqrt(jnp.mean(o * o, axis=-1, keepdims=True) + EPS) * gdn_norm_g.astype(jnp.float32) * jax.nn.silu(z)
    yb = yb.reshape(B, L, V_DIM).astype(p.dtype) @ w_gdn_out
    ya = fourier_mix(p[..., F_OFF:GA_OFF]) @ w_fourier_out
    ga = jax.nn.sigmoid(p[..., GA_OFF:GB_OFF])
    gb = jax.nn.sigmoid(p[..., GB_OFF:IN_COLS])
    return (ga * ya + gb * yb) @ w_merge_out


def moe_ffn(h, w_router, b_router, w_gate, b_gate, w_up, b_up, w_down, b_down):
    N, D = h.shape
    logits = h.astype(jnp.float32) @ w_router.astype(jnp.float32) + b_router.astype(jnp.float32)
    top_val, top_idx = lax.top_k(logits, TOP_K)
    top_w = jax.nn.softmax(top_val, axis=-1)
    flat_e = top_idx.reshape(-1)
    flat_tok = jnp.repeat(jnp.arange(N, dtype=jnp.int32), TOP_K)
    order = jnp.argsort(flat_e)
    e_sorted = flat_e[order]
    counts = jnp.bincount(flat_e, length=N_EXPERTS)
    padded = (counts + MOE_BLOCK - 1) // MOE_BLOCK * MOE_BLOCK
    pad_end = jnp.cumsum(padded)
    pad_start = pad_end - padded
    grp_start = jnp.cumsum(counts) - counts
    dest = pad_start[e_sorted] + jnp.arange(N * TOP_K, dtype=jnp.int32) - grp_start[e_sorted]
    n_blocks = -(-(N * TOP_K + N_EXPERTS * (MOE_BLOCK - 1)) // MOE_BLOCK)
    n_slots = n_blocks * MOE_BLOCK
    slot_tok = jnp.full((n_slots,), N, jnp.int32).at[dest].set(flat_tok[order])
    slot_w = jnp.zeros((n_slots,), jnp.float32).at[dest].set(top_w.reshape(-1)[order])
    block_e = jnp.minimum(jnp.searchsorted(pad_end, jnp.arange(n_blocks, dtype=jnp.int32) * MOE_BLOCK, side='right'),
                          N_EXPERTS - 1)
    h_pad = jnp.concatenate([h, jnp.zeros((1, D), h.dtype)], axis=0)
    xb = h_pad[slot_tok].reshape(n_blocks, MOE_BLOCK, D)

    def expert_block(args):
        xe, e = args
        gate = jnp.minimum(xe @ w_gate[e] + b_gate[e], SWIGLU_LIMIT)
        up = jnp.clip(xe @ w_up[e] + b_up[e], -SWIGLU_LIMIT, SWIGLU_LIMIT)
        act = (up + 1.0) * gate * jax.nn.sigmoid(SWIGLU_ALPHA * gate)
        return act @ w_down[e] + b_down[e]

    yb = lax.map(expert_block, (xb, block_e)).reshape(n_slots, D)
    out = jnp.zeros((N + 1, D), jnp.float32).at[slot_tok].add(yb.astype(jnp.float32) * slot_w[:, None])
    return out[:N].astype(h.dtype)


def setup_inputs(seed: int = 0) -> dict:
    key = jax.random.key(seed)
    ks = jax.random.split(key, 32)
    D = D_MODEL
    nrm = lambda k, shape, s: jax.random.normal(k, shape, jnp.float32) * s
    dt = jnp.exp(jax.random.uniform(ks[11], (DEPTH, 2, NV_HEADS), jnp.float32, math.log(1e-3), math.log(1e-1)))
    return {
        'x': nrm(ks[0], (BATCH, SEQ, D), 1.0),
        'c': nrm(ks[1], (BATCH, D), 1.0),
        'ctx': nrm(ks[2], (BATCH, CTX_LEN, D), 1.0),
        'c_ctx': nrm(ks[3], (D,), 1.0),
        'w_mod': nrm(ks[4], (DEPTH, D, 6 * D), 0.5 * D ** -0.5),
        'b_mod': nrm(ks[5], (DEPTH, 6 * D), 0.02),
        'norm1_g': 1.0 + nrm(ks[6], (DEPTH, D), 0.05),
        'norm2_g': 1.0 + nrm(ks[7], (DEPTH, D), 0.05),
        'w_in': nrm(ks[8], (DEPTH, D, IN_COLS), D ** -0.5),
        'conv_w': nrm(ks[9], (DEPTH, CONV_K, CONV_K, QKV_DIM), 1.0 / CONV_K),
        'a_log': jnp.log(jax.random.uniform(ks[10], (DEPTH, 2, NV_HEADS), jnp.float32, 1.0, 16.0)),
        'dt_bias': dt + jnp.log(-jnp.expm1(-dt)),
        'gdn_norm_g': 1.0 + nrm(ks[12], (DEPTH, DV), 0.05),
        'w_fourier_out': nrm(ks[13], (DEPTH, F_DIM, D), F_DIM ** -0.5),
        'w_gdn_out': nrm(ks[14], (DEPTH, V_DIM, D), V_DIM ** -0.5),
        'w_merge_out': nrm(ks[15], (DEPTH, D, D), D ** -0.5),
        'w_router': nrm(ks[16], (DEPTH, D, N_EXPERTS), D ** -0.5),
        'b_router': nrm(ks[17], (DEPTH, N_EXPERTS), 0.01),
        'w_gate': nrm(ks[18], (DEPTH, N_EXPERTS, D, D_EXPERT), D ** -0.5),
        'b_gate': nrm(ks[19], (DEPTH, N_EXPERTS, D_EXPERT), 0.01),
        'w_up': nrm(ks[20], (DEPTH, N_EXPERTS, D, D_EXPERT), D ** -0.5),
        'b_up': nrm(ks[21], (DEPTH, N_EXPERTS, D_EXPERT), 0.01),
        'w_down': nrm(ks[22], (DEPTH, N_EXPERTS, D_EXPERT, D), D_EXPERT ** -0.5),
        'b_down': nrm(ks[23], (DEPTH, N_EXPERTS, D), 0.01),
        'final_norm_g': 1.0 + nrm(ks[24], (D,), 0.05),
    }


def reference(x, c, ctx, c_ctx, w_mod, b_mod, norm1_g, norm2_g, w_in, conv_w, a_log, dt_bias, gdn_norm_g,
              w_fourier_out, w_gdn_out, w_merge_out, w_router, b_router, w_gate, b_gate, w_up, b_up,
              w_down, b_down, final_norm_g):
    B, L, D = x.shape
    rows = L // GRID_W
    h_ctx = ctx
    for l in range(DEPTH):
        last = l == DEPTH - 1
        mod = (jax.nn.silu(c) @ w_mod[l] + b_mod[l])[:, None, :]
        sh1, sc1, g1, sh2, sc2, g2 = jnp.split(mod, 6, axis=-1)
        mod_c = jax.nn.silu(c_ctx) @ w_mod[l] + b_mod[l]
        csh1, csc1, cg1, csh2, csc2, cg2 = jnp.split(mod_c, 6)

        u_ctx = modulate(rmsnorm(h_ctx, norm1_g[l]), csh1, csc1)
        p_ctx = u_ctx @ (w_in[l][:, :GDN_IN_COLS] if last else w_in[l])
        q, k, v, a_raw, beta = gdn_inputs(p_ctx, conv_w[l], None)
        zero = jnp.zeros((B, NV_HEADS, DK, DV), jnp.float32)
        o_ctx, s_f, s_b = gdn_bidir(q, k, v, a_raw, beta, a_log[l], dt_bias[l], zero, zero)

        u = modulate(rmsnorm(x, norm1_g[l]), sh1, sc1)
        p = u @ w_in[l]
        q, k, v, a_raw, beta = gdn_inputs(p, conv_w[l], rows)
        o, _, _ = gdn_bidir(q, k, v, a_raw, beta, a_log[l], dt_bias[l], s_f, s_b)
        x = x + g1 * merge_branches(p, o, gdn_norm_g[l], w_fourier_out[l], w_gdn_out[l], w_merge_out[l])

        h2 = modulate(rmsnorm(x, norm2_g[l]), sh2, sc2)
        x = x + g2 * moe_ffn(h2.reshape(B * L, D), w_router[l], b_router[l], w_gate[l], b_gate[l],
                             w_up[l], b_up[l], w_down[l], b_down[l]).reshape(B, L, D)

        if not last:
            h_ctx = h_ctx + cg1 * merge_branches(p_ctx, o_ctx, gdn_norm_g[l], w_fourier_out[l], w_gdn_out[l], w_merge_out[l])
            hc2 = modulate(rmsnorm(h_ctx, norm2_g[l]), csh2, csc2)
            n_ctx = h_ctx.shape[1]
            h_ctx = h_ctx + cg2 * moe_ffn(hc2.reshape(B * n_ctx, D), w_router[l], b_router[l], w_gate[l], b_gate[l],
                                          w_up[l], b_up[l], w_down[l], b_down[l]).reshape(B, n_ctx, D)
    return rmsnorm(x, final_norm_g)
```

```python
import contextlib
import numpy as np
import ml_dtypes
import concourse.bass as bass
import concourse.mybir as mybir
from concourse.bass_utils import run_bass_kernel_spmd

F32 = mybir.dt.float32
BF16 = mybir.dt.bfloat16
I32 = mybir.dt.int32
U32 = mybir.dt.uint32
AF = mybir.ActivationFunctionType
ALU = mybir.AluOpType
AX = mybir.AxisListType

PE, ACT, DVE, POOL, SP = 'pe', 'act', 'dve', 'pool', 'sp'
ENGS = [PE, ACT, DVE, POOL, SP]
NDS = 8

D = 1024
L = 4096
NT = 32
OWN0 = 16
CTXL = 256
QKV = 2048
BETA_OFF = 2048
A_OFF = 2064
GDN_IN = 2080
Z_OFF = 2080
F_OFF = 3104
GA_OFF = 3616
GB_OFF = 4640
IN_COLS = 5664
NE = 32
EPS = 1e-6
MOE_CAPS = [8] + [6] * 3 + [5] * 4 + [4] * 8 + [3] * 16


class Sched:
    def __init__(s, nc):
        s.nc = nc
        s.ops = {e: [] for e in ENGS}
        s.res = {}
        s.ndma = {e: 0 for e in ENGS}
        s.epoch = 0

    def op(s, eng, fn, reads=(), writes=(), dma=False):
        idx = len(s.ops[eng])
        me = (eng, idx)
        deps = []
        for k in reads:
            r = s.res.get(k)
            if r and r[0] is not None:
                deps.append(r[0])
        for k in writes:
            r = s.res.get(k)
            if r:
                if r[0] is not None:
                    deps.append(r[0])
                deps.extend(r[1])
        waits = []
        best = {}
        for p in deps:
            pe, pi = p
            if s.ops[pe][pi]['dma']:
                waits.append(p)
                continue
            if pe == eng and eng == PE:
                continue
            if pi > best.get(pe, -1):
                best[pe] = pi
        for pe, pi in best.items():
            waits.append((pe, pi))
        o = dict(fn=fn, waits=waits, sig=False, dma=dma, dsem=None, dval=None, dn=0, ep=s.epoch)
        if dma:
            n = s.ndma[eng]
            s.ndma[eng] += 1
            o['dsem'] = n % NDS
            o['dval'] = 16 * (n // NDS + 1)
            o['dn'] = n
        s.ops[eng].append(o)
        for k in reads:
            s.res.setdefault(k, [None, []])[1].append(me)
        for k in writes:
            s.res[k] = [me, []]
        return me

    def barrier(s):
        for e in ENGS:
            pend = []
            for e2 in ENGS:
                n = len(s.ops[e2])
                if e2 != e:
                    for i in range(n - 1, -1, -1):
                        if s.ops[e2][i]['fn'] is not None and not s.ops[e2][i]['dma']:
                            pend.append((e2, i))
                            break
                cnt = 0
                for i in range(n - 1, -1, -1):
                    if s.ops[e2][i]['dma']:
                        pend.append((e2, i))
                        cnt += 1
                        if cnt >= NDS:
                            break
            s.ops[e].append(dict(fn=None, waits=pend, sig=False, dma=False, dsem=None, dval=None, dn=0, ep=s.epoch))
        s.res = {}
        s.epoch += 1

    def emit(s):
        nc = s.nc
        for e in ENGS:
            for o in s.ops[e]:
                for (pe, pi) in o['waits']:
                    po = s.ops[pe][pi]
                    if not po['dma']:
                        if po['fn'] is None:
                            raise RuntimeError("wait on barrier pseudo-op")
                        po['sig'] = True
        sigcount = {}
        used = set()
        for e in ENGS:
            c = {}
            for i, o in enumerate(s.ops[e]):
                if o['sig']:
                    c[o['ep']] = c.get(o['ep'], 0) + 1
                    used.add((e, o['ep']))
                sigcount[(e, i)] = c.get(o['ep'], 0)
        s.maxsig = max([0] + [sigcount[k] for k in sigcount])
        with contextlib.ExitStack() as st:
            esem = {k: st.enter_context(nc.semaphore("es_%s_%d" % k)) for k in sorted(used)}
            dsem = {e: [st.enter_context(nc.semaphore("ds_%s_%d" % (e, i))) for i in range(NDS)] for e in ENGS}
            block = st.enter_context(nc.Block())

            def run(e, eng):
                known = {}
                knownd = {}
                for o in s.ops[e]:
                    need = {}
                    needd = {}
                    if o['dma'] and o['dn'] >= NDS:
                        needd[(e, o['dsem'])] = o['dval'] - 16
                    for (pe, pi) in o['waits']:
                        po = s.ops[pe][pi]
                        if po['dma']:
                            k = (pe, po['dsem'])
                            needd[k] = max(needd.get(k, 0), po['dval'])
                        else:
                            k = (pe, po['ep'])
                            need[k] = max(need.get(k, 0), sigcount[(pe, pi)])
                    for k, v in need.items():
                        if known.get(k, 0) < v:
                            eng.wait_ge(esem[k], v)
                            known[k] = v
                    for k, v in needd.items():
                        if knownd.get(k, 0) < v:
                            eng.wait_ge(dsem[k[0]][k[1]], v)
                            knownd[k] = v
                    if o['fn'] is None:
                        continue
                    inst = o['fn'](eng)
                    if o['dma']:
                        inst.then_inc(dsem[e][o['dsem']], 16)
                    elif o['sig']:
                        inst.then_inc(esem[(e, o['ep'])], 1)

            block.tensor(lambda eng: run(PE, eng))
            block.scalar(lambda eng: run(ACT, eng))
            block.vector(lambda eng: run(DVE, eng))
            block.gpsimd(lambda eng: run(POOL, eng))
            block.sync(lambda eng: run(SP, eng))


class Prog:
    def __init__(p, nc):
        p.nc = nc
        p.S = Sched(nc)
        p.st = contextlib.ExitStack()
        p.din = {}
        p.dout = {}

    def inp(p, name, shape, dt):
        t = p.nc.dram_tensor(name, list(shape), dt, kind="ExternalInput").ap()
        p.din[name] = t
        return t

    def out(p, name, shape, dt):
        t = p.nc.dram_tensor(name, list(shape), dt, kind="ExternalOutput").ap()
        p.dout[name] = t
        return t

    def scratch(p, name, shape, dt):
        return p.nc.dram_tensor(name, list(shape), dt, kind="Internal").ap()

    def sb(p, name, shape, dt, st=None):
        return (st or p.st).enter_context(p.nc.sbuf_tensor(name, list(shape), dt))

    def psum(p, name, shape, dt):
        return p.st.enter_context(p.nc.psum_tensor(name, list(shape), dt))

    def dma(p, q, out, in_, r=(), w=(), **kw):
        return p.S.op(q, lambda e: e.dma_start(out=out, in_=in_, **kw), r, w, dma=True)

    def mm(p, out, lhsT, rhs, start, stop, r=(), w=()):
        return p.S.op(PE, lambda e: e.matmul(out, lhsT, rhs, start=start, stop=stop), r, w)

    def tr(p, out, in_, ident, r=(), w=()):
        return p.S.op(PE, lambda e: e.transpose(out, in_, ident), r, w)

    def act(p, out, in_, func, r=(), w=(), eng=ACT, **kw):
        return p.S.op(eng, lambda e: e.activation(out=out, in_=in_, func=func, **kw), r, w)

    def ts(p, eng, out, in0, s1, s2, op0, op1=None, r=(), w=(), **kw):
        if op1 is None:
            return p.S.op(eng, lambda e: e.tensor_scalar(out, in0, s1, s2, op0, **kw), r, w)
        return p.S.op(eng, lambda e: e.tensor_scalar(out, in0, s1, s2, op0, op1, **kw), r, w)

    def tt(p, eng, out, in0, in1, op, r=(), w=()):
        return p.S.op(eng, lambda e: e.tensor_tensor(out, in0, in1, op), r, w)

    def stt(p, eng, out, in0, scalar, in1, op0, op1, r=(), w=()):
        return p.S.op(eng, lambda e: e.scalar_tensor_tensor(out, in0, scalar, in1, op0, op1), r, w)

    def cp(p, eng, out, in_, r=(), w=()):
        if eng == ACT:
            return p.S.op(eng, lambda e: e.copy(out, in_), r, w)
        return p.S.op(eng, lambda e: e.tensor_copy(out, in_), r, w)

    def generic(p, eng, fn, r=(), w=()):
        return p.S.op(eng, fn, r, w)


def bf(a):
    return np.ascontiguousarray(a).astype(ml_dtypes.bfloat16)


def fm_layout(v, nchunk):
    return np.ascontiguousarray(np.asarray(v, np.float32).reshape(nchunk, 128).T)


def host_consts():
    c = {}
    c['ident_bf'] = bf(np.eye(128, dtype=np.float32))
    c['ident_f'] = np.eye(128, dtype=np.float32)
    c['ones_f'] = np.ones((128, 128), np.float32)
    t = np.arange(128)
    c['u_incl'] = (t[:, None] <= t[None, :]).astype(np.float32)
    c['u_strict'] = (t[:, None] < t[None, :]).astype(np.float32)
    c['l_incl'] = np.ascontiguousarray(c['u_incl'].T)
    c['l_strict'] = np.ascontiguousarray(c['u_strict'].T)
    rep4 = lambda m: np.ascontiguousarray(np.tile(m, (1, 4)))
    c['ms4_f'] = rep4(c['u_strict']); c['mi4_f'] = rep4(c['u_incl'])
    c['ms4_b'] = rep4(c['l_strict']); c['mi4_b'] = rep4(c['l_incl'])
    c['i4'] = bf(rep4(np.eye(128, dtype=np.float32)))
    lv = np.zeros((2, 7, 128, 512), np.float32)
    for li in range(7):
        sz = 1 << li
        j = t[:, None]; i = t[None, :]
        m = ((j // (2 * sz)) == (i // (2 * sz))) & ((j % (2 * sz)) < sz) & ((i % (2 * sz)) >= sz)
        lv[0, li] = rep4(m.astype(np.float32))
        lv[1, li] = rep4(m.T.astype(np.float32))
    c['lvl'] = bf(lv.transpose(2, 0, 1, 3))
    c['ones_bf'] = bf(np.ones((128, 128), np.float32))
    c['ustrict_bf'] = bf(c['u_strict'])
    c['iota32'] = np.ascontiguousarray(np.tile(np.arange(32, dtype=np.float32)[None, :], (128, 1)))
    c['ecol'] = np.ascontiguousarray(np.arange(128, dtype=np.float32)[:, None])
    c['blk128'] = np.zeros((128, 1), np.float32)
    tb = np.concatenate([[0], np.cumsum(MOE_CAPS)[:-1]]).astype(np.float32) * 128.0
    c['basetab'] = np.ascontiguousarray(np.tile(tb[None, :], (128, 1)))
    c['captab'] = np.ascontiguousarray(np.tile((np.array(MOE_CAPS, np.float32) * 128.0)[None, :], (128, 1)))
    c['rowoff'] = np.ascontiguousarray((np.arange(8)[None, :] * 128 + np.arange(128)[:, None]).astype(np.float32))
    c['tokid'] = np.ascontiguousarray((np.arange(16)[None, :] * 128 + np.arange(128)[:, None]).astype(np.int32))
    return c


class Builder(Prog):
    def __init__(p, nc, debug=None):
        super().__init__(nc)
        p.debug = debug
        p.const_np = host_consts()
        p.psb = [p.psum("ps%d" % i, [128, 512], F32) for i in range(8)]

    def load_const(p, name, dt):
        a = p.const_np[name]
        d = p.inp("c_" + name, a.shape, dt)
        t = p.sb("k_" + name, a.shape, dt)
        p.dma(SP, t[:], d, w=[('k', name)])
        return t

    def phase0(p):
        nc = p.nc
        p.ident_bf = p.load_const('ident_bf', BF16)
        p.ident_f = p.load_const('ident_f', F32)
        p.ones_f = p.load_const('ones_f', F32)
        cT = p.inp("cT", [128, 8, 2], F32)
        bmod = p.inp("bmod", [128, 48], F32)
        n1g = p.inp("n1g", [128, 8], F32)
        n2g = p.inp("n2g", [128, 8], F32)
        wmod = p.inp("w_mod", [D, 6 * D], F32)
        p.eps_t = p.sb("eps_t", [128, 1], F32)
        p.S.op(DVE, lambda e: e.memset(p.eps_t[:], EPS), [], ['eps_t'])
        p.modsb = p.sb("modsb", [128, 48, 2], F32)
        p.vecs = p.sb("vecs", [128, 8, 8], F32)
        st0 = contextlib.ExitStack()
        scT = p.sb("scT", [128, 8, 2], F32, st0)
        bm = p.sb("bm", [128, 48], F32, st0)
        g1t = p.sb("n1g_sb", [128, 8], F32, st0)
        g2t = p.sb("n2g_sb", [128, 8], F32, st0)
        wb = [p.sb("wmodb%d" % i, [128, 8, 512], F32, st0) for i in range(2)]
        p.dma(SP, scT[:], cT, w=['scT'])
        p.dma(SP, bm[:], bmod, w=['bm'])
        p.dma(SP, g1t[:], n1g, w=['n1g'])
        p.dma(SP, g2t[:], n2g, w=['n2g'])
        p.act(scT[:], scT[:], AF.Silu, r=['scT'], w=['scT'])
        wv = wmod.rearrange("(kc q) n -> q kc n", q=128)
        psM = p.psb[0]
        for blk in range(12):
            b = wb[blk % 2]
            p.dma(SP, b[:], wv[:, :, blk * 512:(blk + 1) * 512], w=[('wmodb', blk % 2)])
            for fc in range(4):
                j = blk * 4 + fc
                for kc in range(8):
                    p.mm(psM[:, 2 * j:2 * j + 2], b[:, kc, fc * 128:(fc + 1) * 128], scT[:, kc, :],
                         kc == 0, kc == 7, r=[('wmodb', blk % 2), 'scT'], w=[('ps', 0)])
        pv = psM[:, 0:96].rearrange("q (j m) -> q j m", m=2)
        for m in range(2):
            p.tt(DVE, p.modsb[:, :, m], pv[:, :, m], bm[:], ALU.add, r=[('ps', 0), 'bm'], w=['modsb'])
        p.stt(DVE, p.vecs[:, 0, :], p.modsb[:, 8:16, 0], 1.0, g1t[:], ALU.add, ALU.mult, r=['modsb', 'n1g'], w=['vecs'])
        p.stt(DVE, p.vecs[:, 1, :], p.modsb[:, 8:16, 1], 1.0, g1t[:], ALU.add, ALU.mult, r=['modsb', 'n1g'], w=['vecs'])
        p.stt(DVE, p.vecs[:, 2, :], p.modsb[:, 32:40, 0], 1.0, g2t[:], ALU.add, ALU.mult, r=['modsb', 'n2g'], w=['vecs'])
        p.S.barrier()
        st0.close()
        if p.debug == '0':
            o = p.out("dbg_mod", [128, 48, 2], F32)
            p.dma(SP, o, p.modsb[:], r=['modsb'])
            o2 = p.out("dbg_vecs", [128, 8, 8], F32)
            p.dma(SP, o2, p.vecs[:], r=['vecs'])

    def gs1(p, c): return p.vecs[:, 0, c:c + 1]
    def sh1(p, c): return p.modsb[:, c, 0:1]
    def cgs1(p, c): return p.vecs[:, 1, c:c + 1]
    def csh1(p, c): return p.modsb[:, c, 1:2]

    def norm_T(p, xsrc, gs, sh, uT_dst, bufs, tag):
        xt, xk = bufs['x']
        p.dma(SP, xt[:], xsrc, w=[xk])
        junk, jk = bufs['junk']
        ss, sk = bufs['ss']
        p.act(junk[:], xt[:], AF.Square, r=[xk], w=[jk, sk], accum_out=ss[:, 0:1])
        p.act(ss[:, 2:3], ss[:, 0:1], AF.Ln, r=[sk], w=[sk], scale=1.0 / D, bias=p.eps_t[:, 0:1])
        p.act(ss[:, 3:4], ss[:, 2:3], AF.Exp, r=[sk], w=[sk], scale=-0.5)
        xn, nk = bufs['xn']
        p.ts(DVE, xn[:], xt[:], ss[:, 3:4], None, ALU.mult, r=[xk, sk], w=[nk])
        pb, pk = bufs['ps']
        pbv = pb[:].bitcast(BF16)
        for c in range(8):
            p.tr(pbv[:, c * 128:(c + 1) * 128], xn[:, c * 128:(c + 1) * 128], p.ident_bf[:], r=[nk, ('k', 'ident_bf')], w=[pk])
        if tag == 'split':
            return
        p.norm_T_b(gs, sh, uT_dst, bufs)

    def norm_T_b(p, gs, sh, uT_dst, bufs):
        pb, pk = bufs['ps']
        pbv = pb[:].bitcast(BF16)
        for c in range(8):
            dst, dk = uT_dst(c)
            p.act(dst, pbv[:, c * 128:(c + 1) * 128], AF.Identity, r=[pk, 'modsb', 'vecs'], w=[dk],
                  scale=gs(c), bias=sh(c))

    def phaseA(p):
        x = p.inp("x_seq", [L, D], F32)
        ctx = p.inp("ctx_seq", [CTXL, D], F32)
        w_in = p.inp("w_in", [D, IN_COLS], F32)
        cw = p.inp("conv_diag", [128, 16, 9, 128], F32)
        p.XF = p.scratch("XF", [L, 512], BF16)
        p.QKVs = p.scratch("QKVs", [L + CTXL, QKV], BF16)
        p.ba = p.sb("ba", [128, NT + 2, 32], F32)
        stA = contextlib.ExitStack()
        wA = p.sb("wA", [128, 8, 2592], BF16, stA)
        wv = w_in.rearrange("(kc q) n -> q kc n", q=128)
        for kc in range(8):
            p.dma(POOL, wA[:, kc, 0:2080], wv[:, kc, 0:2080], w=[('wA', kc)])
            p.dma(POOL, wA[:, kc, 2080:2592], wv[:, kc, F_OFF:F_OFF + 512], w=[('wA', kc)])
        cwts = [p.sb("cwt%d" % i, [128, 2, 9, 128], BF16, stA) for i in range(2)]
        NTT = NT + 2
        uT = p.sb("uT", [128, 8, NTT * 128], BF16, stA)
        stA1 = contextlib.ExitStack()
        NB1 = 4
        bufs = [{
            'x': (p.sb("xt%d" % i, [128, D], F32, stA1), ('xt', i)),
            'junk': (p.sb("junk%d" % i, [128, D], BF16, stA1), ('junk', i)),
            'ss': (p.sb("ss%d" % i, [128, 4], F32, stA1), ('ss', i)),
            'xn': (p.sb("xn%d" % i, [128, D], BF16, stA1), ('xn', i)),
            'ps': (p.psb[[0, 1, 6, 7][i]], ('ps', [0, 1, 6, 7][i])),
        } for i in range(NB1)]
        xfb = [p.sb("xfb%d" % i, [128, 512], BF16, stA1) for i in range(2)]
        def a1_args(t):
            if t < NT:
                return x[t * 128:(t + 1) * 128, :], p.gs1, p.sh1
            return ctx[(t - NT) * 128:(t - NT + 1) * 128, :], p.cgs1, p.csh1

        def a1_front(t):
            src, gs, sh = a1_args(t)
            p.norm_T(src, gs, sh, None, bufs[t % NB1], 'split')

        DEPTH = 2
        for t0 in range(DEPTH):
            a1_front(t0)
        for t in range(NTT):
            b = bufs[t % NB1]
            if t + DEPTH < NTT:
                a1_front(t + DEPTH)
            src, gs, sh = a1_args(t)
            p.norm_T_b(gs, sh, lambda c, t=t: (uT[:, c, t * 128:(t + 1) * 128], ('uT', t)), b)
            ps = p.psb[2 + t % 2]
            for kc in range(8):
                p.mm(ps[:, 0:32], uT[:, kc, t * 128:(t + 1) * 128], wA[:, kc, 2048:2080], kc == 0, kc == 7,
                     r=[('wA', kc), ('uT', t)], w=[('ps', 2 + t % 2)])
            p.cp(DVE, p.ba[:, t, :], ps[:, 0:32], r=[('ps', 2 + t % 2)], w=[('ba', t)])
            if t < NT:
                ps = p.psb[4 + t % 2]
                for kc in range(8):
                    p.mm(ps[:, :], uT[:, kc, t * 128:(t + 1) * 128], wA[:, kc, 2080:2592], kc == 0, kc == 7,
                         r=[('wA', kc), ('uT', t)], w=[('ps', 4 + t % 2)])
                xb = xfb[t % 2]
                p.cp(DVE, xb[:], ps[:, :], r=[('ps', 4 + t % 2)], w=[('xfb', t % 2)])
                p.dma(POOL, p.XF[t * 128:(t + 1) * 128, :], xb[:], r=[('xfb', t % 2)], w=[('XF', t)])
        p.S.barrier()
        stA1.close()
        LD = 72
        C0 = LD + L + 64
        CBN = C0 + 258
        cb = [p.sb("cb%d" % i, [128, 3, CBN], BF16, stA) for i in range(2)]
        for i in range(2):
            p.S.op(DVE, (lambda e, t_=cb[i]: e.memset(t_[:], 0.0)), [], [('cb', i)])
        qt = [p.sb("qt%d" % i, [128, 512], BF16, stA) for i in range(2)]
        for ch in range(16):
            cbuf = cb[ch % 2]
            ck = ('cb', ch % 2)
            if ch % 2 == 0:
                cwt = cwts[(ch // 2) % 2]
                cwk = ('cwt', (ch // 2) % 2)
                p.dma(POOL, cwt[:], cw[:, ch:ch + 2, :, :], w=[cwk])
            for st_ in range(9):
                ntok = 512 if st_ < 8 else 256
                t0 = st_ * 512
                ps = p.psb[st_ % 4]
                pk = ('ps', st_ % 4)
                for kc in range(8):
                    p.mm(ps[:, 0:ntok], wA[:, kc, ch * 128:(ch + 1) * 128], uT[:, kc, t0:t0 + ntok], kc == 0, kc == 7,
                         r=[('wA', kc)] + [('uT', t0 // 128 + q) for q in range(ntok // 128)], w=[pk])
                if st_ < 8:
                    o0 = LD + t0
                    p.cp(ACT, cbuf[:, 0, o0:o0 + 512], ps[:, 0:512], r=[pk], w=[ck])
                    sv = ps[:, 0:512].rearrange("q (r c) -> q r c", c=64)
                    p.cp(DVE, cbuf[:, 1, o0:o0 + 512].rearrange("q (r c) -> q r c", c=64)[:, :, 0:63], sv[:, :, 0:63], r=[pk], w=[ck])
                    p.cp(DVE, cbuf[:, 2, o0:o0 + 512].rearrange("q (r c) -> q r c", c=64)[:, :, 1:64], sv[:, :, 1:64], r=[pk], w=[ck])
                else:
                    p.cp(ACT, cbuf[:, 0, C0 + 1:C0 + 257], ps[:, 0:256], r=[pk], w=[ck])
            for t in range(NTT):
                if t % 4 == 0:
                    ps = p.psb[4 + (t // 4) % 4]
                    pk = ('ps', 4 + (t // 4) % 4)
                if t < NT:
                    taps = [(dy, dx) for dy in (-1, 0, 1) for dx in (-1, 0, 1)]
                else:
                    taps = [(0, dx) for dx in (-1, 0, 1)]
                for ti, (dy, dx) in enumerate(taps):
                    if t < NT:
                        base = LD + 128 * t + 64 * dy + dx
                        lhsT = cbuf[:, {-1: 1, 0: 0, 1: 2}[dx], base:base + 128]
                    else:
                        base = C0 + 1 + (t - NT) * 128 + dx
                        lhsT = cbuf[:, 0, base:base + 128]
                    tap = (dy + 1) * 3 + (dx + 1)
                    p.mm(ps[:, (t % 4) * 128:(t % 4 + 1) * 128], lhsT, cwt[:, ch % 2, tap, :], ti == 0, ti == len(taps) - 1,
                         r=[ck, cwk], w=[pk])
                if t % 4 == 3 or t == NTT - 1:
                    nt_ = t % 4 + 1
                    tb = t - t % 4
                    qi_ = (tb // 4) % 2
                    q_ = qt[qi_]
                    p.act(q_[:, 0:nt_ * 128], ps[:, 0:nt_ * 128], AF.Silu, r=[pk], w=[('qt', qi_)])
                    row = tb * 128 if tb < NT else L
                    dstv = p.QKVs[row:row + nt_ * 128, ch * 128:(ch + 1) * 128].rearrange("(a q) c -> q a c", q=128)
                    p.dma(SP, dstv, q_[:, 0:nt_ * 128].rearrange("q (a c) -> q a c", c=128), r=[('qt', qi_)], w=[('QKVs', tb, ch)])
        p.S.barrier()
        stA.close()
        if p.debug == 'A':
            o = p.out("dbg_qkv", [L + CTXL, QKV], BF16)
            p.dma(SP, o, p.QKVs, r=[])
            o = p.out("dbg_ba", [128, NT + 2, 32], F32)
            p.dma(SP, o, p.ba[:], r=[])
            o = p.out("dbg_xf", [L, 512], BF16)
            p.dma(SP, o, p.XF, r=[])


    def phaseG(p):
        DKS = 128 ** -0.5
        alog = p.inp("alog_t", [NT + 2, 16], F32)
        dtb = p.inp("dtb_t", [NT + 2, 16], F32)
        NTT = NT + 2
        p.Od = [p.scratch("O_f", [16 * 128, 1024], F32), p.scratch("O_b", [16 * 128, 1024], F32)]
        stG = contextlib.ExitStack()
        K = {}
        for nm, dt_ in [('u_incl', F32), ('l_incl', F32), ('u_strict', F32), ('l_strict', F32),
                        ('ms4_f', F32), ('mi4_f', F32), ('ms4_b', F32), ('mi4_b', F32), ('i4', BF16), ('lvl', BF16)]:
            a = p.const_np[nm]
            d = p.inp("c_" + nm, a.shape, dt_)
            K[nm] = p.sb("k_" + nm, a.shape, dt_, stG)
            p.dma(SP, K[nm][:], d, w=[('k', nm)])
        kr = lambda *n: [('k', x) for x in n]
        p.S.barrier()
        lgs = p.sb("lgs", [128, NTT, 16], F32, stG)
        bts = p.sb("bts", [128, NTT, 16], F32, stG)
        nbt = p.sb("nbts", [128, NTT, 16], F32, stG)
        prm = p.sb("prm", [128, 2, NTT, 16], F32, stG)
        p.dma(SP, prm[:, 0], alog.partition_broadcast(128), w=['prm'])
        p.dma(SP, prm[:, 1], dtb.partition_broadcast(128), w=['prm'])
        p.act(prm[:, 0], prm[:, 0], AF.Exp, r=['prm'], w=['prm'])
        p.tt(DVE, lgs[:], p.ba[:, :, 16:32], prm[:, 1], ALU.add, r=['prm'], w=['lgs'])
        p.act(lgs[:], lgs[:], AF.Exp, r=['lgs'], w=['lgs'])
        p.ts(DVE, lgs[:], lgs[:], 1.0, None, ALU.add, r=['lgs'], w=['lgs'])
        p.act(lgs[:], lgs[:], AF.Ln, r=['lgs'], w=['lgs'])
        p.stt(DVE, lgs[:], lgs[:], -1.0, prm[:, 0], ALU.mult, ALU.mult, r=['lgs', 'prm'], w=['lgs'])
        p.act(bts[:], p.ba[:, :, 0:16], AF.Exp, r=[], w=['bts'], scale=-1.0)
        p.ts(DVE, bts[:], bts[:], 1.0, None, ALU.add, r=['bts'], w=['bts'])
        p.S.op(DVE, lambda e: e.reciprocal(bts[:], bts[:]), ['bts'], ['bts'])
        p.ts(DVE, nbt[:], bts[:], -1.0, None, ALU.mult, r=['bts'], w=['nbt'])
        Sf = p.sb("S_f32", [128, 16, 128], F32, stG)
        Sb = [p.sb("S_bf%d" % i, [128, 16, 128], BF16, stG) for i in range(2)]
        p.S.op(DVE, lambda e: e.memset(Sf[:], 0.0), [], [('Sf', h) for h in range(16)])
        p.S.op(DVE, lambda e: e.memset(Sb[0][:], 0.0), [], [('Sb', 0, h) for h in range(16)])
        sbi = [0] * 16
        qkvb = [p.sb("qkvb%d" % i, [128, QKV], BF16, stG) for i in range(2)]
        sqs = [p.sb("sq%d" % i, [128, 1024], F32, stG) for i in range(2)]
        nrms = [p.sb("nrm%d" % i, [128, 4, 8], F32, stG) for i in range(2)]
        qkn = [p.sb("qkn%d" % i, [128, 8, 128], BF16, stG) for i in range(2)]
        kT = [p.sb("kT%d" % i, [128, 4, 128], BF16, stG) for i in range(2)]
        qT = [p.sb("qT%d" % i, [128, 4, 128], BF16, stG) for i in range(2)]
        gsc = [p.sb("gsc%d" % i, [128, 6, 16], F32, stG) for i in range(2)]
        CT = []
        for ci in range(2):
            c_ = dict(i=ci, ba=3 * ci)
            for nm_, shp, dt_ in [('LW', [128, 4, 128], F32), ('Eb', [128, 512], F32), ('Ems', [128, 512], F32), ('Emi', [128, 512], F32),
                                  ('Nb', [128, 512], BF16), ('NTb', [128, 512], BF16), ('NTl', [128, 6, 512], BF16),
                                  ('X0', [128, 512], BF16), ('X1', [128, 512], BF16), ('XT0', [128, 512], BF16), ('XT1', [128, 512], BF16),
                                  ('Yb', [128, 512], BF16), ('tmpb', [128, 512], BF16), ('tmpf', [128, 512], F32), ('qkp', [128, 512], BF16),
                                  ('kdec', [128, 512], BF16), ('rb', [128, 512], BF16), ('ub', [128, 512], BF16), ('o1', [128, 512], F32)]:
                c_[nm_] = p.sb("%s_%d" % (nm_, ci), shp, dt_, stG)
            CT.append(c_)
        ob = [p.sb("ob%d" % i, [128, 1024], F32, stG) for i in range(2)]
        PS = lambda i: p.psb[i]
        PK = lambda i: ('ps', i)
        sched = [(32, 0, False), (33, 0, False), (33, 1, False), (32, 1, False)]
        fw = [(t, 0, t >= OWN0) for t in range(NT)]
        bw = [(t, 1, True) for t in range(NT - 1, OWN0 - 1, -1)]
        while fw or bw:
            if fw:
                sched.append(fw.pop(0))
            if bw and (len(fw) < 2 * len(bw) + 1):
                sched.append(bw.pop(0))
        if p.debug == 'G0':
            sched = sched[:4]
        loaded = {}
        step = 0
        nload = 0
        real_op = p.S.op
        recs = []

        def rec_into(lst):
            p.S.op = lambda eng, fn, reads=(), writes=(), dma=False: lst.append((eng, fn, list(reads), list(writes), dma))

        for (t, d, need_out) in sched:
            prep_l, stage_ll, tail_l = [], [], []
            recs.append((prep_l, stage_ll, tail_l))
            rec_into(prep_l)
            bi = nload % 2
            nload += 1
            qb = qkvb[bi]
            sq = sqs[bi]
            nrm = nrms[bi]
            sqk = ('sq', bi)
            nrk = ('nrm', bi)
            row = t * 128 if t < NT else L + (t - NT) * 128
            p.dma(SP, qb[:], p.QKVs[row:row + 128, :], w=[('qkvb', bi)])
            p.tt(DVE, sq[:], qb[:, 0:1024], qb[:, 0:1024], ALU.mult, r=[('qkvb', bi)], w=[sqk])
            p.S.op(DVE, lambda e, nrm=nrm, sq=sq: e.tensor_reduce(nrm[:, 0, :], sq[:].rearrange("q (h c) -> q h c", c=128), AX.X, ALU.add), [sqk], [nrk])
            p.ts(DVE, nrm[:, 1, :], nrm[:, 0, :], EPS, None, ALU.add, r=[nrk], w=[nrk])
            p.act(nrm[:, 2, :], nrm[:, 1, :], AF.Ln, r=[nrk], w=[nrk])
            p.act(nrm[:, 3, :], nrm[:, 2, :], AF.Exp, r=[nrk], w=[nrk], scale=-0.5)
            p.ts(DVE, nrm[:, 3, 0:4], nrm[:, 3, 0:4], DKS, None, ALU.mult, r=[nrk], w=[nrk])
            qn = qkn[bi]
            p.tt(DVE, qn[:], qb[:, 0:1024].rearrange("q (h c) -> q h c", c=128), nrm[:, 3, :].unsqueeze(2).to_broadcast([128, 8, 128]), ALU.mult,
                 r=[('qkvb', bi), nrk], w=[('qkn', bi)])
            kTb, qTb = kT[bi], qT[bi]
            pv = PS(6)[:].bitcast(BF16)
            for h in range(4):
                p.tr(pv[:, h * 128:(h + 1) * 128], qn[:, 4 + h, :], p.ident_bf[:], r=[('qkn', bi)], w=[PK(6)])
            p.cp(ACT, kTb[:].rearrange("q h c -> q (h c)"), pv[:, 0:512], r=[PK(6)], w=[('kT', bi)])
            if need_out:
                pv5 = PS(6)[:].bitcast(BF16)[:, 512:1024]
                for h in range(4):
                    p.tr(pv5[:, h * 128:(h + 1) * 128], qn[:, h, :], p.ident_bf[:], r=[('qkn', bi)], w=[PK(6)])
                p.cp(ACT, qTb[:].rearrange("q h c -> q (h c)"), pv5[:, 0:512], r=[PK(6)], w=[('qT', bi)])
            g = gsc[bi]
            gk = ('gsc', bi)
            lg = lgs[:, t, :]
            ps6 = PS(7)
            p.mm(ps6[:, 0:8], K['u_incl'][:], lgs[:, t, 0:8], True, True, r=kr('u_incl') + ['lgs'], w=[PK(7)])
            p.mm(ps6[:, 8:16], K['l_incl'][:], lgs[:, t, 8:16], True, True, r=kr('l_incl') + ['lgs'], w=[PK(7)])
            p.mm(ps6[:, 16:32], p.ones_f[:], lgs[:, t, :], True, True, r=['lgs'], w=[PK(7)])
            p.cp(DVE, g[:, 0:2, :].rearrange("q a c -> q (a c)"), ps6[:, 0:32], r=[PK(7)], w=[gk])
            p.act(g[:, 2, :], g[:, 0, :], AF.Exp, r=[gk], w=[gk])
            p.ts(DVE, g[:, 3, :], g[:, 2, :], -1.0, None, ALU.mult, r=[gk], w=[gk])
            p.tt(DVE, g[:, 4, :], g[:, 1, :], g[:, 0, :], ALU.subtract, r=[gk], w=[gk])
            p.act(g[:, 4, :], g[:, 4, :], AF.Exp, r=[gk], w=[gk])
            p.act(g[:, 5, :], g[:, 1, :], AF.Exp, r=[gk], w=[gk])
            ms_lhs = K['l_strict'] if d == 0 else K['u_strict']
            mi_rhs = K['u_incl'] if d == 0 else K['l_incl']
            MS4 = K['ms4_f'] if d == 0 else K['ms4_b']
            MI4 = K['mi4_f'] if d == 0 else K['mi4_b']
            H4 = lambda ap: ap.rearrange("q (h c) -> q h c", c=128)
            bci = lambda ap: ap.unsqueeze(2).to_broadcast([128, 4, 128])
            bcm = lambda ap: ap.unsqueeze(1).to_broadcast([128, 4, 128])
            obuf = ob[step % 2]

            def key(c_, n):
                return (n, c_['i'])

            def st_D(c_, grp):
                hd0 = d * 8 + grp * 4
                p.tt(DVE, c_['LW'][:], bcm(ms_lhs[:]), bci(lgs[:, t, hd0:hd0 + 4]), ALU.mult, r=['lgs'], w=[key(c_, 'LW')])
                for j4 in range(4):
                    p.mm(PS(c_['ba'])[:, j4 * 128:(j4 + 1) * 128], c_['LW'][:, j4, :], mi_rhs[:], True, True, r=[key(c_, 'LW')], w=[PK(c_['ba'])])
                for j4 in range(4):
                    hq = (grp * 4 + j4) // 2
                    p.mm(PS(c_['ba'] + 1)[:, j4 * 128:(j4 + 1) * 128], kTb[:, hq, :], kTb[:, hq, :], True, True, r=[('kT', bi)], w=[PK(c_['ba'] + 1)])
                if need_out:
                    for j4 in range(4):
                        hq = (grp * 4 + j4) // 2
                        p.mm(PS(c_['ba'] + 2)[:, j4 * 128:(j4 + 1) * 128], kTb[:, hq, :], qTb[:, hq, :], True, True,
                             r=[('kT', bi), ('qT', bi)], w=[PK(c_['ba'] + 2)])

            def st_E(c_, grp):
                hd0 = d * 8 + grp * 4
                p.act(c_['Eb'][:], PS(c_['ba'])[:, :], AF.Exp, r=[PK(c_['ba'])], w=[key(c_, 'Eb')])
                p.tt(DVE, c_['Ems'][:], c_['Eb'][:], MS4[:], ALU.mult, r=[key(c_, 'Eb')], w=[key(c_, 'Ems')])
                p.tt(DVE, H4(c_['Ems'][:]), H4(c_['Ems'][:]), bci(nbt[:, t, hd0:hd0 + 4]), ALU.mult, r=[key(c_, 'Ems'), 'nbt'], w=[key(c_, 'Ems')])
                p.tt(DVE, c_['Nb'][:], PS(c_['ba'] + 1)[:, :], c_['Ems'][:], ALU.mult, r=[PK(c_['ba'] + 1), key(c_, 'Ems')], w=[key(c_, 'Nb')])
                if need_out:
                    p.tt(DVE, c_['Emi'][:], c_['Eb'][:], MI4[:], ALU.mult, r=[key(c_, 'Eb')], w=[key(c_, 'Emi')])
                    p.tt(DVE, c_['qkp'][:], PS(c_['ba'] + 2)[:, :], c_['Emi'][:], ALU.mult, r=[PK(c_['ba'] + 2), key(c_, 'Emi')], w=[key(c_, 'qkp')])

            def st_NT(c_, grp):
                pv3 = PS(c_['ba'])[:].bitcast(BF16)
                for j4 in range(4):
                    p.tr(pv3[:, j4 * 128:(j4 + 1) * 128], c_['Nb'][:, j4 * 128:(j4 + 1) * 128], p.ident_bf[:], r=[key(c_, 'Nb')], w=[PK(c_['ba'])])
                p.cp(ACT, c_['NTb'][:], pv3[:, 0:512], r=[PK(c_['ba'])], w=[key(c_, 'NTb')])
                p.tt(DVE, c_['NTl'][:], K['lvl'][:, 1 - d, 1:7, :], c_['NTb'][:].unsqueeze(1).to_broadcast([128, 6, 512]), ALU.mult,
                     r=[key(c_, 'NTb')], w=[key(c_, 'NTl')])
                p.tt(DVE, c_['tmpb'][:], c_['Nb'][:], K['lvl'][:, d, 0, :], ALU.mult, r=[key(c_, 'Nb')], w=[key(c_, 'tmpb')])
                p.tt(DVE, c_['X0'][:], c_['tmpb'][:], K['i4'][:], ALU.add, r=[key(c_, 'tmpb')], w=[key(c_, 'X0')])
                p.tt(DVE, c_['tmpb'][:], c_['NTb'][:], K['lvl'][:, 1 - d, 0, :], ALU.mult, r=[key(c_, 'NTb'), key(c_, 'tmpb')], w=[key(c_, 'tmpb')])
                p.tt(DVE, c_['XT0'][:], c_['tmpb'][:], K['i4'][:], ALU.add, r=[key(c_, 'tmpb')], w=[key(c_, 'XT0')])

            def st_L1(c_, grp, li):
                xs = (li - 1) % 2
                X, XT = c_['X%d' % xs], c_['XT%d' % xs]
                for j4 in range(4):
                    sl = slice(j4 * 128, (j4 + 1) * 128)
                    p.mm(PS(c_['ba'])[:, sl], c_['NTl'][:, li - 1, sl], X[:, sl], True, True, r=[key(c_, 'NTl'), key(c_, 'X%d' % xs)], w=[PK(c_['ba'])])
                p.cp(ACT, c_['Yb'][:], PS(c_['ba'])[:, :], r=[PK(c_['ba'])], w=[key(c_, 'Yb')])

            def st_L2(c_, grp, li):
                xs = (li - 1) % 2
                X, XT = c_['X%d' % xs], c_['XT%d' % xs]
                Xn, XTn = c_['X%d' % (1 - xs)], c_['XT%d' % (1 - xs)]
                for j4 in range(4):
                    sl = slice(j4 * 128, (j4 + 1) * 128)
                    p.mm(PS(c_['ba'] + 1)[:, sl], XT[:, sl], c_['Yb'][:, sl], True, False, r=[key(c_, 'XT%d' % xs), key(c_, 'Yb')], w=[PK(c_['ba'] + 1)])
                    p.mm(PS(c_['ba'] + 1)[:, sl], p.ident_bf[:], X[:, sl], False, True, r=[key(c_, 'X%d' % xs)], w=[PK(c_['ba'] + 1)])
                if li < 6:
                    for j4 in range(4):
                        sl = slice(j4 * 128, (j4 + 1) * 128)
                        p.mm(PS(c_['ba'] + 2)[:, sl], c_['Yb'][:, sl], XT[:, sl], True, False, r=[key(c_, 'XT%d' % xs), key(c_, 'Yb')], w=[PK(c_['ba'] + 2)])
                        p.mm(PS(c_['ba'] + 2)[:, sl], p.ident_bf[:], XT[:, sl], False, True, r=[key(c_, 'XT%d' % xs)], w=[PK(c_['ba'] + 2)])
                p.cp(DVE if li % 2 == 0 else ACT, Xn[:], PS(c_['ba'] + 1)[:, :], r=[PK(c_['ba'] + 1)], w=[key(c_, 'X%d' % (1 - xs))])
                if li < 6:
                    p.cp(ACT if li % 2 == 0 else DVE, XTn[:], PS(c_['ba'] + 2)[:, :], r=[PK(c_['ba'] + 2)], w=[key(c_, 'XT%d' % (1 - xs))])

            def st_S1(c_, grp):
                hd0 = d * 8 + grp * 4
                hq0 = 4 + grp * 2
                p.tt(DVE, c_['kdec'][:].rearrange("q (a b c) -> q a b c", b=2, c=128),
                     qn[:, hq0:hq0 + 2, :].unsqueeze(2).to_broadcast([128, 2, 2, 128]),
                     g[:, 4, hd0:hd0 + 4].rearrange("q (a b) -> q a b", b=2).unsqueeze(3).to_broadcast([128, 2, 2, 128]), ALU.mult,
                     r=[('qkn', bi), gk], w=[key(c_, 'kdec')])
                for j4 in range(4):
                    hd = hd0 + j4
                    hv = grp * 4 + j4
                    sl = slice(j4 * 128, (j4 + 1) * 128)
                    So = Sb[sbi[hd]]
                    p.mm(PS(c_['ba'])[:, sl], kTb[:, hv // 2, :], So[:, hd, :], True, True, r=[('kT', bi), ('Sb', sbi[hd], hd)], w=[PK(c_['ba'])])
                p.tt(DVE, H4(c_['tmpf'][:]), H4(PS(c_['ba'])[:, :]), bci(g[:, 3, hd0:hd0 + 4]), ALU.mult, r=[PK(c_['ba']), gk], w=[key(c_, 'tmpf')])
                v0 = 1024 + grp * 512
                p.tt(DVE, c_['rb'][:], c_['tmpf'][:], qb[:, v0:v0 + 512], ALU.add, r=[key(c_, 'tmpf'), ('qkvb', bi)], w=[key(c_, 'rb')])

            def st_S2(c_, grp):
                hd0 = d * 8 + grp * 4
                X = c_['X0']
                for j4 in range(4):
                    sl = slice(j4 * 128, (j4 + 1) * 128)
                    p.mm(PS(c_['ba'] + 1)[:, sl], X[:, sl], c_['rb'][:, sl], True, True, r=[key(c_, 'X0'), key(c_, 'rb')], w=[PK(c_['ba'] + 1)])
                for j4 in range(4):
                    hd = hd0 + j4
                    sl = slice(j4 * 128, (j4 + 1) * 128)
                    p.act(c_['ub'][:, sl], PS(c_['ba'] + 1)[:, sl], AF.Copy, r=[PK(c_['ba'] + 1), 'bts'], w=[key(c_, 'ub')], scale=bts[:, t, hd:hd + 1])

            def st_S3(c_, grp):
                hd0 = d * 8 + grp * 4
                for j4 in range(4):
                    sl = slice(j4 * 128, (j4 + 1) * 128)
                    p.mm(PS(c_['ba'])[:, sl], c_['kdec'][:, sl], c_['ub'][:, sl], True, True, r=[key(c_, 'kdec'), key(c_, 'ub')], w=[PK(c_['ba'])])
                if need_out:
                    for j4 in range(4):
                        hd = hd0 + j4
                        hv = grp * 4 + j4
                        sl = slice(j4 * 128, (j4 + 1) * 128)
                        So = Sb[sbi[hd]]
                        p.mm(PS(c_['ba'] + 1)[:, sl], qTb[:, hv // 2, :], So[:, hd, :], True, True, r=[('qT', bi), ('Sb', sbi[hd], hd)], w=[PK(c_['ba'] + 1)])
                    for j4 in range(4):
                        hd = hd0 + j4
                        sl = slice(j4 * 128, (j4 + 1) * 128)
                        p.act(c_['o1'][:, sl], PS(c_['ba'] + 1)[:, sl], AF.Copy, r=[PK(c_['ba'] + 1), gk], w=[key(c_, 'o1')], scale=g[:, 2, hd:hd + 1])
                    for j4 in range(4):
                        sl = slice(j4 * 128, (j4 + 1) * 128)
                        p.mm(PS(c_['ba'] + 2)[:, sl], c_['qkp'][:, sl], c_['ub'][:, sl], True, True, r=[key(c_, 'qkp'), key(c_, 'ub')], w=[PK(c_['ba'] + 2)])
                    p.tt(DVE, obuf[:, grp * 512:(grp + 1) * 512], PS(c_['ba'] + 2)[:, :], c_['o1'][:], ALU.add, r=[PK(c_['ba'] + 2), key(c_, 'o1')],
                         w=[('ob', step % 2, grp)])
                hk = [('Sf', hd0 + j) for j in range(4)]
                p.tt(DVE, Sf[:, hd0:hd0 + 4, :], Sf[:, hd0:hd0 + 4, :], bci(g[:, 5, hd0:hd0 + 4]), ALU.mult, r=hk + [gk], w=hk)
                p.tt(DVE, Sf[:, hd0:hd0 + 4, :], Sf[:, hd0:hd0 + 4, :], H4(PS(c_['ba'])[:, :]), ALU.add, r=hk + [PK(c_['ba'])], w=hk)
                nb_ = 1 - sbi[hd0]
                p.cp(ACT, Sb[nb_][:, hd0:hd0 + 4, :], Sf[:, hd0:hd0 + 4, :], r=hk, w=[('Sb', nb_, hd0 + j) for j in range(4)])
                for j in range(4):
                    sbi[hd0 + j] = nb_

            stages = [st_D, st_E, st_NT]
            for li in range(1, 7):
                stages.append(lambda c_, grp, li=li: st_L1(c_, grp, li))
                stages.append(lambda c_, grp, li=li: st_L2(c_, grp, li))
            stages += [st_S1, st_S2, st_S3]
            for stg in stages:
                sl_ = []
                stage_ll.append(sl_)
                rec_into(sl_)
                for grp in range(2):
                    stg(CT[grp], grp)
            rec_into(tail_l)
            if need_out:
                p.dma(SP, p.Od[d][(t - OWN0) * 128:(t - OWN0 + 1) * 128, :], obuf[:], r=[('ob', step % 2, 0), ('ob', step % 2, 1)],
                      w=[('Od', d, t)])
            step += 1
            if p.debug in ('G0', 'G') and (t, d) == (32, 1):
                o = p.out("dbg_S", [128, 16, 128], F32)
                p.dma(SP, o, Sf[:], r=[('Sf', h) for h in range(16)])
        p.S.op = real_op
        for o_ in recs[0][0]:
            real_op(*o_)
        for i_, (prep_l, stage_ll, tail_l) in enumerate(recs):
            nxt = list(recs[i_ + 1][0]) if i_ + 1 < len(recs) else []
            per = -(-len(nxt) // max(1, len(stage_ll) - 2)) if nxt else 0
            for sl_ in stage_ll:
                for o_ in sl_:
                    real_op(*o_)
                for _ in range(per):
                    if nxt:
                        real_op(*nxt.pop(0))
            for o_ in nxt:
                real_op(*o_)
            for o_ in tail_l:
                real_op(*o_)
        p.S.barrier()
        stG.close()
        if p.debug == 'G':
            for d in range(2):
                o = p.out("dbg_O%d" % d, [16 * 128, 1024], F32)
                p.dma(SP, o, p.Od[d], r=[])


    def bcast_rows(p, dst, vec, key):
        dg = p.bc_dg
        for c in range(8):
            p.ts(DVE, dg[:, c * 128:(c + 1) * 128], p.ident_f[:], vec(c), None, ALU.mult, r=['modsb', 'vecs'], w=['bc_dg'])
        for hf_ in range(2):
            p.mm(p.psb[7][:, :], p.ones_f[:], dg[:, hf_ * 512:(hf_ + 1) * 512], True, True, r=['bc_dg'], w=[('ps', 7)])
            p.cp(ACT, dst[:, hf_ * 512:(hf_ + 1) * 512], p.psb[7][:, :], r=[('ps', 7)], w=[key])

    def phaseD(p):
        x = p.inp("x_seq", [L, D], F32) if "x_seq" not in p.din else p.din["x_seq"]
        w_in = p.din["w_in"]
        tabs = p.inp("dft_tab", [4, 4, 128, 2, 8, 512], BF16)
        cdft = p.inp("cdft", [128, 2, 128], BF16)
        gng = p.inp("gng_t", [1024], F32)
        wfo = p.inp("w_fo", [512, D], F32)
        wgo = p.inp("w_go", [D, D], F32)
        wmo = p.inp("w_mo", [D, D], F32)
        wr = p.inp("w_router", [D, NE], F32)
        br = p.inp("b_router", [NE], F32)
        p.X1 = p.scratch("X1", [2048, D], F32)
        p.H = p.scratch("H", [2049, D], BF16)
        p.logits = p.sb("logits", [128, 16, NE], F32)
        p.g2_b = p.sb("g2_b", [128, 1024], F32)
        stD = contextlib.ExitStack()
        p.bc_dg = p.sb("bc_dg", [128, 1024], F32, stD)
        p.bcast_rows(p.g2_b, lambda c: p.modsb[:, 40 + c, 0:1], 'g2_b')
        fmT = p.sb("fmT", [128, 4, 2048], BF16, stD)
        st1 = contextlib.ExitStack()
        Xs = p.sb("Xs", [128, 32, 512], BF16, st1)
        for lc in range(32):
            p.dma(SP, Xs[:, lc, :], p.XF[lc * 128:(lc + 1) * 128, :], w=[('Xs', lc)])
        tb = [p.sb("tb%d" % i, [128, 2, 8, 512], BF16, st1) for i in range(2)]
        cd = p.sb("cd", [128, 2, 128], BF16, st1)
        p.dma(SP, cd[:], cdft, w=['cd'])
        AB = p.sb("AB", [128, 8, 512], BF16, st1)
        SC = float(1.0 / np.sqrt(4096.0 * 128.0))
        nl = 0
        for kt in range(4):
            for q4 in range(4):
                tbuf = tb[nl % 2]
                tk = ('tb', nl % 2)
                nl += 1
                p.dma(SP, tbuf[:], tabs[kt, q4], w=[tk])
                for lc in range(8):
                    la = q4 * 8 + lc
                    for g_ in range(4):
                        for ab in range(2):
                            p.mm(p.psb[g_ * 2 + ab][:, :], Xs[:, la, g_ * 128:(g_ + 1) * 128], tbuf[:, ab, lc, :], la == 0, la == 31,
                                 r=[('Xs', la), tk], w=[('ps', g_ * 2 + ab)])
            for i8 in range(8):
                p.cp(ACT if i8 % 2 == 0 else DVE, AB[:, i8, :], p.psb[i8][:, :], r=[('ps', i8)], w=[('AB', i8)])
            for g_ in range(4):
                p.mm(p.psb[g_][:, :], cd[:, 0, :], AB[:, g_ * 2, :], True, False, r=['cd', ('AB', g_ * 2)], w=[('ps', g_)])
                p.mm(p.psb[g_][:, :], cd[:, 1, :], AB[:, g_ * 2 + 1, :], False, True, r=['cd', ('AB', g_ * 2 + 1)], w=[('ps', g_)])
                p.act(fmT[:, g_, kt * 512:(kt + 1) * 512], p.psb[g_][:, :], AF.Copy, r=[('ps', g_)], w=[('fmT', g_, kt)], scale=SC)
        p.S.barrier()
        st1.close()
        if p.debug == 'D1':
            o = p.out("dbg_fmT", [128, 4, 2048], BF16)
            p.dma(SP, o, fmT[:], r=[])
            p.S.barrier(); stD.close()
            return
        st2 = stD
        wv = w_in.rearrange("(kc q) n -> q kc n", q=128)
        Wzg = p.sb("Wzg", [128, 8, 3072], BF16, st2)
        for kc in range(8):
            p.dma(POOL, Wzg[:, kc, 0:1024], wv[:, kc, Z_OFF:Z_OFF + 1024], w=['Wzg'])
            p.dma(POOL, Wzg[:, kc, 1024:3072], wv[:, kc, GA_OFF:GA_OFF + 2048], w=['Wzg'])
        Wfo = p.sb("Wfo", [128, 4, D], BF16, st2)
        p.dma(POOL, Wfo[:], wfo.rearrange("(kc q) n -> q kc n", q=128), w=['Wfo'])
        Wgo = p.sb("Wgo", [128, 8, D], BF16, st2)
        p.dma(POOL, Wgo[:], wgo.rearrange("(kc q) n -> q kc n", q=128), w=['Wgo'])
        Wmo = p.sb("Wmo", [128, 8, D], BF16, st2)
        p.dma(POOL, Wmo[:], wmo.rearrange("(kc q) n -> q kc n", q=128), w=['Wmo'])
        Wr = p.sb("Wr", [128, 8, NE], F32, st2)
        p.dma(SP, Wr[:], wr.rearrange("(kc q) n -> q kc n", q=128), w=['Wr'])
        brb = p.sb("brb", [128, NE], F32, st2)
        p.dma(SP, brb[:], br.partition_broadcast(128), w=['brb'])
        gnb = p.sb("gnb", [128, 1024], F32, st2)
        p.dma(SP, gnb[:], gng.partition_broadcast(128), w=['gnb'])
        g1_b = p.sb("g1_b", [128, 1024], F32, st2)
        gs2_b = p.sb("gs2_b", [128, 1024], F32, st2)
        sh2_b = p.sb("sh2_b", [128, 1024], F32, st2)
        p.bcast_rows(g1_b, lambda c: p.modsb[:, 16 + c, 0:1], 'g1_b')
        p.bcast_rows(gs2_b, lambda c: p.vecs[:, 2, c:c + 1], 'gs2_b')
        p.bcast_rows(sh2_b, lambda c: p.modsb[:, 24 + c, 0:1], 'sh2_b')
        zrow = p.sb("zrow", [1, D], BF16, st2)
        p.S.op(DVE, lambda e: e.memset(zrow[:], 0.0), [], ['zrow'])
        p.dma(SP, p.H[2048:2049, :], zrow[:], r=['zrow'], w=[('H', 'z')])
        p.S.barrier()
        uT = p.sb("uTd", [128, 8, 512], BF16, st2)
        ybT = p.sb("ybT", [128, 8, 512], BF16, st2)
        mT = p.sb("mT", [128, 8, 512], BF16, st2)
        bufs = {
            'x': (p.sb("xtd", [128, D], F32, st2), 'xtd'),
            'junk': (p.sb("junkd", [128, D], BF16, st2), 'junkd'),
            'ss': (p.sb("ssd", [128, 4], F32, st2), 'ssd'),
            'xn': (p.sb("xnd", [128, D], BF16, st2), 'xnd'),
            'ps': (p.psb[0], ('ps', 0)),
        }
        zs = p.sb("zs", [128, D], F32, st2)
        of_ = p.sb("of_", [128, D], F32, st2)
        ob_ = p.sb("ob_", [128, D], F32, st2)
        on8 = p.sb("on8", [128, 4, 8], F32, st2)
        ybin = p.sb("ybin", [128, D], BF16, st2)
        gw = [p.sb("gw%d" % i, [128, 512], BF16, st2) for i in range(4)]
        x1 = p.sb("x1", [128, D], F32, st2)
        xt2 = p.sb("xt2d", [128, D], F32, st2)
        h2 = p.sb("h2", [128, D], F32, st2)
        h2b = p.sb("h2b", [128, D], BF16, st2)
        h2T = p.sb("h2T", [128, 8, 128], F32, st2)
        s2 = p.sb("s2", [128, 4], F32, st2)
        real_op = p.S.op
        recD = []

        def rec_into(lst):
            p.S.op = lambda eng, fn, reads=(), writes=(), dma=False: lst.append((eng, fn, list(reads), list(writes), dma))

        for sti in range(4):
            head_l, mid_l, tail_l = [], [], []
            recD.append((head_l, mid_l, tail_l))
            rec_into(head_l)
            for ti in range(4):
                tt_ = sti * 4 + ti
                t = OWN0 + tt_
                p.norm_T(x[t * 128:(t + 1) * 128, :], p.gs1, p.sh1, lambda c: (uT[:, c, ti * 128:(ti + 1) * 128], ('uTd', ti)), bufs, 'd')
                for hf_ in range(2):
                    ps = p.psb[1 + hf_]
                    for kc in range(8):
                        p.mm(ps[:, :], uT[:, kc, ti * 128:(ti + 1) * 128], Wzg[:, kc, hf_ * 512:(hf_ + 1) * 512], kc == 0, kc == 7,
                             r=[('uTd', ti), 'Wzg'], w=[('ps', 1 + hf_)])
                    sl = slice(hf_ * 512, (hf_ + 1) * 512)
                    p.act(zs[:, sl], ps[:, :], AF.Silu, r=[('ps', 1 + hf_)], w=[('zs', hf_)])
                p.dma(SP, of_[:], p.Od[0][tt_ * 128:(tt_ + 1) * 128, :], w=['of_'])
                p.dma(SP, ob_[:], p.Od[1][tt_ * 128:(tt_ + 1) * 128, :], w=['ob_'])
                p.tt(DVE, of_[:], of_[:], ob_[:], ALU.add, r=['of_', 'ob_'], w=['of_'])
                p.tt(DVE, ob_[:], of_[:], of_[:], ALU.mult, r=['of_', 'ob_'], w=['ob_'])
                p.S.op(DVE, lambda e: e.tensor_reduce(on8[:, 0, :], ob_[:].rearrange("q (h c) -> q h c", c=128), AX.X, ALU.add), ['ob_'], ['on8'])
                p.ts(DVE, on8[:, 1, :], on8[:, 0, :], 1.0 / 128, EPS, ALU.mult, ALU.add, r=['on8'], w=['on8'])
                p.act(on8[:, 2, :], on8[:, 1, :], AF.Ln, r=['on8'], w=['on8'])
                p.act(on8[:, 3, :], on8[:, 2, :], AF.Exp, r=['on8'], w=['on8'], scale=-0.5)
                for h in range(8):
                    hs = slice(h * 128, (h + 1) * 128)
                    p.stt(DVE, of_[:, hs], of_[:, hs], on8[:, 3, h:h + 1], gnb[:, hs], ALU.mult, ALU.mult, r=['of_', 'on8', 'gnb'], w=['of_'])
                p.tt(DVE, ybin[:], of_[:], zs[:], ALU.mult, r=['of_', ('zs', 0), ('zs', 1)], w=['ybin'])
                pv = p.psb[3][:].bitcast(BF16)
                for c in range(8):
                    p.tr(pv[:, c * 128:(c + 1) * 128], ybin[:, c * 128:(c + 1) * 128], p.ident_bf[:], r=['ybin'], w=[('ps', 3)])
                p.cp(ACT, ybT[:, :, ti * 128:(ti + 1) * 128], pv[:, :].rearrange("q (c k) -> q c k", k=128), r=[('ps', 3)], w=[('ybT', ti)])
            rec_into(mid_l)
            allu = [('uTd', i) for i in range(4)]
            ally = [('ybT', i) for i in range(4)]
            k0 = sti * 512
            for dc in range(8):
                dsl = slice(dc * 128, (dc + 1) * 128)
                b0 = 4 * (dc % 2)
                for fc in range(4):
                    p.mm(p.psb[b0][:, :], Wfo[:, fc, dsl], fmT[:, fc, k0:k0 + 512], fc == 0, fc == 3, r=['Wfo'], w=[('ps', b0)])
                for fc in range(8):
                    p.mm(p.psb[b0 + 1][:, :], Wgo[:, fc, dsl], ybT[:, fc, :], fc == 0, fc == 7, r=['Wgo'] + ally, w=[('ps', b0 + 1)])
                for kc in range(8):
                    p.mm(p.psb[b0 + 2][:, :], Wzg[:, kc, 1024 + dc * 128:1024 + (dc + 1) * 128], uT[:, kc, :], kc == 0, kc == 7, r=['Wzg'] + allu, w=[('ps', b0 + 2)])
                for kc in range(8):
                    p.mm(p.psb[b0 + 3][:, :], Wzg[:, kc, 2048 + dc * 128:2048 + (dc + 1) * 128], uT[:, kc, :], kc == 0, kc == 7, r=['Wzg'] + allu, w=[('ps', b0 + 3)])
                for br_, (pg, py) in enumerate([(b0 + 2, b0), (b0 + 3, b0 + 1)]):
                    gb_ = gw[(dc % 2) * 2 + br_]
                    gk_ = ('gw', (dc % 2) * 2 + br_)
                    p.act(gb_[:], p.psb[pg][:, :], AF.Sigmoid, r=[('ps', pg)], w=[gk_])
                    p.tt(DVE, gb_[:], gb_[:], p.psb[py][:, :], ALU.mult, r=[gk_, ('ps', py)], w=[gk_])
                p.tt(DVE, mT[:, dc, :], gw[(dc % 2) * 2][:], gw[(dc % 2) * 2 + 1][:], ALU.add, r=[('gw', (dc % 2) * 2), ('gw', (dc % 2) * 2 + 1)], w=[('mT', dc)])
            allm = [('mT', i) for i in range(8)]
            rec_into(tail_l)
            for ti in range(4):
                tt_ = sti * 4 + ti
                t = OWN0 + tt_
                xt, xk = xt2, 'xt2'
                p.dma(SP, xt[:], x[t * 128:(t + 1) * 128, :], w=[xk])
                for hf_ in range(2):
                    ps = p.psb[4 + hf_]
                    sl = slice(hf_ * 512, (hf_ + 1) * 512)
                    for dc in range(8):
                        p.mm(ps[:, :], mT[:, dc, ti * 128:(ti + 1) * 128], Wmo[:, dc, sl], dc == 0, dc == 7, r=allm + ['Wmo'], w=[('ps', 4 + hf_)])
                    p.tt(DVE, x1[:, sl], ps[:, :], g1_b[:, sl], ALU.mult, r=[('ps', 4 + hf_)], w=['x1'])
                p.tt(DVE, x1[:], x1[:], xt[:], ALU.add, r=['x1', xk], w=['x1'])
                p.dma(POOL, p.X1[tt_ * 128:(tt_ + 1) * 128, :], x1[:], r=['x1'], w=[('X1', tt_)])
                p.act(h2[:], x1[:], AF.Square, r=['x1'], w=['h2', 's2'], accum_out=s2[:, 0:1])
                p.act(s2[:, 2:3], s2[:, 0:1], AF.Ln, r=['s2'], w=['s2'], scale=1.0 / D, bias=p.eps_t[:, 0:1])
                p.act(s2[:, 3:4], s2[:, 2:3], AF.Exp, r=['s2'], w=['s2'], scale=-0.5)
                p.stt(DVE, h2[:], x1[:], s2[:, 3:4], gs2_b[:], ALU.mult, ALU.mult, r=['x1', 's2', 'h2'], w=['h2'])
                p.tt(DVE, h2[:], h2[:], sh2_b[:], ALU.add, r=['h2'], w=['h2'])
                p.cp(ACT, h2b[:], h2[:], r=['h2'], w=['h2b'])
                p.dma(POOL, p.H[tt_ * 128:(tt_ + 1) * 128, :], h2b[:], r=['h2b'], w=[('H', tt_)])
                for hf_ in range(2):
                    for c in range(4):
                        p.tr(p.psb[6][:, c * 128:(c + 1) * 128], h2[:, (hf_ * 4 + c) * 128:(hf_ * 4 + c + 1) * 128], p.ident_f[:], r=['h2'], w=[('ps', 6)])
                    p.cp(ACT, h2T[:, hf_ * 4:(hf_ + 1) * 4, :], p.psb[6][:, :].rearrange("q (c k) -> q c k", k=128), r=[('ps', 6)], w=['h2T'])
                for kc in range(8):
                    p.mm(p.psb[7][:, 0:NE], h2T[:, kc, :], Wr[:, kc, :], kc == 0, kc == 7, r=['h2T', 'Wr'], w=[('ps', 7)])
                p.tt(DVE, p.logits[:, tt_, :], p.psb[7][:, 0:NE], brb[:], ALU.add, r=[('ps', 7), 'brb'], w=[('logits', tt_)])
        p.S.op = real_op
        for o_ in recD[0][0]:
            real_op(*o_)
        for i_, (head_l, mid_l, tail_l) in enumerate(recD):
            for o_ in mid_l:
                real_op(*o_)
            nxt = list(recD[i_ + 1][0]) if i_ + 1 < len(recD) else []
            tl = list(tail_l)
            CH = 6
            while tl or nxt:
                for _ in range(CH):
                    if tl:
                        real_op(*tl.pop(0))
                for _ in range(CH):
                    if nxt:
                        real_op(*nxt.pop(0))
        p.S.barrier()
        stD.close()
        if p.debug == 'D':
            o = p.out("dbg_x1", [2048, D], F32)
            p.dma(SP, o, p.X1, r=[])
            o = p.out("dbg_logits", [128, 16, NE], F32)
            p.dma(SP, o, p.logits[:], r=[])
            o = p.out("dbg_H", [2049, D], BF16)
            p.dma(SP, o, p.H, r=[])


    def phaseE(p):
        CAPS = MOE_CAPS
        TB = [sum(CAPS[:i]) for i in range(NE)]
        NB = sum(CAPS)
        DUMMY = NB * 128
        CAPMAX = max(CAPS) * 128
        wg = p.inp("w_gate", [NE, D, D], F32)
        wu = p.inp("w_up", [NE, D, D], F32)
        wd = p.inp("w_down", [NE, D, D], F32)
        bg = p.inp("b_gate", [NE, D], F32)
        bu = p.inp("b_up", [NE, D], F32)
        bd = p.inp("b_down", [NE, D], F32)
        fng = p.inp("fng", [D], F32)
        yout = p.out("y", [2048, D], F32)
        Y = p.scratch("Yslots", [NB * 128 + 128, D], F32)
        IDX = p.scratch("IDX", [NB * 128 + 128, 1], I32)
        stE = contextlib.ExitStack()
        K = {}
        for nm, dt_ in [('ones_bf', BF16), ('ustrict_bf', BF16), ('iota32', F32), ('ecol', F32), ('blk128', F32),
                        ('tokid', I32), ('l_strict', F32), ('basetab', F32), ('captab', F32), ('rowoff', F32)]:
            a = p.const_np[nm]
            d = p.inp("c_" + nm, a.shape, dt_) if ("c_" + nm) not in p.din else p.din["c_" + nm]
            K[nm] = p.sb("ke_" + nm, a.shape, dt_, stE)
            p.dma(SP, K[nm][:], d, w=[('k', nm)])
        Bg = p.sb("Bg", [32, D], BF16, stE)
        Bu = p.sb("Bu", [32, D], BF16, stE)
        Bd = p.sb("Bd", [32, D], BF16, stE)
        p.dma(POOL, Bg[:], bg, w=['Bg'])
        p.dma(POOL, Bu[:], bu, w=['Bu'])
        p.dma(POOL, Bd[:], bd, w=['Bd'])
        fnb = p.sb("fnb", [128, D], F32, stE)
        p.dma(SP, fnb[:], fng.partition_broadcast(128), w=['fnb'])
        i2048 = p.sb("i2048", [128, NB], I32, stE)
        p.S.op(DVE, lambda e: e.memset(i2048[:], 2048), [], ['i2048'])
        p.dma(SP, IDX[0:NB * 128, :].rearrange("(q b) o -> q (b o)", q=128), i2048[:], r=['i2048'], w=['IDX'])
        zy = p.sb("zy", [128, D], F32, stE)
        p.S.op(DVE, lambda e: e.memset(zy[:], 0.0), [], ['zy'])
        p.dma(SP, Y[DUMMY:DUMMY + 128, :], zy[:], r=['zy'], w=['Yz'])
        p.S.barrier()
        mx = p.sb("mx", [128, 16, 8], F32, stE)
        mi = p.sb("mi", [128, 16, 8], U32, stE)
        idf = p.sb("idf", [128, 16, 4], F32, stE)
        wts = p.sb("wts", [128, 16, 4], F32, stE)
        nm0 = p.sb("nm0", [128, 16], F32, stE)
        ssum = p.sb("ssum", [128, 16], F32, stE)
        Mf = p.sb("Mf", [128, 16, NE], F32, stE)
        Mb = p.sb("Mb", [128, 16, NE], BF16, stE)
        for tt_ in range(16):
            p.S.op(DVE, lambda e, tt_=tt_: e.max(mx[:, tt_, :], p.logits[:, tt_, :]), [], [('mx', tt_)])
            p.S.op(DVE, lambda e, tt_=tt_: e.max_index(mi[:, tt_, :], mx[:, tt_, :], p.logits[:, tt_, :]), [('mx', tt_)], [('mi', tt_)])
            p.cp(DVE, idf[:, tt_, :], mi[:, tt_, 0:4], r=[('mi', tt_)], w=[('idf', tt_)])
            p.ts(DVE, nm0[:, tt_:tt_ + 1], mx[:, tt_, 0:1], -1.0, None, ALU.mult, r=[('mx', tt_)], w=[('nm0', tt_)])
            p.act(wts[:, tt_, :], mx[:, tt_, 0:4], AF.Exp, r=[('mx', tt_), ('nm0', tt_)], w=[('wts', tt_)], bias=nm0[:, tt_:tt_ + 1], scale=1.0)
            p.S.op(DVE, lambda e, tt_=tt_: e.tensor_reduce(ssum[:, tt_:tt_ + 1], wts[:, tt_, :], AX.X, ALU.add), [('wts', tt_)], [('ssum', tt_)])
            p.S.op(DVE, lambda e, tt_=tt_: e.reciprocal(ssum[:, tt_:tt_ + 1], ssum[:, tt_:tt_ + 1]), [('ssum', tt_)], [('ssum', tt_)])
            p.ts(DVE, wts[:, tt_, :], wts[:, tt_, :], ssum[:, tt_:tt_ + 1], None, ALU.mult, r=[('wts', tt_), ('ssum', tt_)], w=[('wts', tt_)])
            p.ts(DVE, Mf[:, tt_, :], K['iota32'][:], idf[:, tt_, 0:1], None, ALU.is_equal, r=[('idf', tt_)], w=[('Mf', tt_)])
            for j in range(1, 4):
                p.stt(DVE, Mf[:, tt_, :], K['iota32'][:], idf[:, tt_, j:j + 1], Mf[:, tt_, :], ALU.is_equal, ALU.add, r=[('idf', tt_), ('Mf', tt_)], w=[('Mf', tt_)])
            p.cp(DVE, Mb[:, tt_, :], Mf[:, tt_, :], r=[('Mf', tt_)], w=[('Mb', tt_)])
        allM = [('Mb', i) for i in range(16)]
        for tt_ in range(16):
            p.mm(p.psb[0][:, 0:NE], K['ones_bf'][:], Mb[:, tt_, :], tt_ == 0, tt_ == 15, r=allM, w=[('ps', 0)])
        for tt_ in range(16):
            p.mm(p.psb[1][0:32, 0:1], Mb[:, tt_, :], K['ones_bf'][:, 0:1], tt_ == 0, tt_ == 15, r=allM, w=[('ps', 1)])
        cntb = p.sb("cntb", [128, NE], F32, stE)
        cc = p.sb("cc", [32, 8], F32, stE)
        sq32 = p.sb("sq32", [32, 3, NE], F32, stE)
        Pm = p.sb("Pm", [32, NE], F32, stE)
        p.cp(DVE, cntb[:], p.psb[0][:, 0:NE], r=[('ps', 0)], w=['cntb'])
        p.cp(DVE, cc[:, 0:1], p.psb[1][0:32, 0:1], r=[('ps', 1)], w=['cc'])
        p.ts(DVE, sq32[:, 0, :], cntb[0:32, :], cc[:, 0:1], None, ALU.is_gt, r=['cntb', 'cc'], w=['sq32'])
        p.ts(DVE, sq32[:, 1, :], cntb[0:32, :], cc[:, 0:1], None, ALU.is_equal, r=['cntb', 'cc'], w=['sq32'])
        p.tt(DVE, sq32[:, 1, :], sq32[:, 1, :], K['l_strict'][0:32, 0:32], ALU.mult, r=['sq32'], w=['sq32'])
        p.tt(DVE, sq32[:, 0, :], sq32[:, 0, :], sq32[:, 1, :], ALU.add, r=['sq32'], w=['sq32'])
        p.S.op(DVE, lambda e: e.tensor_reduce(cc[:, 1:2], sq32[:, 0, :], AX.X, ALU.add), ['sq32'], ['cc'])
        p.ts(DVE, Pm[:], K['iota32'][0:32, :], cc[:, 1:2], None, ALU.is_equal, r=['cc'], w=['Pm'])
        p.tt(DVE, sq32[:, 0, :], Pm[:], K['basetab'][0:32, :], ALU.mult, r=['Pm', 'sq32'], w=['sq32'])
        p.S.op(DVE, lambda e: e.tensor_reduce(cc[:, 2:3], sq32[:, 0, :], AX.X, ALU.add), ['sq32'], ['cc'])
        p.tt(DVE, sq32[:, 1, :], Pm[:], K['captab'][0:32, :], ALU.mult, r=['Pm', 'sq32'], w=['sq32'])
        p.S.op(DVE, lambda e: e.tensor_reduce(cc[:, 3:4], sq32[:, 1, :], AX.X, ALU.add), ['sq32'], ['cc'])
        lb = p.sb("lb", [32, 3, 128], F32, stE)
        one32f = p.sb("one32f", [32, 128], F32, stE)
        p.S.op(DVE, lambda e: e.memset(one32f[:], 1.0), [], ['one32f'])
        p.ts(DVE, lb[:, 0, :], one32f[:], cc[:, 2:3], None, ALU.mult, r=['one32f', 'cc'], w=['lb'])
        p.ts(DVE, lb[:, 1, :], one32f[:], cc[:, 3:4], None, ALU.mult, r=['one32f', 'cc'], w=['lb'])
        p.ts(DVE, lb[:, 2, :], one32f[:], K['ecol'][0:32, 0:1], None, ALU.mult, r=['one32f'], w=['lb'])
        p.mm(p.psb[0][:, 0:NE], lb[:, 0, :], p.ident_f[0:32, 0:32], True, True, r=['lb'], w=[('ps', 0)])
        p.mm(p.psb[0][:, 32:64], lb[:, 1, :], p.ident_f[0:32, 0:32], True, True, r=['lb'], w=[('ps', 0)])
        p.mm(p.psb[0][:, 64:96], lb[:, 2, :], Pm[:], True, True, r=['lb', 'Pm'], w=[('ps', 0)])
        bcb = p.sb("bcb", [128, 3, NE], F32, stE)
        p.ts(DVE, bcb[:, 0, :], p.psb[0][:, 0:NE], -float(DUMMY), None, ALU.add, r=[('ps', 0)], w=['bcb'])
        p.cp(DVE, bcb[:, 1, :], p.psb[0][:, 32:64], r=[('ps', 0)], w=['bcb'])
        p.ts(DVE, bcb[:, 2, :], p.psb[0][:, 64:96], 128.0, None, ALU.mult, r=[('ps', 0)], w=['bcb'])
        idwf = p.sb("idwf", [128, NE], F32, stE)
        idw = p.sb("idw", [128, NE], I32, stE)
        p.ts(DVE, idwf[:], bcb[:, 2, :], K['rowoff'][:, 0:1], None, ALU.add, r=['bcb'], w=['idwf'])
        p.cp(DVE, idw[:], idwf[:], r=['idwf'], w=['idw'])
        rk = p.sb("rk", [128, NE], F32, stE)
        sel = p.sb("sel", [128, NE], F32, stE)
        destf = p.sb("destf", [128, 16, 4], F32, stE)
        desti = p.sb("desti", [128, 16, 4], I32, stE)
        for tt_ in range(16):
            ps = p.psb[2 + tt_ % 2]
            pk = ('ps', 2 + tt_ % 2)
            for t2 in range(tt_):
                p.mm(ps[:, 0:NE], K['ones_bf'][:], Mb[:, t2, :], t2 == 0, False, r=allM, w=[pk])
            p.mm(ps[:, 0:NE], K['ustrict_bf'][:], Mb[:, tt_, :], tt_ == 0, True, r=allM, w=[pk])
            p.tt(DVE, sel[:], ps[:, 0:NE], bcb[:, 1, :], ALU.is_lt, r=[pk, 'bcb'], w=['sel'])
            p.tt(DVE, rk[:], ps[:, 0:NE], bcb[:, 0, :], ALU.add, r=[pk, 'bcb'], w=['rk'])
            p.tt(DVE, rk[:], rk[:], sel[:], ALU.mult, r=['rk', 'sel'], w=['rk'])
            p.ts(DVE, rk[:], rk[:], float(DUMMY), None, ALU.add, r=['rk'], w=['rk'])
            for j in range(4):
                p.ts(DVE, sel[:], K['iota32'][:], idf[:, tt_, j:j + 1], None, ALU.is_equal, r=[('idf', tt_)], w=['sel'])
                p.tt(DVE, sel[:], sel[:], rk[:], ALU.mult, r=['sel', 'rk'], w=['sel'])
                p.S.op(DVE, lambda e, tt_=tt_, j=j: e.tensor_reduce(destf[:, tt_, j:j + 1], sel[:], AX.X, ALU.add), ['sel'], [('destf', tt_)])
            p.cp(DVE, desti[:, tt_, :], destf[:, tt_, :], r=[('destf', tt_)], w=[('desti', tt_)])
            for j in range(4):
                p.S.op(POOL, lambda e, tt_=tt_, j=j: e.indirect_dma_start(
                    out=IDX[:, :], out_offset=bass.IndirectOffsetOnAxis(ap=desti[:, tt_, j:j + 1], axis=0),
                    in_=K['tokid'][:, tt_:tt_ + 1], in_offset=None), [('desti', tt_), 'IDX0'], [('IDXs', tt_, j)], dma=True)
        p.S.barrier()
        idx_sb = p.sb("idx_sb", [128, NB], I32, stE)
        p.dma(SP, idx_sb[:], IDX[0:NB * 128, :].rearrange("(b q) o -> q (b o)", q=128), w=['idx_sb'], allow_slow_non_contiguous=True)
        p.S.barrier()
        stX = contextlib.ExitStack()
        Wb = [[p.sb("W%s%d" % (n_, i), [128, 8, D], BF16, stX) for n_ in "gud"] for i in range(2)]
        xg = [p.sb("xg%d" % i, [128, D], BF16, stX) for i in range(2)]
        xT = p.sb("xTe", [128, 8, CAPMAX], BF16, stX)
        aT = p.sb("aT", [128, 8, CAPMAX], BF16, stX)
        ohb = p.sb("ohb", [32, 512], BF16, stX)
        ones32 = p.sb("ones32", [32, 512], BF16, stX)
        p.S.op(DVE, lambda e: e.memset(ones32[:], 1.0), [], ['ones32'])
        wk = [[p.sb("wk%d_%d" % (i, j), [128, 512], BF16, stX) for j in range(4)] for i in range(2)]
        ysb = [p.sb("ysb%d" % i, [128, D], F32, stX) for i in range(2)]
        w2d = [w_.rearrange("e (q j) n -> (e q) (j n)", j=8) for w_ in (wg, wu, wd)]
        ng_ = 0
        nd_ = 0
        nch = 0
        for ex in range(NE):
            wbi = ex % 2
            capt = CAPS[ex]
            for wi_ in range(3):
                p.S.op(POOL, lambda e, wbi=wbi, wi_=wi_, ex=ex: e.indirect_dma_start(
                    out=Wb[wbi][wi_][:].rearrange("q j n -> q (j n)"), out_offset=None, in_=w2d[wi_][:, :],
                    in_offset=bass.IndirectOffsetOnAxis(ap=idw[:, ex:ex + 1], axis=0)), ['idw'], [('W', wbi, wi_)], dma=True)
            p.ts(DVE, ohb[:], ones32[:], Pm[:, ex:ex + 1], None, ALU.mult, r=['ones32', 'Pm'], w=['ohb'])
            for k in range(capt):
                b = TB[ex] + k
                gi = ng_ % 2
                ng_ += 1
                p.S.op(POOL, lambda e, b=b, gi=gi: e.indirect_dma_start(
                    out=xg[gi][:, :], out_offset=None, in_=p.H[:, :],
                    in_offset=bass.IndirectOffsetOnAxis(ap=idx_sb[:, b:b + 1], axis=0)), ['idx_sb'], [('xg', gi)], dma=True)
                pb_ = 0 if k % 2 == 0 else 7
                pv = p.psb[pb_][:].bitcast(BF16)
                xgv = xg[gi][:].rearrange("s (q j) -> s j q", j=8)
                for c in range(8):
                    p.tr(pv[:, c * 128:(c + 1) * 128], xgv[:, c, :], p.ident_bf[:], r=[('xg', gi)], w=[('ps', pb_)])
                p.cp(ACT if k % 2 == 0 else DVE, xT[:, :, k * 128:(k + 1) * 128], pv[:, :].rearrange("q (c k) -> q c k", k=128),
                     r=[('ps', pb_)], w=[('xTe', k)])
            allx = [('xTe', k) for k in range(capt)]
            nsl = capt * 128
            chunks = [(c0, min(512, nsl - c0)) for c0 in range(0, nsl, 512)]
            for fc in range(8):
                fs = slice(fc * 128, (fc + 1) * 128)
                for (c0, n_) in chunks:
                    st_ = nch % 2
                    nch += 1
                    bG, bU = 1 + 2 * st_, 2 + 2 * st_
                    for (wi_, Bt, bk) in ((0, Bg, bG), (1, Bu, bU)):
                        for kc in range(8):
                            p.mm(p.psb[bk][:, 0:n_], Wb[wbi][wi_][:, kc, :].rearrange("q (f j) -> q j f", j=8)[:, fc, :], xT[:, kc, c0:c0 + n_],
                                 kc == 0, False, r=[('W', wbi, wi_)] + allx, w=[('ps', bk)])
                        p.mm(p.psb[bk][:, 0:n_], Bt[:].rearrange("e (f j) -> e j f", j=8)[:, fc, :], ohb[:, 0:n_], False, True, r=['ohb'], w=[('ps', bk)])
                    g_, sg_, u_, t_ = wk[st_]
                    wkk = lambda j: ('wk', st_, j)
                    p.ts(DVE, g_[:, 0:n_], p.psb[bG][:, 0:n_], 7.0, None, ALU.min, r=[('ps', bG)], w=[wkk(0)])
                    p.act(sg_[:, 0:n_], g_[:, 0:n_], AF.Sigmoid, r=[wkk(0)], w=[wkk(1)], scale=1.702)
                    p.ts(DVE, u_[:, 0:n_], p.psb[bU][:, 0:n_], 7.0, -7.0, ALU.min, ALU.max, r=[('ps', bU)], w=[wkk(2)])
                    p.stt(DVE, t_[:, 0:n_], u_[:, 0:n_], 1.0, g_[:, 0:n_], ALU.add, ALU.mult, r=[wkk(2), wkk(0)], w=[wkk(3)])
                    p.tt(DVE, aT[:, fc, c0:c0 + n_], t_[:, 0:n_], sg_[:, 0:n_], ALU.mult, r=[wkk(3), wkk(1)], w=[('aT', fc)])
            alla = [('aT', fc) for fc in range(8)]
            for k in range(capt):
                b = TB[ex] + k
                yi = nd_ % 2
                nd_ += 1
                yb_ = ysb[yi]
                for hf_ in range(2):
                    hs = slice(hf_ * 512, (hf_ + 1) * 512)
                    pb_ = 5 if hf_ == 0 else 6
                    for fc in range(8):
                        p.mm(p.psb[pb_][:, :], aT[:, fc, k * 128:(k + 1) * 128], Wb[wbi][2][:, fc, hs], fc == 0, False, r=alla + [('W', wbi, 2)], w=[('ps', pb_)])
                    p.mm(p.psb[pb_][:, :], ohb[:, 0:128], Bd[:, hs], False, True, r=['ohb'], w=[('ps', pb_)])
                    p.cp(ACT, yb_[:, hs], p.psb[pb_][:, :], r=[('ps', pb_)], w=[('ysb', yi)])
                p.dma(SP, Y[b * 128:(b + 1) * 128, :], yb_[:], r=[('ysb', yi)], w=[('Y', b)])
        p.S.barrier()
        stX.close()
        yg = [p.sb("yg%d" % i, [128, D], F32, stE) for i in range(4)]
        acc = p.sb("acc", [128, D], F32, stE)
        x1ts = [p.sb("x1t%d" % i, [128, D], F32, stE) for i in range(2)]
        outb = [p.sb("outb%d" % i, [128, D], F32, stE) for i in range(2)]
        p.dma(SP, x1ts[0][:], p.X1[0:128, :], w=[('x1t', 0)])
        fs_ = p.sb("fs_", [128, 4], F32, stE)
        ng = 0
        for tt_ in range(16):
            x1t = x1ts[tt_ % 2]
            xk_ = ('x1t', tt_ % 2)
            ot = outb[tt_ % 2]
            ok_ = ('outb', tt_ % 2)
            if tt_ + 1 < 16:
                p.dma(SP, x1ts[(tt_ + 1) % 2][:], p.X1[(tt_ + 1) * 128:(tt_ + 2) * 128, :], w=[('x1t', (tt_ + 1) % 2)])
            for j in range(4):
                yb_ = yg[ng % 4]
                yk = ('yg', ng % 4)
                ng += 1
                p.S.op(POOL, lambda e, tt_=tt_, j=j, yb_=yb_: e.indirect_dma_start(
                    out=yb_[:, :], out_offset=None, in_=Y[:, :],
                    in_offset=bass.IndirectOffsetOnAxis(ap=desti[:, tt_, j:j + 1], axis=0)), [], [yk], dma=True)
                if j == 0:
                    p.ts(DVE, acc[:], yb_[:], wts[:, tt_, 0:1], None, ALU.mult, r=[yk], w=['acc'])
                else:
                    p.stt(DVE, acc[:], yb_[:], wts[:, tt_, j:j + 1], acc[:], ALU.mult, ALU.add, r=[yk, 'acc'], w=['acc'])
            p.tt(DVE, acc[:], acc[:], p.g2_b[:], ALU.mult, r=['acc'], w=['acc'])
            p.tt(DVE, acc[:], acc[:], x1t[:], ALU.add, r=['acc', xk_], w=['acc'])
            p.act(ot[:], acc[:], AF.Square, r=['acc'], w=[ok_, 'fs_'], accum_out=fs_[:, 0:1])
            p.act(fs_[:, 2:3], fs_[:, 0:1], AF.Ln, r=['fs_'], w=['fs_'], scale=1.0 / D, bias=p.eps_t[:, 0:1])
            p.act(fs_[:, 3:4], fs_[:, 2:3], AF.Exp, r=['fs_'], w=['fs_'], scale=-0.5)
            p.stt(DVE, ot[:], acc[:], fs_[:, 3:4], fnb[:], ALU.mult, ALU.mult, r=['acc', 'fs_', ok_], w=[ok_])
            p.dma(SP, yout[tt_ * 128:(tt_ + 1) * 128, :], ot[:], r=[ok_], w=[('yout', tt_)])
        p.S.barrier()
        stE.close()


def core_inputs(inputs, core, consts):
    b, hf = core // 2, core % 2
    rev = (hf == 0)
    m = {}
    x = np.asarray(inputs['x'][b], np.float32)
    m['x_seq'] = np.ascontiguousarray(x[::-1] if rev else x)
    cp = np.stack([inputs['c'][b], inputs['c_ctx']], 0).astype(np.float32)
    m['cT'] = np.ascontiguousarray(cp.reshape(2, 8, 128).transpose(2, 1, 0))
    m['bmod'] = fm_layout(inputs['b_mod'][0], 48)
    m['n1g'] = fm_layout(inputs['norm1_g'][0], 8)
    m['n2g'] = fm_layout(inputs['norm2_g'][0], 8)
    m['w_mod'] = np.ascontiguousarray(inputs['w_mod'][0], np.float32)
    cx = np.asarray(inputs['ctx'][b], np.float32)
    m['ctx_seq'] = np.ascontiguousarray(cx[::-1] if rev else cx)
    w_in = np.array(inputs['w_in'][0], np.float32)
    if rev:
        w2 = w_in.copy()
        w2[:, 2048:2056], w2[:, 2056:2064] = w_in[:, 2056:2064], w_in[:, 2048:2056]
        w2[:, 2064:2072], w2[:, 2072:2080] = w_in[:, 2072:2080], w_in[:, 2064:2072]
        w_in = w2
    m['w_in'] = np.ascontiguousarray(w_in)
    cwv = np.asarray(inputs['conv_w'][0], np.float32).reshape(9, 16, 128)
    if rev:
        cwv = cwv[::-1]
    cd = np.zeros((128, 16, 9, 128), np.float32)
    qi = np.arange(128)
    cd[qi, :, :, qi] = cwv.transpose(2, 1, 0)
    m['conv_diag'] = cd
    al = np.asarray(inputs['a_log'][0], np.float32)
    db = np.asarray(inputs['dt_bias'][0], np.float32)
    if rev:
        al, db = al[::-1], db[::-1]
    m['alog_t'] = np.ascontiguousarray(np.tile(al.reshape(1, 16), (NT + 2, 1)))
    m['dtb_t'] = np.ascontiguousarray(np.tile(db.reshape(1, 16), (NT + 2, 1)))
    pos = np.arange(L)[::-1] if rev else np.arange(L)
    own = pos[2048:]
    ang = (2.0 * np.pi / L) * ((pos[:, None].astype(np.int64) * own[None, :].astype(np.int64)) % L)
    tab = np.stack([np.cos(ang), np.sin(ang)], 0).astype(np.float32)
    tab = tab.reshape(2, 4, 8, 128, 4, 512).transpose(4, 1, 3, 0, 2, 5)
    m['dft_tab'] = bf(tab)
    cang = (2.0 * np.pi / 128) * ((np.arange(128)[:, None] * np.arange(128)[None, :]) % 128)
    m['cdft'] = bf(np.stack([np.cos(cang), -np.sin(cang)], 1))
    m['gng_t'] = np.ascontiguousarray(np.tile(np.asarray(inputs['gdn_norm_g'][0], np.float32), 8))
    m['w_fo'] = np.ascontiguousarray(inputs['w_fourier_out'][0], np.float32)
    m['w_go'] = np.ascontiguousarray(inputs['w_gdn_out'][0], np.float32)
    m['w_mo'] = np.ascontiguousarray(inputs['w_merge_out'][0], np.float32)
    m['w_router'] = np.ascontiguousarray(inputs['w_router'][0], np.float32)
    m['b_router'] = np.ascontiguousarray(inputs['b_router'][0], np.float32)
    for nm_, key in [('w_gate', 'w_gate'), ('w_up', 'w_up'), ('w_down', 'w_down'), ('b_gate', 'b_gate'), ('b_up', 'b_up'), ('b_down', 'b_down')]:
        m[nm_] = np.ascontiguousarray(inputs[key][0], np.float32)
    m['fng'] = np.ascontiguousarray(inputs['final_norm_g'], np.float32)
    for k, v in consts.items():
        m['c_' + k] = v
    return m


def build(debug=None):
    nc = bass.Bass("TRN2", target_bir_lowering=False)
    p = Builder(nc, debug)
    p.phase0()
    if debug == 'AD1':
        p.phaseA()
        p.debug = 'D1'
        p.phaseD()
        p.S.barrier(); p.S.emit(); p.st.close()
        return nc, p
    if debug == 'Gs':
        p.QKVs = p.inp("QKVs_in", [L + CTXL, QKV], BF16)
        p.ba = p.sb("ba", [128, NT + 2, 32], F32)
        p.dma(SP, p.ba[:], p.inp("ba_in", [128, NT + 2, 32], F32), w=['ba'])
        p.S.barrier()
        p.debug = 'G0'
    elif debug in ('Ds1', 'Ds'):
        p.XF = p.inp("XF_in", [L, 512], BF16)
        p.Od = [p.inp("Of_in", [2048, 1024], F32), p.inp("Ob_in", [2048, 1024], F32)]
        p.inp("w_in", [D, IN_COLS], F32)
        p.debug = 'D1' if debug == 'Ds1' else 'D'
    elif debug != '0':
        p.phaseA()
    if debug not in ('0', 'A', 'Ds', 'Ds1'):
        p.phaseG()
    if debug not in ('0', 'A', 'G', 'G0', 'Gs'):
        p.phaseD()
    if debug in (None, 'E'):
        p.phaseE()
    p.S.barrier()
    p.S.emit()
    p.st.close()
    return nc, p


def run(inputs, debug=None):
    nc, p = build(debug)
    maps = []
    for c in range(8):
        m = core_inputs(inputs, c, p.const_np)
        maps.append({k: m[k] for k in p.din})
    res = run_bass_kernel_spmd(nc, maps, core_ids=list(range(8)))
    return res.results


def kernel(**inputs):
    nc, p = build(None)
    maps = []
    for c in range(8):
        m = core_inputs(inputs, c, p.const_np)
        maps.append({k: m[k] for k in p.din})
    res = run_bass_kernel_spmd(nc, maps, core_ids=list(range(8)))
    out = np.zeros((4, L, D), np.float32)
    for c in range(8):
        b, hf = c // 2, c % 2
        y = np.asarray(res.results[c]['y'], np.float32)
        if hf == 1:
            out[b, 2048:] = y
        else:
            out[b, :2048] = y[::-1]
    return out
```

```python
import contextlib
import numpy as np
import ml_dtypes
import concourse.bass as bass
import concourse.mybir as mybir
from concourse.bass_utils import run_bass_kernel_spmd

F32 = mybir.dt.float32
BF16 = mybir.dt.bfloat16
I32 = mybir.dt.int32
U32 = mybir.dt.uint32
AF = mybir.ActivationFunctionType
ALU = mybir.AluOpType
AX = mybir.AxisListType

PE, ACT, DVE, POOL, SP = 'pe', 'act', 'dve', 'pool', 'sp'
ENGS = [PE, ACT, DVE, POOL, SP]
NDS = 8

D = 1024
L = 4096
NT = 32
OWN0 = 16
CTXL = 256
QKV = 2048
BETA_OFF = 2048
A_OFF = 2064
GDN_IN = 2080
Z_OFF = 2080
F_OFF = 3104
GA_OFF = 3616
GB_OFF = 4640
IN_COLS = 5664
NE = 32
EPS = 1e-6
MOE_CAPS = [8] + [6] * 3 + [5] * 4 + [4] * 8 + [3] * 16


class Sched:
    def __init__(s, nc):
        s.nc = nc
        s.ops = {e: [] for e in ENGS}
        s.res = {}
        s.ndma = {e: 0 for e in ENGS}
        s.epoch = 0

    def op(s, eng, fn, reads=(), writes=(), dma=False):
        idx = len(s.ops[eng])
        me = (eng, idx)
        deps = []
        for k in reads:
            r = s.res.get(k)
            if r and r[0] is not None:
                deps.append(r[0])
        for k in writes:
            r = s.res.get(k)
            if r:
                if r[0] is not None:
                    deps.append(r[0])
                deps.extend(r[1])
        waits = []
        best = {}
        for p in deps:
            pe, pi = p
            if s.ops[pe][pi]['dma']:
                waits.append(p)
                continue
            if pe == eng and eng == PE:
                continue
            if pi > best.get(pe, -1):
                best[pe] = pi
        for pe, pi in best.items():
            waits.append((pe, pi))
        o = dict(fn=fn, waits=waits, sig=False, dma=dma, dsem=None, dval=None, dn=0, ep=s.epoch)
        if dma:
            n = s.ndma[eng]
            s.ndma[eng] += 1
            o['dsem'] = n % NDS
            o['dval'] = 16 * (n // NDS + 1)
            o['dn'] = n
        s.ops[eng].append(o)
        for k in reads:
            s.res.setdefault(k, [None, []])[1].append(me)
        for k in writes:
            s.res[k] = [me, []]
        return me

    def barrier(s):
        for e in ENGS:
            pend = []
            for e2 in ENGS:
                n = len(s.ops[e2])
                if e2 != e:
                    for i in range(n - 1, -1, -1):
                        if s.ops[e2][i]['fn'] is not None and not s.ops[e2][i]['dma']:
                            pend.append((e2, i))
                            break
                cnt = 0
                for i in range(n - 1, -1, -1):
                    if s.ops[e2][i]['dma']:
                        pend.append((e2, i))
                        cnt += 1
                        if cnt >= NDS:
                            break
            s.ops[e].append(dict(fn=None, waits=pend, sig=False, dma=False, dsem=None, dval=None, dn=0, ep=s.epoch))
        s.res = {}
        s.epoch += 1

    def emit(s):
        nc = s.nc
        for e in ENGS:
            for o in s.ops[e]:
                for (pe, pi) in o['waits']:
                    po = s.ops[pe][pi]
                    if not po['dma']:
                        if po['fn'] is None:
                            raise RuntimeError("wait on barrier pseudo-op")
                        po['sig'] = True
        sigcount = {}
        used = set()
        for e in ENGS:
            c = {}
            for i, o in enumerate(s.ops[e]):
                if o['sig']:
                    c[o['ep']] = c.get(o['ep'], 0) + 1
                    used.add((e, o['ep']))
                sigcount[(e, i)] = c.get(o['ep'], 0)
        s.maxsig = max([0] + [sigcount[k] for k in sigcount])
        with contextlib.ExitStack() as st:
            esem = {k: st.enter_context(nc.semaphore("es_%s_%d" % k)) for k in sorted(used)}
            dsem = {e: [st.enter_context(nc.semaphore("ds_%s_%d" % (e, i))) for i in range(NDS)] for e in ENGS}
            block = st.enter_context(nc.Block())

            def run(e, eng):
                known = {}
                knownd = {}
                for o in s.ops[e]:
                    need = {}
                    needd = {}
                    if o['dma'] and o['dn'] >= NDS:
                        needd[(e, o['dsem'])] = o['dval'] - 16
                    for (pe, pi) in o['waits']:
                        po = s.ops[pe][pi]
                        if po['dma']:
                            k = (pe, po['dsem'])
                            needd[k] = max(needd.get(k, 0), po['dval'])
                        else:
                            k = (pe, po['ep'])
                            need[k] = max(need.get(k, 0), sigcount[(pe, pi)])
                    for k, v in need.items():
                        if known.get(k, 0) < v:
                            eng.wait_ge(esem[k], v)
                            known[k] = v
                    for k, v in needd.items():
                        if knownd.get(k, 0) < v:
                            eng.wait_ge(dsem[k[0]][k[1]], v)
                            knownd[k] = v
                    if o['fn'] is None:
                        continue
                    inst = o['fn'](eng)
                    if o['dma']:
                        inst.then_inc(dsem[e][o['dsem']], 16)
                    elif o['sig']:
                        inst.then_inc(esem[(e, o['ep'])], 1)

            block.tensor(lambda eng: run(PE, eng))
            block.scalar(lambda eng: run(ACT, eng))
            block.vector(lambda eng: run(DVE, eng))
            block.gpsimd(lambda eng: run(POOL, eng))
            block.sync(lambda eng: run(SP, eng))


class Prog:
    def __init__(p, nc):
        p.nc = nc
        p.S = Sched(nc)
        p.st = contextlib.ExitStack()
        p.din = {}
        p.dout = {}

    def inp(p, name, shape, dt):
        t = p.nc.dram_tensor(name, list(shape), dt, kind="ExternalInput").ap()
        p.din[name] = t
        return t

    def out(p, name, shape, dt):
        t = p.nc.dram_tensor(name, list(shape), dt, kind="ExternalOutput").ap()
        p.dout[name] = t
        return t

    def scratch(p, name, shape, dt):
        return p.nc.dram_tensor(name, list(shape), dt, kind="Internal").ap()

    def sb(p, name, shape, dt, st=None):
        return (st or p.st).enter_context(p.nc.sbuf_tensor(name, list(shape), dt))

    def psum(p, name, shape, dt):
        return p.st.enter_context(p.nc.psum_tensor(name, list(shape), dt))

    def dma(p, q, out, in_, r=(), w=(), **kw):
        return p.S.op(q, lambda e: e.dma_start(out=out, in_=in_, **kw), r, w, dma=True)

    def mm(p, out, lhsT, rhs, start, stop, r=(), w=()):
        return p.S.op(PE, lambda e: e.matmul(out, lhsT, rhs, start=start, stop=stop), r, w)

    def tr(p, out, in_, ident, r=(), w=()):
        return p.S.op(PE, lambda e: e.transpose(out, in_, ident), r, w)

    def act(p, out, in_, func, r=(), w=(), eng=ACT, **kw):
        return p.S.op(eng, lambda e: e.activation(out=out, in_=in_, func=func, **kw), r, w)

    def ts(p, eng, out, in0, s1, s2, op0, op1=None, r=(), w=(), **kw):
        if op1 is None:
            return p.S.op(eng, lambda e: e.tensor_scalar(out, in0, s1, s2, op0, **kw), r, w)
        return p.S.op(eng, lambda e: e.tensor_scalar(out, in0, s1, s2, op0, op1, **kw), r, w)

    def tt(p, eng, out, in0, in1, op, r=(), w=()):
        return p.S.op(eng, lambda e: e.tensor_tensor(out, in0, in1, op), r, w)

    def stt(p, eng, out, in0, scalar, in1, op0, op1, r=(), w=()):
        return p.S.op(eng, lambda e: e.scalar_tensor_tensor(out, in0, scalar, in1, op0, op1), r, w)

    def cp(p, eng, out, in_, r=(), w=()):
        if eng == ACT:
            return p.S.op(eng, lambda e: e.copy(out, in_), r, w)
        return p.S.op(eng, lambda e: e.tensor_copy(out, in_), r, w)

    def generic(p, eng, fn, r=(), w=()):
        return p.S.op(eng, fn, r, w)


def bf(a):
    return np.ascontiguousarray(a).astype(ml_dtypes.bfloat16)


def fm_layout(v, nchunk):
    return np.ascontiguousarray(np.asarray(v, np.float32).reshape(nchunk, 128).T)


def host_consts():
    c = {}
    c['ident_bf'] = bf(np.eye(128, dtype=np.float32))
    c['ident_f'] = np.eye(128, dtype=np.float32)
    c['ones_f'] = np.ones((128, 128), np.float32)
    t = np.arange(128)
    c['u_incl'] = (t[:, None] <= t[None, :]).astype(np.float32)
    c['u_strict'] = (t[:, None] < t[None, :]).astype(np.float32)
    c['l_incl'] = np.ascontiguousarray(c['u_incl'].T)
    c['l_strict'] = np.ascontiguousarray(c['u_strict'].T)
    rep4 = lambda m: np.ascontiguousarray(np.tile(m, (1, 4)))
    c['ms4_f'] = rep4(c['u_strict']); c['mi4_f'] = rep4(c['u_incl'])
    c['ms4_b'] = rep4(c['l_strict']); c['mi4_b'] = rep4(c['l_incl'])
    c['i4'] = bf(rep4(np.eye(128, dtype=np.float32)))
    lv = np.zeros((2, 7, 128, 512), np.float32)
    for li in range(7):
        sz = 1 << li
        j = t[:, None]; i = t[None, :]
        m = ((j // (2 * sz)) == (i // (2 * sz))) & ((j % (2 * sz)) < sz) & ((i % (2 * sz)) >= sz)
        lv[0, li] = rep4(m.astype(np.float32))
        lv[1, li] = rep4(m.T.astype(np.float32))
    c['lvl'] = bf(lv.transpose(2, 0, 1, 3))
    c['ones_bf'] = bf(np.ones((128, 128), np.float32))
    c['ustrict_bf'] = bf(c['u_strict'])
    c['iota32'] = np.ascontiguousarray(np.tile(np.arange(32, dtype=np.float32)[None, :], (128, 1)))
    c['ecol'] = np.ascontiguousarray(np.arange(128, dtype=np.float32)[:, None])
    c['blk128'] = np.zeros((128, 1), np.float32)
    tb = np.concatenate([[0], np.cumsum(MOE_CAPS)[:-1]]).astype(np.float32) * 128.0
    c['basetab'] = np.ascontiguousarray(np.tile(tb[None, :], (128, 1)))
    c['captab'] = np.ascontiguousarray(np.tile((np.array(MOE_CAPS, np.float32) * 128.0)[None, :], (128, 1)))
    c['rowoff'] = np.ascontiguousarray((np.arange(8)[None, :] * 128 + np.arange(128)[:, None]).astype(np.float32))
    c['tokid'] = np.ascontiguousarray((np.arange(16)[None, :] * 128 + np.arange(128)[:, None]).astype(np.int32))
    return c


class Builder(Prog):
    def __init__(p, nc, debug=None):
        super().__init__(nc)
        p.debug = debug
        p.const_np = host_consts()
        p.psb = [p.psum("ps%d" % i, [128, 512], F32) for i in range(8)]

    def load_const(p, name, dt):
        a = p.const_np[name]
        d = p.inp("c_" + name, a.shape, dt)
        t = p.sb("k_" + name, a.shape, dt)
        p.dma(SP, t[:], d, w=[('k', name)])
        return t

    def phase0(p):
        nc = p.nc
        p.ident_bf = p.load_const('ident_bf', BF16)
        p.ident_f = p.load_const('ident_f', F32)
        p.ones_f = p.load_const('ones_f', F32)
        cT = p.inp("cT", [128, 8, 2], F32)
        bmod = p.inp("bmod", [128, 48], F32)
        n1g = p.inp("n1g", [128, 8], F32)
        n2g = p.inp("n2g", [128, 8], F32)
        wmod = p.inp("w_mod", [D, 6 * D], F32)
        p.eps_t = p.sb("eps_t", [128, 1], F32)
        p.S.op(DVE, lambda e: e.memset(p.eps_t[:], EPS), [], ['eps_t'])
        p.modsb = p.sb("modsb", [128, 48, 2], F32)
        p.vecs = p.sb("vecs", [128, 8, 8], F32)
        st0 = contextlib.ExitStack()
        scT = p.sb("scT", [128, 8, 2], F32, st0)
        bm = p.sb("bm", [128, 48], F32, st0)
        g1t = p.sb("n1g_sb", [128, 8], F32, st0)
        g2t = p.sb("n2g_sb", [128, 8], F32, st0)
        wb = [p.sb("wmodb%d" % i, [128, 8, 512], F32, st0) for i in range(2)]
        p.dma(SP, scT[:], cT, w=['scT'])
        p.dma(SP, bm[:], bmod, w=['bm'])
        p.dma(SP, g1t[:], n1g, w=['n1g'])
        p.dma(SP, g2t[:], n2g, w=['n2g'])
        p.act(scT[:], scT[:], AF.Silu, r=['scT'], w=['scT'])
        wv = wmod.rearrange("(kc q) n -> q kc n", q=128)
        psM = p.psb[0]
        for blk in range(12):
            b = wb[blk % 2]
            p.dma(SP, b[:], wv[:, :, blk * 512:(blk + 1) * 512], w=[('wmodb', blk % 2)])
            for fc in range(4):
                j = blk * 4 + fc
                for kc in range(8):
                    p.mm(psM[:, 2 * j:2 * j + 2], b[:, kc, fc * 128:(fc + 1) * 128], scT[:, kc, :],
                         kc == 0, kc == 7, r=[('wmodb', blk % 2), 'scT'], w=[('ps', 0)])
        pv = psM[:, 0:96].rearrange("q (j m) -> q j m", m=2)
        for m in range(2):
            p.tt(DVE, p.modsb[:, :, m], pv[:, :, m], bm[:], ALU.add, r=[('ps', 0), 'bm'], w=['modsb'])
        p.stt(DVE, p.vecs[:, 0, :], p.modsb[:, 8:16, 0], 1.0, g1t[:], ALU.add, ALU.mult, r=['modsb', 'n1g'], w=['vecs'])
        p.stt(DVE, p.vecs[:, 1, :], p.modsb[:, 8:16, 1], 1.0, g1t[:], ALU.add, ALU.mult, r=['modsb', 'n1g'], w=['vecs'])
        p.stt(DVE, p.vecs[:, 2, :], p.modsb[:, 32:40, 0], 1.0, g2t[:], ALU.add, ALU.mult, r=['modsb', 'n2g'], w=['vecs'])
        p.S.barrier()
        st0.close()
        if p.debug == '0':
            o = p.out("dbg_mod", [128, 48, 2], F32)
            p.dma(SP, o, p.modsb[:], r=['modsb'])
            o2 = p.out("dbg_vecs", [128, 8, 8], F32)
            p.dma(SP, o2, p.vecs[:], r=['vecs'])

    def gs1(p, c): return p.vecs[:, 0, c:c + 1]
    def sh1(p, c): return p.modsb[:, c, 0:1]
    def cgs1(p, c): return p.vecs[:, 1, c:c + 1]
    def csh1(p, c): return p.modsb[:, c, 1:2]

    def norm_T(p, xsrc, gs, sh, uT_dst, bufs, tag):
        xt, xk = bufs['x']
        p.dma(SP, xt[:], xsrc, w=[xk])
        junk, jk = bufs['junk']
        ss, sk = bufs['ss']
        p.act(junk[:], xt[:], AF.Square, r=[xk], w=[jk, sk], accum_out=ss[:, 0:1])
        p.act(ss[:, 2:3], ss[:, 0:1], AF.Ln, r=[sk], w=[sk], scale=1.0 / D, bias=p.eps_t[:, 0:1])
        p.act(ss[:, 3:4], ss[:, 2:3], AF.Exp, r=[sk], w=[sk], scale=-0.5)
        xn, nk = bufs['xn']
        p.ts(DVE, xn[:], xt[:], ss[:, 3:4], None, ALU.mult, r=[xk, sk], w=[nk])
        pb, pk = bufs['ps']
        pbv = pb[:].bitcast(BF16)
        for c in range(8):
            p.tr(pbv[:, c * 128:(c + 1) * 128], xn[:, c * 128:(c + 1) * 128], p.ident_bf[:], r=[nk, ('k', 'ident_bf')], w=[pk])
        if tag == 'split':
            return
        p.norm_T_b(gs, sh, uT_dst, bufs)

    def norm_T_b(p, gs, sh, uT_dst, bufs):
        pb, pk = bufs['ps']
        pbv = pb[:].bitcast(BF16)
        for c in range(8):
            dst, dk = uT_dst(c)
            if c % 2 == 0:
                p.act(dst, pbv[:, c * 128:(c + 1) * 128], AF.Identity, r=[pk, 'modsb', 'vecs'], w=[dk],
                      scale=gs(c), bias=sh(c))
            else:
                p.ts(DVE, dst, pbv[:, c * 128:(c + 1) * 128], gs(c), sh(c), ALU.mult, ALU.add, r=[pk, 'modsb', 'vecs'], w=[dk])

    def phaseA(p):
        x = p.inp("x_seq", [L, D], F32)
        ctx = p.inp("ctx_seq", [CTXL, D], F32)
        w_in = p.inp("w_in", [D, IN_COLS], F32)
        cw = p.inp("conv_diag", [128, 16, 9, 128], F32)
        p.XF = p.scratch("XF", [L, 512], BF16)
        p.QKVs = p.scratch("QKVs", [L + CTXL, QKV], BF16)
        p.ba = p.sb("ba", [128, NT + 2, 32], F32)
        stA = contextlib.ExitStack()
        wA = p.sb("wA", [128, 8, 2592], BF16, stA)
        wv = w_in.rearrange("(kc q) n -> q kc n", q=128)
        for kc in range(8):
            p.dma(POOL, wA[:, kc, 0:2080], wv[:, kc, 0:2080], w=[('wA', kc)])
            p.dma(POOL, wA[:, kc, 2080:2592], wv[:, kc, F_OFF:F_OFF + 512], w=[('wA', kc)])
        cwts = [p.sb("cwt%d" % i, [128, 2, 9, 128], BF16, stA) for i in range(2)]
        NTT = NT + 2
        uT = p.sb("uT", [128, 8, NTT * 128], BF16, stA)
        stA1 = contextlib.ExitStack()
        NB1 = 4
        bufs = [{
            'x': (p.sb("xt%d" % i, [128, D], F32, stA1), ('xt', i)),
            'junk': (p.sb("junk%d" % i, [128, D], BF16, stA1), ('junk', i)),
            'ss': (p.sb("ss%d" % i, [128, 4], F32, stA1), ('ss', i)),
            'xn': (p.sb("xn%d" % i, [128, D], BF16, stA1), ('xn', i)),
            'ps': (p.psb[[0, 1, 6, 7][i]], ('ps', [0, 1, 6, 7][i])),
        } for i in range(NB1)]
        xfb = [p.sb("xfb%d" % i, [128, 512], BF16, stA1) for i in range(2)]
        def a1_args(t):
            if t < NT:
                return x[t * 128:(t + 1) * 128, :], p.gs1, p.sh1
            return ctx[(t - NT) * 128:(t - NT + 1) * 128, :], p.cgs1, p.csh1

        def a1_front(t):
            src, gs, sh = a1_args(t)
            p.norm_T(src, gs, sh, None, bufs[t % NB1], 'split')

        DEPTH = 2
        for t0 in range(DEPTH):
            a1_front(t0)
        for t in range(NTT):
            b = bufs[t % NB1]
            if t + DEPTH < NTT:
                a1_front(t + DEPTH)
            src, gs, sh = a1_args(t)
            p.norm_T_b(gs, sh, lambda c, t=t: (uT[:, c, t * 128:(t + 1) * 128], ('uT', t)), b)
            ps = p.psb[2 + t % 2]
            for kc in range(8):
                p.mm(ps[:, 0:32], uT[:, kc, t * 128:(t + 1) * 128], wA[:, kc, 2048:2080], kc == 0, kc == 7,
                     r=[('wA', kc), ('uT', t)], w=[('ps', 2 + t % 2)])
            p.cp(DVE, p.ba[:, t, :], ps[:, 0:32], r=[('ps', 2 + t % 2)], w=[('ba', t)])
            if t < NT:
                ps = p.psb[4 + t % 2]
                for kc in range(8):
                    p.mm(ps[:, :], uT[:, kc, t * 128:(t + 1) * 128], wA[:, kc, 2080:2592], kc == 0, kc == 7,
                         r=[('wA', kc), ('uT', t)], w=[('ps', 4 + t % 2)])
                xb = xfb[t % 2]
                p.cp(DVE, xb[:], ps[:, :], r=[('ps', 4 + t % 2)], w=[('xfb', t % 2)])
                p.dma(POOL, p.XF[t * 128:(t + 1) * 128, :], xb[:], r=[('xfb', t % 2)], w=[('XF', t)])
        p.S.barrier()
        stA1.close()
        LD = 72
        C0 = LD + L + 64
        CBN = C0 + 258
        cb = [p.sb("cb%d" % i, [128, 3, CBN], BF16, stA) for i in range(2)]
        for i in range(2):
            p.S.op(DVE, (lambda e, t_=cb[i]: e.memset(t_[:], 0.0)), [], [('cb', i)])
        qt = [p.sb("qt%d" % i, [128, 512], BF16, stA) for i in range(2)]
        for ch in range(16):
            cbuf = cb[ch % 2]
            ck = ('cb', ch % 2)
            if ch % 2 == 0:
                cwt = cwts[(ch // 2) % 2]
                cwk = ('cwt', (ch // 2) % 2)
                p.dma(POOL, cwt[:], cw[:, ch:ch + 2, :, :], w=[cwk])
            for st_ in range(9):
                ntok = 512 if st_ < 8 else 256
                t0 = st_ * 512
                ps = p.psb[st_ % 4]
                pk = ('ps', st_ % 4)
                for kc in range(8):
                    p.mm(ps[:, 0:ntok], wA[:, kc, ch * 128:(ch + 1) * 128], uT[:, kc, t0:t0 + ntok], kc == 0, kc == 7,
                         r=[('wA', kc)] + [('uT', t0 // 128 + q) for q in range(ntok // 128)], w=[pk])
                if st_ < 8:
                    o0 = LD + t0
                    p.cp(ACT, cbuf[:, 0, o0:o0 + 512], ps[:, 0:512], r=[pk], w=[ck])
                    sv = ps[:, 0:512].rearrange("q (r c) -> q r c", c=64)
                    p.cp(DVE, cbuf[:, 1, o0:o0 + 512].rearrange("q (r c) -> q r c", c=64)[:, :, 0:63], sv[:, :, 0:63], r=[pk], w=[ck])
                    p.cp(DVE, cbuf[:, 2, o0:o0 + 512].rearrange("q (r c) -> q r c", c=64)[:, :, 1:64], sv[:, :, 1:64], r=[pk], w=[ck])
                else:
                    p.cp(ACT, cbuf[:, 0, C0 + 1:C0 + 257], ps[:, 0:256], r=[pk], w=[ck])
            for t in range(NTT):
                if t % 4 == 0:
                    ps = p.psb[4 + (t // 4) % 4]
                    pk = ('ps', 4 + (t // 4) % 4)
                if t < NT:
                    taps = [(dy, dx) for dy in (-1, 0, 1) for dx in (-1, 0, 1)]
                else:
                    taps = [(0, dx) for dx in (-1, 0, 1)]
                for ti, (dy, dx) in enumerate(taps):
                    if t < NT:
                        base = LD + 128 * t + 64 * dy + dx
                        lhsT = cbuf[:, {-1: 1, 0: 0, 1: 2}[dx], base:base + 128]
                    else:
                        base = C0 + 1 + (t - NT) * 128 + dx
                        lhsT = cbuf[:, 0, base:base + 128]
                    tap = (dy + 1) * 3 + (dx + 1)
                    p.mm(ps[:, (t % 4) * 128:(t % 4 + 1) * 128], lhsT, cwt[:, ch % 2, tap, :], ti == 0, ti == len(taps) - 1,
                         r=[ck, cwk], w=[pk])
                if t % 4 == 3 or t == NTT - 1:
                    nt_ = t % 4 + 1
                    tb = t - t % 4
                    qi_ = (tb // 4) % 2
                    q_ = qt[qi_]
                    p.act(q_[:, 0:nt_ * 128], ps[:, 0:nt_ * 128], AF.Silu, r=[pk], w=[('qt', qi_)])
                    row = tb * 128 if tb < NT else L
                    dstv = p.QKVs[row:row + nt_ * 128, ch * 128:(ch + 1) * 128].rearrange("(a q) c -> q a c", q=128)
                    p.dma(SP, dstv, q_[:, 0:nt_ * 128].rearrange("q (a c) -> q a c", c=128), r=[('qt', qi_)], w=[('QKVs', tb, ch)])
        p.S.barrier()
        stA.close()
        if p.debug == 'A':
            o = p.out("dbg_qkv", [L + CTXL, QKV], BF16)
            p.dma(SP, o, p.QKVs, r=[])
            o = p.out("dbg_ba", [128, NT + 2, 32], F32)
            p.dma(SP, o, p.ba[:], r=[])
            o = p.out("dbg_xf", [L, 512], BF16)
            p.dma(SP, o, p.XF, r=[])


    def phaseG(p):
        DKS = 128 ** -0.5
        alog = p.inp("alog_t", [NT + 2, 16], F32)
        dtb = p.inp("dtb_t", [NT + 2, 16], F32)
        NTT = NT + 2
        p.Od = [p.scratch("O_f", [16 * 128, 1024], F32), p.scratch("O_b", [16 * 128, 1024], F32)]
        stG = contextlib.ExitStack()
        K = {}
        for nm, dt_ in [('u_incl', F32), ('l_incl', F32), ('u_strict', F32), ('l_strict', F32),
                        ('ms4_f', F32), ('mi4_f', F32), ('ms4_b', F32), ('mi4_b', F32), ('i4', BF16), ('lvl', BF16)]:
            a = p.const_np[nm]
            d = p.inp("c_" + nm, a.shape, dt_)
            K[nm] = p.sb("k_" + nm, a.shape, dt_, stG)
            p.dma(SP, K[nm][:], d, w=[('k', nm)])
        kr = lambda *n: [('k', x) for x in n]
        p.S.barrier()
        lgs = p.sb("lgs", [128, NTT, 16], F32, stG)
        bts = p.sb("bts", [128, NTT, 16], F32, stG)
        nbt = p.sb("nbts", [128, NTT, 16], F32, stG)
        prm = p.sb("prm", [128, 2, NTT, 16], F32, stG)
        p.dma(SP, prm[:, 0], alog.partition_broadcast(128), w=['prm'])
        p.dma(SP, prm[:, 1], dtb.partition_broadcast(128), w=['prm'])
        p.act(prm[:, 0], prm[:, 0], AF.Exp, r=['prm'], w=['prm'])
        p.tt(DVE, lgs[:], p.ba[:, :, 16:32], prm[:, 1], ALU.add, r=['prm'], w=['lgs'])
        p.act(lgs[:], lgs[:], AF.Exp, r=['lgs'], w=['lgs'])
        p.ts(DVE, lgs[:], lgs[:], 1.0, None, ALU.add, r=['lgs'], w=['lgs'])
        p.act(lgs[:], lgs[:], AF.Ln, r=['lgs'], w=['lgs'])
        p.stt(DVE, lgs[:], lgs[:], -1.0, prm[:, 0], ALU.mult, ALU.mult, r=['lgs', 'prm'], w=['lgs'])
        p.act(bts[:], p.ba[:, :, 0:16], AF.Exp, r=[], w=['bts'], scale=-1.0)
        p.ts(DVE, bts[:], bts[:], 1.0, None, ALU.add, r=['bts'], w=['bts'])
        p.S.op(DVE, lambda e: e.reciprocal(bts[:], bts[:]), ['bts'], ['bts'])
        p.ts(DVE, nbt[:], bts[:], -1.0, None, ALU.mult, r=['bts'], w=['nbt'])
        Sf = p.sb("S_f32", [128, 16, 128], F32, stG)
        Sb = [p.sb("S_bf%d" % i, [128, 16, 128], BF16, stG) for i in range(2)]
        p.S.op(DVE, lambda e: e.memset(Sf[:], 0.0), [], [('Sf', h) for h in range(16)])
        p.S.op(DVE, lambda e: e.memset(Sb[0][:], 0.0), [], [('Sb', 0, h) for h in range(16)])
        sbi = [0] * 16
        qkvb = [p.sb("qkvb%d" % i, [128, QKV], BF16, stG) for i in range(2)]
        sqs = [p.sb("sq%d" % i, [128, 1024], F32, stG) for i in range(2)]
        nrms = [p.sb("nrm%d" % i, [128, 4, 8], F32, stG) for i in range(2)]
        qkn = [p.sb("qkn%d" % i, [128, 8, 128], BF16, stG) for i in range(2)]
        kT = [p.sb("kT%d" % i, [128, 4, 128], BF16, stG) for i in range(2)]
        qT = [p.sb("qT%d" % i, [128, 4, 128], BF16, stG) for i in range(2)]
        gsc = [p.sb("gsc%d" % i, [128, 6, 16], F32, stG) for i in range(2)]
        CT = []
        for ci in range(2):
            c_ = dict(i=ci, ba=3 * ci)
            for nm_, shp, dt_ in [('LW', [128, 4, 128], F32), ('Eb', [128, 512], F32), ('Ems', [128, 512], F32), ('Emi', [128, 512], F32),
                                  ('Nb', [128, 512], BF16), ('NTb', [128, 512], BF16), ('NTl', [128, 6, 512], BF16),
                                  ('X0', [128, 512], BF16), ('X1', [128, 512], BF16), ('XT0', [128, 512], BF16), ('XT1', [128, 512], BF16),
                                  ('Yb', [128, 512], BF16), ('tmpb', [128, 512], BF16), ('tmpf', [128, 512], F32), ('qkp', [128, 512], BF16),
                                  ('kdec', [128, 512], BF16), ('rb', [128, 512], BF16), ('ub', [128, 512], BF16), ('o1', [128, 512], F32)]:
                c_[nm_] = p.sb("%s_%d" % (nm_, ci), shp, dt_, stG)
            CT.append(c_)
        ob = [p.sb("ob%d" % i, [128, 1024], F32, stG) for i in range(2)]
        PS = lambda i: p.psb[i]
        PK = lambda i: ('ps', i)
        sched = [(32, 0, False), (33, 0, False), (33, 1, False), (32, 1, False)]
        fw = [(t, 0, t >= OWN0) for t in range(NT)]
        bw = [(t, 1, True) for t in range(NT - 1, OWN0 - 1, -1)]
        while fw or bw:
            if fw:
                sched.append(fw.pop(0))
            if bw and (len(fw) < 2 * len(bw) + 1):
                sched.append(bw.pop(0))
        if p.debug == 'G0':
            sched = sched[:4]
        loaded = {}
        step = 0
        nload = 0
        real_op = p.S.op
        recs = []

        def rec_into(lst):
            p.S.op = lambda eng, fn, reads=(), writes=(), dma=False: lst.append((eng, fn, list(reads), list(writes), dma))

        for (t, d, need_out) in sched:
            prep_l, stage_ll, tail_l = [], [], []
            recs.append((prep_l, stage_ll, tail_l))
            rec_into(prep_l)
            bi = nload % 2
            nload += 1
            qb = qkvb[bi]
            sq = sqs[bi]
            nrm = nrms[bi]
            sqk = ('sq', bi)
            nrk = ('nrm', bi)
            row = t * 128 if t < NT else L + (t - NT) * 128
            p.dma(SP, qb[:], p.QKVs[row:row + 128, :], w=[('qkvb', bi)])
            p.tt(DVE, sq[:], qb[:, 0:1024], qb[:, 0:1024], ALU.mult, r=[('qkvb', bi)], w=[sqk])
            p.S.op(DVE, lambda e, nrm=nrm, sq=sq: e.tensor_reduce(nrm[:, 0, :], sq[:].rearrange("q (h c) -> q h c", c=128), AX.X, ALU.add), [sqk], [nrk])
            p.ts(DVE, nrm[:, 1, :], nrm[:, 0, :], EPS, None, ALU.add, r=[nrk], w=[nrk])
            p.act(nrm[:, 2, :], nrm[:, 1, :], AF.Ln, r=[nrk], w=[nrk])
            p.act(nrm[:, 3, :], nrm[:, 2, :], AF.Exp, r=[nrk], w=[nrk], scale=-0.5)
            p.ts(DVE, nrm[:, 3, 0:4], nrm[:, 3, 0:4], DKS, None, ALU.mult, r=[nrk], w=[nrk])
            qn = qkn[bi]
            p.tt(DVE, qn[:], qb[:, 0:1024].rearrange("q (h c) -> q h c", c=128), nrm[:, 3, :].unsqueeze(2).to_broadcast([128, 8, 128]), ALU.mult,
                 r=[('qkvb', bi), nrk], w=[('qkn', bi)])
            kTb, qTb = kT[bi], qT[bi]
            pv = PS(6)[:].bitcast(BF16)
            for h in range(4):
                p.tr(pv[:, h * 128:(h + 1) * 128], qn[:, 4 + h, :], p.ident_bf[:], r=[('qkn', bi)], w=[PK(6)])
            p.cp(ACT, kTb[:].rearrange("q h c -> q (h c)"), pv[:, 0:512], r=[PK(6)], w=[('kT', bi)])
            if need_out:
                pv5 = PS(6)[:].bitcast(BF16)[:, 512:1024]
                for h in range(4):
                    p.tr(pv5[:, h * 128:(h + 1) * 128], qn[:, h, :], p.ident_bf[:], r=[('qkn', bi)], w=[PK(6)])
                p.cp(ACT, qTb[:].rearrange("q h c -> q (h c)"), pv5[:, 0:512], r=[PK(6)], w=[('qT', bi)])
            g = gsc[bi]
            gk = ('gsc', bi)
            lg = lgs[:, t, :]
            ps6 = PS(7)
            p.mm(ps6[:, 0:8], K['u_incl'][:], lgs[:, t, 0:8], True, True, r=kr('u_incl') + ['lgs'], w=[PK(7)])
            p.mm(ps6[:, 8:16], K['l_incl'][:], lgs[:, t, 8:16], True, True, r=kr('l_incl') + ['lgs'], w=[PK(7)])
            p.mm(ps6[:, 16:32], p.ones_f[:], lgs[:, t, :], True, True, r=['lgs'], w=[PK(7)])
            p.cp(DVE, g[:, 0:2, :].rearrange("q a c -> q (a c)"), ps6[:, 0:32], r=[PK(7)], w=[gk])
            p.act(g[:, 2, :], g[:, 0, :], AF.Exp, r=[gk], w=[gk])
            p.ts(DVE, g[:, 3, :], g[:, 2, :], -1.0, None, ALU.mult, r=[gk], w=[gk])
            p.tt(DVE, g[:, 4, :], g[:, 1, :], g[:, 0, :], ALU.subtract, r=[gk], w=[gk])
            p.act(g[:, 4, :], g[:, 4, :], AF.Exp, r=[gk], w=[gk])
            p.act(g[:, 5, :], g[:, 1, :], AF.Exp, r=[gk], w=[gk])
            ms_lhs = K['l_strict'] if d == 0 else K['u_strict']
            mi_rhs = K['u_incl'] if d == 0 else K['l_incl']
            MS4 = K['ms4_f'] if d == 0 else K['ms4_b']
            MI4 = K['mi4_f'] if d == 0 else K['mi4_b']
            H4 = lambda ap: ap.rearrange("q (h c) -> q h c", c=128)
            bci = lambda ap: ap.unsqueeze(2).to_broadcast([128, 4, 128])
            bcm = lambda ap: ap.unsqueeze(1).to_broadcast([128, 4, 128])
            obuf = ob[step % 2]

            def key(c_, n):
                return (n, c_['i'])

            def st_D(c_, grp):
                hd0 = d * 8 + grp * 4
                p.tt(DVE, c_['LW'][:], bcm(ms_lhs[:]), bci(lgs[:, t, hd0:hd0 + 4]), ALU.mult, r=['lgs'], w=[key(c_, 'LW')])
                for j4 in range(4):
                    p.mm(PS(c_['ba'])[:, j4 * 128:(j4 + 1) * 128], c_['LW'][:, j4, :], mi_rhs[:], True, True, r=[key(c_, 'LW')], w=[PK(c_['ba'])])
                for j4 in range(4):
                    hq = (grp * 4 + j4) // 2
                    p.mm(PS(c_['ba'] + 1)[:, j4 * 128:(j4 + 1) * 128], kTb[:, hq, :], kTb[:, hq, :], True, True, r=[('kT', bi)], w=[PK(c_['ba'] + 1)])
                if need_out:
                    for j4 in range(4):
                        hq = (grp * 4 + j4) // 2
                        p.mm(PS(c_['ba'] + 2)[:, j4 * 128:(j4 + 1) * 128], kTb[:, hq, :], qTb[:, hq, :], True, True,
                             r=[('kT', bi), ('qT', bi)], w=[PK(c_['ba'] + 2)])

            def st_E(c_, grp):
                hd0 = d * 8 + grp * 4
                p.act(c_['Eb'][:], PS(c_['ba'])[:, :], AF.Exp, r=[PK(c_['ba'])], w=[key(c_, 'Eb')])
                p.tt(DVE, c_['Ems'][:], c_['Eb'][:], MS4[:], ALU.mult, r=[key(c_, 'Eb')], w=[key(c_, 'Ems')])
                p.tt(DVE, H4(c_['Ems'][:]), H4(c_['Ems'][:]), bci(nbt[:, t, hd0:hd0 + 4]), ALU.mult, r=[key(c_, 'Ems'), 'nbt'], w=[key(c_, 'Ems')])
                p.tt(DVE, c_['Nb'][:], PS(c_['ba'] + 1)[:, :], c_['Ems'][:], ALU.mult, r=[PK(c_['ba'] + 1), key(c_, 'Ems')], w=[key(c_, 'Nb')])
                if need_out:
                    p.tt(DVE, c_['Emi'][:], c_['Eb'][:], MI4[:], ALU.mult, r=[key(c_, 'Eb')], w=[key(c_, 'Emi')])
                    p.tt(DVE, c_['qkp'][:], PS(c_['ba'] + 2)[:, :], c_['Emi'][:], ALU.mult, r=[PK(c_['ba'] + 2), key(c_, 'Emi')], w=[key(c_, 'qkp')])

            def st_NT(c_, grp):
                pv3 = PS(c_['ba'])[:].bitcast(BF16)
                for j4 in range(4):
                    p.tr(pv3[:, j4 * 128:(j4 + 1) * 128], c_['Nb'][:, j4 * 128:(j4 + 1) * 128], p.ident_bf[:], r=[key(c_, 'Nb')], w=[PK(c_['ba'])])
                p.cp(ACT, c_['NTb'][:], pv3[:, 0:512], r=[PK(c_['ba'])], w=[key(c_, 'NTb')])
                p.tt(DVE, c_['NTl'][:], K['lvl'][:, 1 - d, 1:7, :], c_['NTb'][:].unsqueeze(1).to_broadcast([128, 6, 512]), ALU.mult,
                     r=[key(c_, 'NTb')], w=[key(c_, 'NTl')])
                p.tt(DVE, c_['tmpb'][:], c_['Nb'][:], K['lvl'][:, d, 0, :], ALU.mult, r=[key(c_, 'Nb')], w=[key(c_, 'tmpb')])
                p.tt(DVE, c_['X0'][:], c_['tmpb'][:], K['i4'][:], ALU.add, r=[key(c_, 'tmpb')], w=[key(c_, 'X0')])
                p.tt(DVE, c_['tmpb'][:], c_['NTb'][:], K['lvl'][:, 1 - d, 0, :], ALU.mult, r=[key(c_, 'NTb'), key(c_, 'tmpb')], w=[key(c_, 'tmpb')])
                p.tt(DVE, c_['XT0'][:], c_['tmpb'][:], K['i4'][:], ALU.add, r=[key(c_, 'tmpb')], w=[key(c_, 'XT0')])

            def st_L1(c_, grp, li):
                xs = (li - 1) % 2
                X, XT = c_['X%d' % xs], c_['XT%d' % xs]
                for j4 in range(4):
                    sl = slice(j4 * 128, (j4 + 1) * 128)
                    p.mm(PS(c_['ba'])[:, sl], c_['NTl'][:, li - 1, sl], X[:, sl], True, True, r=[key(c_, 'NTl'), key(c_, 'X%d' % xs)], w=[PK(c_['ba'])])
                p.cp(ACT, c_['Yb'][:], PS(c_['ba'])[:, :], r=[PK(c_['ba'])], w=[key(c_, 'Yb')])

            def st_L2(c_, grp, li):
                xs = (li - 1) % 2
                X, XT = c_['X%d' % xs], c_['XT%d' % xs]
                Xn, XTn = c_['X%d' % (1 - xs)], c_['XT%d' % (1 - xs)]
                for j4 in range(4):
                    sl = slice(j4 * 128, (j4 + 1) * 128)
                    p.mm(PS(c_['ba'] + 1)[:, sl], XT[:, sl], c_['Yb'][:, sl], True, False, r=[key(c_, 'XT%d' % xs), key(c_, 'Yb')], w=[PK(c_['ba'] + 1)])
                    p.mm(PS(c_['ba'] + 1)[:, sl], p.ident_bf[:], X[:, sl], False, True, r=[key(c_, 'X%d' % xs)], w=[PK(c_['ba'] + 1)])
                if li < 6:
                    for j4 in range(4):
                        sl = slice(j4 * 128, (j4 + 1) * 128)
                        p.mm(PS(c_['ba'] + 2)[:, sl], c_['Yb'][:, sl], XT[:, sl], True, False, r=[key(c_, 'XT%d' % xs), key(c_, 'Yb')], w=[PK(c_['ba'] + 2)])
                        p.mm(PS(c_['ba'] + 2)[:, sl], p.ident_bf[:], XT[:, sl], False, True, r=[key(c_, 'XT%d' % xs)], w=[PK(c_['ba'] + 2)])
                p.cp(DVE if li % 2 == 0 else ACT, Xn[:], PS(c_['ba'] + 1)[:, :], r=[PK(c_['ba'] + 1)], w=[key(c_, 'X%d' % (1 - xs))])
                if li < 6:
                    p.cp(ACT if li % 2 == 0 else DVE, XTn[:], PS(c_['ba'] + 2)[:, :], r=[PK(c_['ba'] + 2)], w=[key(c_, 'XT%d' % (1 - xs))])

            def st_S1(c_, grp):
                hd0 = d * 8 + grp * 4
                hq0 = 4 + grp * 2
                p.tt(DVE, c_['kdec'][:].rearrange("q (a b c) -> q a b c", b=2, c=128),
                     qn[:, hq0:hq0 + 2, :].unsqueeze(2).to_broadcast([128, 2, 2, 128]),
                     g[:, 4, hd0:hd0 + 4].rearrange("q (a b) -> q a b", b=2).unsqueeze(3).to_broadcast([128, 2, 2, 128]), ALU.mult,
                     r=[('qkn', bi), gk], w=[key(c_, 'kdec')])
                for j4 in range(4):
                    hd = hd0 + j4
                    hv = grp * 4 + j4
                    sl = slice(j4 * 128, (j4 + 1) * 128)
                    So = Sb[sbi[hd]]
                    p.mm(PS(c_['ba'])[:, sl], kTb[:, hv // 2, :], So[:, hd, :], True, True, r=[('kT', bi), ('Sb', sbi[hd], hd)], w=[PK(c_['ba'])])
                p.tt(DVE, H4(c_['tmpf'][:]), H4(PS(c_['ba'])[:, :]), bci(g[:, 3, hd0:hd0 + 4]), ALU.mult, r=[PK(c_['ba']), gk], w=[key(c_, 'tmpf')])
                v0 = 1024 + grp * 512
                p.tt(DVE, c_['rb'][:], c_['tmpf'][:], qb[:, v0:v0 + 512], ALU.add, r=[key(c_, 'tmpf'), ('qkvb', bi)], w=[key(c_, 'rb')])

            def st_S2(c_, grp):
                hd0 = d * 8 + grp * 4
                X = c_['X0']
                for j4 in range(4):
                    sl = slice(j4 * 128, (j4 + 1) * 128)
                    p.mm(PS(c_['ba'] + 1)[:, sl], X[:, sl], c_['rb'][:, sl], True, True, r=[key(c_, 'X0'), key(c_, 'rb')], w=[PK(c_['ba'] + 1)])
                for j4 in range(4):
                    hd = hd0 + j4
                    sl = slice(j4 * 128, (j4 + 1) * 128)
                    p.act(c_['ub'][:, sl], PS(c_['ba'] + 1)[:, sl], AF.Copy, r=[PK(c_['ba'] + 1), 'bts'], w=[key(c_, 'ub')], scale=bts[:, t, hd:hd + 1])

            def st_S3(c_, grp):
                hd0 = d * 8 + grp * 4
                for j4 in range(4):
                    sl = slice(j4 * 128, (j4 + 1) * 128)
                    p.mm(PS(c_['ba'])[:, sl], c_['kdec'][:, sl], c_['ub'][:, sl], True, True, r=[key(c_, 'kdec'), key(c_, 'ub')], w=[PK(c_['ba'])])
                if need_out:
                    for j4 in range(4):
                        hd = hd0 + j4
                        hv = grp * 4 + j4
                        sl = slice(j4 * 128, (j4 + 1) * 128)
                        So = Sb[sbi[hd]]
                        p.mm(PS(c_['ba'] + 1)[:, sl], qTb[:, hv // 2, :], So[:, hd, :], True, True, r=[('qT', bi), ('Sb', sbi[hd], hd)], w=[PK(c_['ba'] + 1)])
                    for j4 in range(4):
                        hd = hd0 + j4
                        sl = slice(j4 * 128, (j4 + 1) * 128)
                        p.act(c_['o1'][:, sl], PS(c_['ba'] + 1)[:, sl], AF.Copy, r=[PK(c_['ba'] + 1), gk], w=[key(c_, 'o1')], scale=g[:, 2, hd:hd + 1])
                    for j4 in range(4):
                        sl = slice(j4 * 128, (j4 + 1) * 128)
                        p.mm(PS(c_['ba'] + 2)[:, sl], c_['qkp'][:, sl], c_['ub'][:, sl], True, True, r=[key(c_, 'qkp'), key(c_, 'ub')], w=[PK(c_['ba'] + 2)])
                    p.tt(DVE, obuf[:, grp * 512:(grp + 1) * 512], PS(c_['ba'] + 2)[:, :], c_['o1'][:], ALU.add, r=[PK(c_['ba'] + 2), key(c_, 'o1')],
                         w=[('ob', step % 2, grp)])
                hk = [('Sf', hd0 + j) for j in range(4)]
                p.tt(DVE, Sf[:, hd0:hd0 + 4, :], Sf[:, hd0:hd0 + 4, :], bci(g[:, 5, hd0:hd0 + 4]), ALU.mult, r=hk + [gk], w=hk)
                p.tt(DVE, Sf[:, hd0:hd0 + 4, :], Sf[:, hd0:hd0 + 4, :], H4(PS(c_['ba'])[:, :]), ALU.add, r=hk + [PK(c_['ba'])], w=hk)
                nb_ = 1 - sbi[hd0]
                p.cp(ACT, Sb[nb_][:, hd0:hd0 + 4, :], Sf[:, hd0:hd0 + 4, :], r=hk, w=[('Sb', nb_, hd0 + j) for j in range(4)])
                for j in range(4):
                    sbi[hd0 + j] = nb_

            stages = [st_D, st_E, st_NT]
            for li in range(1, 7):
                stages.append(lambda c_, grp, li=li: st_L1(c_, grp, li))
                stages.append(lambda c_, grp, li=li: st_L2(c_, grp, li))
            stages += [st_S1, st_S2, st_S3]
            for stg in stages:
                sl_ = []
                stage_ll.append(sl_)
                rec_into(sl_)
                for grp in range(2):
                    stg(CT[grp], grp)
            rec_into(tail_l)
            if need_out:
                p.dma(SP, p.Od[d][(t - OWN0) * 128:(t - OWN0 + 1) * 128, :], obuf[:], r=[('ob', step % 2, 0), ('ob', step % 2, 1)],
                      w=[('Od', d, t)])
            step += 1
            if p.debug in ('G0', 'G') and (t, d) == (32, 1):
                o = p.out("dbg_S", [128, 16, 128], F32)
                p.dma(SP, o, Sf[:], r=[('Sf', h) for h in range(16)])
        p.S.op = real_op
        for o_ in recs[0][0]:
            real_op(*o_)
        for i_, (prep_l, stage_ll, tail_l) in enumerate(recs):
            nxt = list(recs[i_ + 1][0]) if i_ + 1 < len(recs) else []
            per = -(-len(nxt) // max(1, len(stage_ll) - 2)) if nxt else 0
            for sl_ in stage_ll:
                for o_ in sl_:
                    real_op(*o_)
                for _ in range(per):
                    if nxt:
                        real_op(*nxt.pop(0))
            for o_ in nxt:
                real_op(*o_)
            for o_ in tail_l:
                real_op(*o_)
        p.S.barrier()
        stG.close()
        if p.debug == 'G':
            for d in range(2):
                o = p.out("dbg_O%d" % d, [16 * 128, 1024], F32)
                p.dma(SP, o, p.Od[d], r=[])


    def bcast_rows(p, dst, vec, key):
        dg = p.bc_dg
        for c in range(8):
            p.ts(DVE, dg[:, c * 128:(c + 1) * 128], p.ident_f[:], vec(c), None, ALU.mult, r=['modsb', 'vecs'], w=['bc_dg'])
        for hf_ in range(2):
            p.mm(p.psb[7][:, :], p.ones_f[:], dg[:, hf_ * 512:(hf_ + 1) * 512], True, True, r=['bc_dg'], w=[('ps', 7)])
            p.cp(ACT, dst[:, hf_ * 512:(hf_ + 1) * 512], p.psb[7][:, :], r=[('ps', 7)], w=[key])

    def phaseD(p):
        x = p.inp("x_seq", [L, D], F32) if "x_seq" not in p.din else p.din["x_seq"]
        w_in = p.din["w_in"]
        tabs = p.inp("dft_tab", [4, 4, 128, 2, 8, 512], BF16)
        cdft = p.inp("cdft", [128, 2, 128], BF16)
        gng = p.inp("gng_t", [1024], F32)
        wfo = p.inp("w_fo", [512, D], F32)
        wgo = p.inp("w_go", [D, D], F32)
        wmo = p.inp("w_mo", [D, D], F32)
        wr = p.inp("w_router", [D, NE], F32)
        br = p.inp("b_router", [NE], F32)
        p.X1 = p.scratch("X1", [2048, D], F32)
        p.H = p.scratch("H", [2049, D], BF16)
        p.logits = p.sb("logits", [128, 16, NE], F32)
        p.g2_b = p.sb("g2_b", [128, 1024], F32)
        stD = contextlib.ExitStack()
        p.bc_dg = p.sb("bc_dg", [128, 1024], F32, stD)
        p.bcast_rows(p.g2_b, lambda c: p.modsb[:, 40 + c, 0:1], 'g2_b')
        fmT = p.sb("fmT", [128, 4, 2048], BF16, stD)
        st1 = contextlib.ExitStack()
        Xs = p.sb("Xs", [128, 32, 512], BF16, st1)
        for lc in range(32):
            p.dma(SP, Xs[:, lc, :], p.XF[lc * 128:(lc + 1) * 128, :], w=[('Xs', lc)])
        tb = [p.sb("tb%d" % i, [128, 2, 8, 512], BF16, st1) for i in range(2)]
        cd = p.sb("cd", [128, 2, 128], BF16, st1)
        p.dma(SP, cd[:], cdft, w=['cd'])
        AB = p.sb("AB", [128, 8, 512], BF16, st1)
        SC = float(1.0 / np.sqrt(4096.0 * 128.0))
        nl = 0
        for kt in range(4):
            for q4 in range(4):
                tbuf = tb[nl % 2]
                tk = ('tb', nl % 2)
                nl += 1
                p.dma(SP, tbuf[:], tabs[kt, q4], w=[tk])
                for lc in range(8):
                    la = q4 * 8 + lc
                    for g_ in range(4):
                        for ab in range(2):
                            p.mm(p.psb[g_ * 2 + ab][:, :], Xs[:, la, g_ * 128:(g_ + 1) * 128], tbuf[:, ab, lc, :], la == 0, la == 31,
                                 r=[('Xs', la), tk], w=[('ps', g_ * 2 + ab)])
            for i8 in range(8):
                p.cp(ACT if i8 % 2 == 0 else DVE, AB[:, i8, :], p.psb[i8][:, :], r=[('ps', i8)], w=[('AB', i8)])
            for g_ in range(4):
                p.mm(p.psb[g_][:, :], cd[:, 0, :], AB[:, g_ * 2, :], True, False, r=['cd', ('AB', g_ * 2)], w=[('ps', g_)])
                p.mm(p.psb[g_][:, :], cd[:, 1, :], AB[:, g_ * 2 + 1, :], False, True, r=['cd', ('AB', g_ * 2 + 1)], w=[('ps', g_)])
                p.act(fmT[:, g_, kt * 512:(kt + 1) * 512], p.psb[g_][:, :], AF.Copy, r=[('ps', g_)], w=[('fmT', g_, kt)], scale=SC)
        p.S.barrier()
        st1.close()
        if p.debug == 'D1':
            o = p.out("dbg_fmT", [128, 4, 2048], BF16)
            p.dma(SP, o, fmT[:], r=[])
            p.S.barrier(); stD.close()
            return
        st2 = stD
        wv = w_in.rearrange("(kc q) n -> q kc n", q=128)
        Wzg = p.sb("Wzg", [128, 8, 3072], BF16, st2)
        for kc in range(8):
            p.dma(POOL, Wzg[:, kc, 0:1024], wv[:, kc, Z_OFF:Z_OFF + 1024], w=['Wzg'])
            p.dma(POOL, Wzg[:, kc, 1024:3072], wv[:, kc, GA_OFF:GA_OFF + 2048], w=['Wzg'])
        Wfo = p.sb("Wfo", [128, 4, D], BF16, st2)
        p.dma(POOL, Wfo[:], wfo.rearrange("(kc q) n -> q kc n", q=128), w=['Wfo'])
        Wgo = p.sb("Wgo", [128, 8, D], BF16, st2)
        p.dma(POOL, Wgo[:], wgo.rearrange("(kc q) n -> q kc n", q=128), w=['Wgo'])
        Wmo = p.sb("Wmo", [128, 8, D], BF16, st2)
        p.dma(POOL, Wmo[:], wmo.rearrange("(kc q) n -> q kc n", q=128), w=['Wmo'])
        Wr = p.sb("Wr", [128, 8, NE], F32, st2)
        p.dma(SP, Wr[:], wr.rearrange("(kc q) n -> q kc n", q=128), w=['Wr'])
        brb = p.sb("brb", [128, NE], F32, st2)
        p.dma(SP, brb[:], br.partition_broadcast(128), w=['brb'])
        gnb = p.sb("gnb", [128, 1024], F32, st2)
        p.dma(SP, gnb[:], gng.partition_broadcast(128), w=['gnb'])
        g1_b = p.sb("g1_b", [128, 1024], F32, st2)
        gs2_b = p.sb("gs2_b", [128, 1024], F32, st2)
        sh2_b = p.sb("sh2_b", [128, 1024], F32, st2)
        p.bcast_rows(g1_b, lambda c: p.modsb[:, 16 + c, 0:1], 'g1_b')
        p.bcast_rows(gs2_b, lambda c: p.vecs[:, 2, c:c + 1], 'gs2_b')
        p.bcast_rows(sh2_b, lambda c: p.modsb[:, 24 + c, 0:1], 'sh2_b')
        zrow = p.sb("zrow", [1, D], BF16, st2)
        p.S.op(DVE, lambda e: e.memset(zrow[:], 0.0), [], ['zrow'])
        p.dma(SP, p.H[2048:2049, :], zrow[:], r=['zrow'], w=[('H', 'z')])
        p.S.barrier()
        uT = p.sb("uTd", [128, 8, 512], BF16, st2)
        ybT = p.sb("ybT", [128, 8, 512], BF16, st2)
        mT = p.sb("mT", [128, 8, 512], BF16, st2)
        bufs = {
            'x': (p.sb("xtd", [128, D], F32, st2), 'xtd'),
            'junk': (p.sb("junkd", [128, D], BF16, st2), 'junkd'),
            'ss': (p.sb("ssd", [128, 4], F32, st2), 'ssd'),
            'xn': (p.sb("xnd", [128, D], BF16, st2), 'xnd'),
            'ps': (p.psb[0], ('ps', 0)),
        }
        zs = p.sb("zs", [128, D], F32, st2)
        of_ = p.sb("of_", [128, D], F32, st2)
        ob_ = p.sb("ob_", [128, D], F32, st2)
        on8 = p.sb("on8", [128, 4, 8], F32, st2)
        ybin = p.sb("ybin", [128, D], BF16, st2)
        gw = [p.sb("gw%d" % i, [128, 512], BF16, st2) for i in range(4)]
        x1 = p.sb("x1", [128, D], F32, st2)
        xt2 = p.sb("xt2d", [128, D], F32, st2)
        h2 = p.sb("h2", [128, D], F32, st2)
        h2b = p.sb("h2b", [128, D], BF16, st2)
        h2T = p.sb("h2T", [128, 8, 128], F32, st2)
        s2 = p.sb("s2", [128, 4], F32, st2)
        real_op = p.S.op
        recD = []

        def rec_into(lst):
            p.S.op = lambda eng, fn, reads=(), writes=(), dma=False: lst.append((eng, fn, list(reads), list(writes), dma))

        for sti in range(4):
            head_l, mid_l, tail_l = [], [], []
            recD.append((head_l, mid_l, tail_l))
            rec_into(head_l)
            for ti in range(4):
                tt_ = sti * 4 + ti
                t = OWN0 + tt_
                p.norm_T(x[t * 128:(t + 1) * 128, :], p.gs1, p.sh1, lambda c: (uT[:, c, ti * 128:(ti + 1) * 128], ('uTd', ti)), bufs, 'd')
                for hf_ in range(2):
                    ps = p.psb[1 + hf_]
                    for kc in range(8):
                        p.mm(ps[:, :], uT[:, kc, ti * 128:(ti + 1) * 128], Wzg[:, kc, hf_ * 512:(hf_ + 1) * 512], kc == 0, kc == 7,
                             r=[('uTd', ti), 'Wzg'], w=[('ps', 1 + hf_)])
                    sl = slice(hf_ * 512, (hf_ + 1) * 512)
                    p.act(zs[:, sl], ps[:, :], AF.Silu, r=[('ps', 1 + hf_)], w=[('zs', hf_)])
                p.dma(SP, of_[:], p.Od[0][tt_ * 128:(tt_ + 1) * 128, :], w=['of_'])
                p.dma(SP, ob_[:], p.Od[1][tt_ * 128:(tt_ + 1) * 128, :], w=['ob_'])
                p.tt(DVE, of_[:], of_[:], ob_[:], ALU.add, r=['of_', 'ob_'], w=['of_'])
                p.tt(DVE, ob_[:], of_[:], of_[:], ALU.mult, r=['of_', 'ob_'], w=['ob_'])
                p.S.op(DVE, lambda e: e.tensor_reduce(on8[:, 0, :], ob_[:].rearrange("q (h c) -> q h c", c=128), AX.X, ALU.add), ['ob_'], ['on8'])
                p.ts(DVE, on8[:, 1, :], on8[:, 0, :], 1.0 / 128, EPS, ALU.mult, ALU.add, r=['on8'], w=['on8'])
                p.act(on8[:, 2, :], on8[:, 1, :], AF.Ln, r=['on8'], w=['on8'])
                p.act(on8[:, 3, :], on8[:, 2, :], AF.Exp, r=['on8'], w=['on8'], scale=-0.5)
                for h in range(8):
                    hs = slice(h * 128, (h + 1) * 128)
                    p.stt(DVE, of_[:, hs], of_[:, hs], on8[:, 3, h:h + 1], gnb[:, hs], ALU.mult, ALU.mult, r=['of_', 'on8', 'gnb'], w=['of_'])
                p.tt(DVE, ybin[:], of_[:], zs[:], ALU.mult, r=['of_', ('zs', 0), ('zs', 1)], w=['ybin'])
                pv = p.psb[3][:].bitcast(BF16)
                for c in range(8):
                    p.tr(pv[:, c * 128:(c + 1) * 128], ybin[:, c * 128:(c + 1) * 128], p.ident_bf[:], r=['ybin'], w=[('ps', 3)])
                p.cp(ACT, ybT[:, :, ti * 128:(ti + 1) * 128], pv[:, :].rearrange("q (c k) -> q c k", k=128), r=[('ps', 3)], w=[('ybT', ti)])
            rec_into(mid_l)
            allu = [('uTd', i) for i in range(4)]
            ally = [('ybT', i) for i in range(4)]
            k0 = sti * 512
            for dc in range(8):
                dsl = slice(dc * 128, (dc + 1) * 128)
                b0 = 4 * (dc % 2)
                for fc in range(4):
                    p.mm(p.psb[b0][:, :], Wfo[:, fc, dsl], fmT[:, fc, k0:k0 + 512], fc == 0, fc == 3, r=['Wfo'], w=[('ps', b0)])
                for fc in range(8):
                    p.mm(p.psb[b0 + 1][:, :], Wgo[:, fc, dsl], ybT[:, fc, :], fc == 0, fc == 7, r=['Wgo'] + ally, w=[('ps', b0 + 1)])
                for kc in range(8):
                    p.mm(p.psb[b0 + 2][:, :], Wzg[:, kc, 1024 + dc * 128:1024 + (dc + 1) * 128], uT[:, kc, :], kc == 0, kc == 7, r=['Wzg'] + allu, w=[('ps', b0 + 2)])
                for kc in range(8):
                    p.mm(p.psb[b0 + 3][:, :], Wzg[:, kc, 2048 + dc * 128:2048 + (dc + 1) * 128], uT[:, kc, :], kc == 0, kc == 7, r=['Wzg'] + allu, w=[('ps', b0 + 3)])
                for br_, (pg, py) in enumerate([(b0 + 2, b0), (b0 + 3, b0 + 1)]):
                    gb_ = gw[(dc % 2) * 2 + br_]
                    gk_ = ('gw', (dc % 2) * 2 + br_)
                    p.act(gb_[:], p.psb[pg][:, :], AF.Sigmoid, r=[('ps', pg)], w=[gk_])
                    p.tt(DVE, gb_[:], gb_[:], p.psb[py][:, :], ALU.mult, r=[gk_, ('ps', py)], w=[gk_])
                p.tt(DVE, mT[:, dc, :], gw[(dc % 2) * 2][:], gw[(dc % 2) * 2 + 1][:], ALU.add, r=[('gw', (dc % 2) * 2), ('gw', (dc % 2) * 2 + 1)], w=[('mT', dc)])
            allm = [('mT', i) for i in range(8)]
            rec_into(tail_l)
            for ti in range(4):
                tt_ = sti * 4 + ti
                t = OWN0 + tt_
                xt, xk = xt2, 'xt2'
                p.dma(SP, xt[:], x[t * 128:(t + 1) * 128, :], w=[xk])
                for hf_ in range(2):
                    ps = p.psb[4 + hf_]
                    sl = slice(hf_ * 512, (hf_ + 1) * 512)
                    for dc in range(8):
                        p.mm(ps[:, :], mT[:, dc, ti * 128:(ti + 1) * 128], Wmo[:, dc, sl], dc == 0, dc == 7, r=allm + ['Wmo'], w=[('ps', 4 + hf_)])
                    p.tt(DVE, x1[:, sl], ps[:, :], g1_b[:, sl], ALU.mult, r=[('ps', 4 + hf_)], w=['x1'])
                p.tt(DVE, x1[:], x1[:], xt[:], ALU.add, r=['x1', xk], w=['x1'])
                p.dma(POOL, p.X1[tt_ * 128:(tt_ + 1) * 128, :], x1[:], r=['x1'], w=[('X1', tt_)])
                p.act(h2[:], x1[:], AF.Square, r=['x1'], w=['h2', 's2'], accum_out=s2[:, 0:1])
                p.act(s2[:, 2:3], s2[:, 0:1], AF.Ln, r=['s2'], w=['s2'], scale=1.0 / D, bias=p.eps_t[:, 0:1])
                p.act(s2[:, 3:4], s2[:, 2:3], AF.Exp, r=['s2'], w=['s2'], scale=-0.5)
                p.stt(DVE, h2[:], x1[:], s2[:, 3:4], gs2_b[:], ALU.mult, ALU.mult, r=['x1', 's2', 'h2'], w=['h2'])
                p.tt(DVE, h2[:], h2[:], sh2_b[:], ALU.add, r=['h2'], w=['h2'])
                p.cp(ACT, h2b[:], h2[:], r=['h2'], w=['h2b'])
                p.dma(POOL, p.H[tt_ * 128:(tt_ + 1) * 128, :], h2b[:], r=['h2b'], w=[('H', tt_)])
                for hf_ in range(2):
                    for c in range(4):
                        p.tr(p.psb[6][:, c * 128:(c + 1) * 128], h2[:, (hf_ * 4 + c) * 128:(hf_ * 4 + c + 1) * 128], p.ident_f[:], r=['h2'], w=[('ps', 6)])
                    p.cp(ACT, h2T[:, hf_ * 4:(hf_ + 1) * 4, :], p.psb[6][:, :].rearrange("q (c k) -> q c k", k=128), r=[('ps', 6)], w=['h2T'])
                for kc in range(8):
                    p.mm(p.psb[7][:, 0:NE], h2T[:, kc, :], Wr[:, kc, :], kc == 0, kc == 7, r=['h2T', 'Wr'], w=[('ps', 7)])
                p.tt(DVE, p.logits[:, tt_, :], p.psb[7][:, 0:NE], brb[:], ALU.add, r=[('ps', 7), 'brb'], w=[('logits', tt_)])
        p.S.op = real_op
        for o_ in recD[0][0]:
            real_op(*o_)
        for i_, (head_l, mid_l, tail_l) in enumerate(recD):
            for o_ in mid_l:
                real_op(*o_)
            nxt = list(recD[i_ + 1][0]) if i_ + 1 < len(recD) else []
            tl = list(tail_l)
            CH = 6
            while tl or nxt:
                for _ in range(CH):
                    if tl:
                        real_op(*tl.pop(0))
                for _ in range(CH):
                    if nxt:
                        real_op(*nxt.pop(0))
        p.S.barrier()
        stD.close()
        if p.debug == 'D':
            o = p.out("dbg_x1", [2048, D], F32)
            p.dma(SP, o, p.X1, r=[])
            o = p.out("dbg_logits", [128, 16, NE], F32)
            p.dma(SP, o, p.logits[:], r=[])
            o = p.out("dbg_H", [2049, D], BF16)
            p.dma(SP, o, p.H, r=[])


    def phaseE(p):
        CAPS = MOE_CAPS
        TB = [sum(CAPS[:i]) for i in range(NE)]
        NB = sum(CAPS)
        DUMMY = NB * 128
        CAPMAX = max(CAPS) * 128
        wg = p.inp("w_gate", [NE, D, D], F32)
        wu = p.inp("w_up", [NE, D, D], F32)
        wd = p.inp("w_down", [NE, D, D], F32)
        bg = p.inp("b_gate", [NE, D], F32)
        bu = p.inp("b_up", [NE, D], F32)
        bd = p.inp("b_down", [NE, D], F32)
        fng = p.inp("fng", [D], F32)
        yout = p.out("y", [2048, D], F32)
        Y = p.scratch("Yslots", [NB * 128 + 128, D], F32)
        IDX = p.scratch("IDX", [NB * 128 + 128, 1], I32)
        stE = contextlib.ExitStack()
        K = {}
        for nm, dt_ in [('ones_bf', BF16), ('ustrict_bf', BF16), ('iota32', F32), ('ecol', F32), ('blk128', F32),
                        ('tokid', I32), ('l_strict', F32), ('basetab', F32), ('captab', F32), ('rowoff', F32)]:
            a = p.const_np[nm]
            d = p.inp("c_" + nm, a.shape, dt_) if ("c_" + nm) not in p.din else p.din["c_" + nm]
            K[nm] = p.sb("ke_" + nm, a.shape, dt_, stE)
            p.dma(SP, K[nm][:], d, w=[('k', nm)])
        Bg = p.sb("Bg", [32, D], BF16, stE)
        Bu = p.sb("Bu", [32, D], BF16, stE)
        Bd = p.sb("Bd", [32, D], BF16, stE)
        p.dma(POOL, Bg[:], bg, w=['Bg'])
        p.dma(POOL, Bu[:], bu, w=['Bu'])
        p.dma(POOL, Bd[:], bd, w=['Bd'])
        fnb = p.sb("fnb", [128, D], F32, stE)
        p.dma(SP, fnb[:], fng.partition_broadcast(128), w=['fnb'])
        i2048 = p.sb("i2048", [128, NB], I32, stE)
        p.S.op(DVE, lambda e: e.memset(i2048[:], 2048), [], ['i2048'])
        p.dma(SP, IDX[0:NB * 128, :].rearrange("(q b) o -> q (b o)", q=128), i2048[:], r=['i2048'], w=['IDX'])
        zy = p.sb("zy", [128, D], F32, stE)
        p.S.op(DVE, lambda e: e.memset(zy[:], 0.0), [], ['zy'])
        p.dma(SP, Y[DUMMY:DUMMY + 128, :], zy[:], r=['zy'], w=['Yz'])
        p.S.barrier()
        mx = p.sb("mx", [128, 16, 8], F32, stE)
        mi = p.sb("mi", [128, 16, 8], U32, stE)
        idf = p.sb("idf", [128, 16, 4], F32, stE)
        wts = p.sb("wts", [128, 16, 4], F32, stE)
        nm0 = p.sb("nm0", [128, 16], F32, stE)
        ssum = p.sb("ssum", [128, 16], F32, stE)
        Mf = p.sb("Mf", [128, 16, NE], F32, stE)
        Mb = p.sb("Mb", [128, 16, NE], BF16, stE)
        for tt_ in range(16):
            p.S.op(DVE, lambda e, tt_=tt_: e.max(mx[:, tt_, :], p.logits[:, tt_, :]), [], [('mx', tt_)])
            p.S.op(DVE, lambda e, tt_=tt_: e.max_index(mi[:, tt_, :], mx[:, tt_, :], p.logits[:, tt_, :]), [('mx', tt_)], [('mi', tt_)])
            p.cp(DVE, idf[:, tt_, :], mi[:, tt_, 0:4], r=[('mi', tt_)], w=[('idf', tt_)])
            p.ts(DVE, nm0[:, tt_:tt_ + 1], mx[:, tt_, 0:1], -1.0, None, ALU.mult, r=[('mx', tt_)], w=[('nm0', tt_)])
            p.act(wts[:, tt_, :], mx[:, tt_, 0:4], AF.Exp, r=[('mx', tt_), ('nm0', tt_)], w=[('wts', tt_)], bias=nm0[:, tt_:tt_ + 1], scale=1.0)
            p.S.op(DVE, lambda e, tt_=tt_: e.tensor_reduce(ssum[:, tt_:tt_ + 1], wts[:, tt_, :], AX.X, ALU.add), [('wts', tt_)], [('ssum', tt_)])
            p.S.op(DVE, lambda e, tt_=tt_: e.reciprocal(ssum[:, tt_:tt_ + 1], ssum[:, tt_:tt_ + 1]), [('ssum', tt_)], [('ssum', tt_)])
            p.ts(DVE, wts[:, tt_, :], wts[:, tt_, :], ssum[:, tt_:tt_ + 1], None, ALU.mult, r=[('wts', tt_), ('ssum', tt_)], w=[('wts', tt_)])
            p.ts(DVE, Mf[:, tt_, :], K['iota32'][:], idf[:, tt_, 0:1], None, ALU.is_equal, r=[('idf', tt_)], w=[('Mf', tt_)])
            for j in range(1, 4):
                p.stt(DVE, Mf[:, tt_, :], K['iota32'][:], idf[:, tt_, j:j + 1], Mf[:, tt_, :], ALU.is_equal, ALU.add, r=[('idf', tt_), ('Mf', tt_)], w=[('Mf', tt_)])
            p.cp(DVE, Mb[:, tt_, :], Mf[:, tt_, :], r=[('Mf', tt_)], w=[('Mb', tt_)])
        allM = [('Mb', i) for i in range(16)]
        for tt_ in range(16):
            p.mm(p.psb[0][:, 0:NE], K['ones_bf'][:], Mb[:, tt_, :], tt_ == 0, tt_ == 15, r=allM, w=[('ps', 0)])
        for tt_ in range(16):
            p.mm(p.psb[1][0:32, 0:1], Mb[:, tt_, :], K['ones_bf'][:, 0:1], tt_ == 0, tt_ == 15, r=allM, w=[('ps', 1)])
        cntb = p.sb("cntb", [128, NE], F32, stE)
        cc = p.sb("cc", [32, 8], F32, stE)
        sq32 = p.sb("sq32", [32, 3, NE], F32, stE)
        Pm = p.sb("Pm", [32, NE], F32, stE)
        p.cp(DVE, cntb[:], p.psb[0][:, 0:NE], r=[('ps', 0)], w=['cntb'])
        p.cp(DVE, cc[:, 0:1], p.psb[1][0:32, 0:1], r=[('ps', 1)], w=['cc'])
        p.ts(DVE, sq32[:, 0, :], cntb[0:32, :], cc[:, 0:1], None, ALU.is_gt, r=['cntb', 'cc'], w=['sq32'])
        p.ts(DVE, sq32[:, 1, :], cntb[0:32, :], cc[:, 0:1], None, ALU.is_equal, r=['cntb', 'cc'], w=['sq32'])
        p.tt(DVE, sq32[:, 1, :], sq32[:, 1, :], K['l_strict'][0:32, 0:32], ALU.mult, r=['sq32'], w=['sq32'])
        p.tt(DVE, sq32[:, 0, :], sq32[:, 0, :], sq32[:, 1, :], ALU.add, r=['sq32'], w=['sq32'])
        p.S.op(DVE, lambda e: e.tensor_reduce(cc[:, 1:2], sq32[:, 0, :], AX.X, ALU.add), ['sq32'], ['cc'])
        p.ts(DVE, Pm[:], K['iota32'][0:32, :], cc[:, 1:2], None, ALU.is_equal, r=['cc'], w=['Pm'])
        p.tt(DVE, sq32[:, 0, :], Pm[:], K['basetab'][0:32, :], ALU.mult, r=['Pm', 'sq32'], w=['sq32'])
        p.S.op(DVE, lambda e: e.tensor_reduce(cc[:, 2:3], sq32[:, 0, :], AX.X, ALU.add), ['sq32'], ['cc'])
        p.tt(DVE, sq32[:, 1, :], Pm[:], K['captab'][0:32, :], ALU.mult, r=['Pm', 'sq32'], w=['sq32'])
        p.S.op(DVE, lambda e: e.tensor_reduce(cc[:, 3:4], sq32[:, 1, :], AX.X, ALU.add), ['sq32'], ['cc'])
        lb = p.sb("lb", [32, 3, 128], F32, stE)
        one32f = p.sb("one32f", [32, 128], F32, stE)
        p.S.op(DVE, lambda e: e.memset(one32f[:], 1.0), [], ['one32f'])
        p.ts(DVE, lb[:, 0, :], one32f[:], cc[:, 2:3], None, ALU.mult, r=['one32f', 'cc'], w=['lb'])
        p.ts(DVE, lb[:, 1, :], one32f[:], cc[:, 3:4], None, ALU.mult, r=['one32f', 'cc'], w=['lb'])
        p.ts(DVE, lb[:, 2, :], one32f[:], K['ecol'][0:32, 0:1], None, ALU.mult, r=['one32f'], w=['lb'])
        p.mm(p.psb[0][:, 0:NE], lb[:, 0, :], p.ident_f[0:32, 0:32], True, True, r=['lb'], w=[('ps', 0)])
        p.mm(p.psb[0][:, 32:64], lb[:, 1, :], p.ident_f[0:32, 0:32], True, True, r=['lb'], w=[('ps', 0)])
        p.mm(p.psb[0][:, 64:96], lb[:, 2, :], Pm[:], True, True, r=['lb', 'Pm'], w=[('ps', 0)])
        bcb = p.sb("bcb", [128, 3, NE], F32, stE)
        p.ts(DVE, bcb[:, 0, :], p.psb[0][:, 0:NE], -float(DUMMY), None, ALU.add, r=[('ps', 0)], w=['bcb'])
        p.cp(DVE, bcb[:, 1, :], p.psb[0][:, 32:64], r=[('ps', 0)], w=['bcb'])
        p.ts(DVE, bcb[:, 2, :], p.psb[0][:, 64:96], 128.0, None, ALU.mult, r=[('ps', 0)], w=['bcb'])
        idwf = p.sb("idwf", [128, NE], F32, stE)
        idw = p.sb("idw", [128, NE], I32, stE)
        p.ts(DVE, idwf[:], bcb[:, 2, :], K['rowoff'][:, 0:1], None, ALU.add, r=['bcb'], w=['idwf'])
        p.cp(DVE, idw[:], idwf[:], r=['idwf'], w=['idw'])
        rk = p.sb("rk", [128, NE], F32, stE)
        sel = p.sb("sel", [128, NE], F32, stE)
        destf = p.sb("destf", [128, 16, 4], F32, stE)
        desti = p.sb("desti", [128, 16, 4], I32, stE)
        for tt_ in range(16):
            ps = p.psb[2 + tt_ % 2]
            pk = ('ps', 2 + tt_ % 2)
            for t2 in range(tt_):
                p.mm(ps[:, 0:NE], K['ones_bf'][:], Mb[:, t2, :], t2 == 0, False, r=allM, w=[pk])
            p.mm(ps[:, 0:NE], K['ustrict_bf'][:], Mb[:, tt_, :], tt_ == 0, True, r=allM, w=[pk])
            p.tt(DVE, sel[:], ps[:, 0:NE], bcb[:, 1, :], ALU.is_lt, r=[pk, 'bcb'], w=['sel'])
            p.tt(DVE, rk[:], ps[:, 0:NE], bcb[:, 0, :], ALU.add, r=[pk, 'bcb'], w=['rk'])
            p.tt(DVE, rk[:], rk[:], sel[:], ALU.mult, r=['rk', 'sel'], w=['rk'])
            p.ts(DVE, rk[:], rk[:], float(DUMMY), None, ALU.add, r=['rk'], w=['rk'])
            for j in range(4):
                p.ts(DVE, sel[:], K['iota32'][:], idf[:, tt_, j:j + 1], None, ALU.is_equal, r=[('idf', tt_)], w=['sel'])
                p.tt(DVE, sel[:], sel[:], rk[:], ALU.mult, r=['sel', 'rk'], w=['sel'])
                p.S.op(DVE, lambda e, tt_=tt_, j=j: e.tensor_reduce(destf[:, tt_, j:j + 1], sel[:], AX.X, ALU.add), ['sel'], [('destf', tt_)])
            p.cp(DVE, desti[:, tt_, :], destf[:, tt_, :], r=[('destf', tt_)], w=[('desti', tt_)])
            for j in range(4):
                p.S.op(POOL, lambda e, tt_=tt_, j=j: e.indirect_dma_start(
                    out=IDX[:, :], out_offset=bass.IndirectOffsetOnAxis(ap=desti[:, tt_, j:j + 1], axis=0),
                    in_=K['tokid'][:, tt_:tt_ + 1], in_offset=None), [('desti', tt_), 'IDX0'], [('IDXs', tt_, j)], dma=True)
        p.S.barrier()
        idx_sb = p.sb("idx_sb", [128, NB], I32, stE)
        p.dma(SP, idx_sb[:], IDX[0:NB * 128, :].rearrange("(b q) o -> q (b o)", q=128), w=['idx_sb'], allow_slow_non_contiguous=True)
        p.S.barrier()
        stX = contextlib.ExitStack()
        Wb = [[p.sb("W%s%d" % (n_, i), [128, 8, D], BF16, stX) for n_ in "gud"] for i in range(2)]
        xg = [p.sb("xg%d" % i, [128, D], BF16, stX) for i in range(2)]
        xT = p.sb("xTe", [128, 8, CAPMAX], BF16, stX)
        aT = p.sb("aT", [128, 8, CAPMAX], BF16, stX)
        ohb = p.sb("ohb", [32, 512], BF16, stX)
        ones32 = p.sb("ones32", [32, 512], BF16, stX)
        p.S.op(DVE, lambda e: e.memset(ones32[:], 1.0), [], ['ones32'])
        wk = [[p.sb("wk%d_%d" % (i, j), [128, 512], BF16, stX) for j in range(4)] for i in range(2)]
        ysb = [p.sb("ysb%d" % i, [128, D], F32, stX) for i in range(2)]
        w2d = [w_.rearrange("e (q j) n -> (e q) (j n)", j=8) for w_ in (wg, wu, wd)]
        ng_ = 0
        nd_ = 0
        nch = 0
        for ex in range(NE):
            wbi = ex % 2
            capt = CAPS[ex]
            for wi_ in range(3):
                p.S.op(POOL, lambda e, wbi=wbi, wi_=wi_, ex=ex: e.indirect_dma_start(
                    out=Wb[wbi][wi_][:].rearrange("q j n -> q (j n)"), out_offset=None, in_=w2d[wi_][:, :],
                    in_offset=bass.IndirectOffsetOnAxis(ap=idw[:, ex:ex + 1], axis=0)), ['idw'], [('W', wbi, wi_)], dma=True)
            p.ts(DVE, ohb[:], ones32[:], Pm[:, ex:ex + 1], None, ALU.mult, r=['ones32', 'Pm'], w=['ohb'])
            for k in range(capt):
                b = TB[ex] + k
                gi = ng_ % 2
                ng_ += 1
                p.S.op(POOL, lambda e, b=b, gi=gi: e.indirect_dma_start(
                    out=xg[gi][:, :], out_offset=None, in_=p.H[:, :],
                    in_offset=bass.IndirectOffsetOnAxis(ap=idx_sb[:, b:b + 1], axis=0)), ['idx_sb'], [('xg', gi)], dma=True)
                pb_ = 0 if k % 2 == 0 else 7
                pv = p.psb[pb_][:].bitcast(BF16)
                xgv = xg[gi][:].rearrange("s (q j) -> s j q", j=8)
                for c in range(8):
                    p.tr(pv[:, c * 128:(c + 1) * 128], xgv[:, c, :], p.ident_bf[:], r=[('xg', gi)], w=[('ps', pb_)])
                p.cp(ACT if k % 2 == 0 else DVE, xT[:, :, k * 128:(k + 1) * 128], pv[:, :].rearrange("q (c k) -> q c k", k=128),
                     r=[('ps', pb_)], w=[('xTe', k)])
            allx = [('xTe', k) for k in range(capt)]
            nsl = capt * 128
            chunks = [(c0, min(512, nsl - c0)) for c0 in range(0, nsl, 512)]
            for fc in range(8):
                fs = slice(fc * 128, (fc + 1) * 128)
                for (c0, n_) in chunks:
                    st_ = nch % 2
                    nch += 1
                    bG, bU = 1 + 2 * st_, 2 + 2 * st_
                    for (wi_, Bt, bk) in ((0, Bg, bG), (1, Bu, bU)):
                        for kc in range(8):
                            p.mm(p.psb[bk][:, 0:n_], Wb[wbi][wi_][:, kc, :].rearrange("q (f j) -> q j f", j=8)[:, fc, :], xT[:, kc, c0:c0 + n_],
                                 kc == 0, False, r=[('W', wbi, wi_)] + allx, w=[('ps', bk)])
                        p.mm(p.psb[bk][:, 0:n_], Bt[:].rearrange("e (f j) -> e j f", j=8)[:, fc, :], ohb[:, 0:n_], False, True, r=['ohb'], w=[('ps', bk)])
                    g_, sg_, u_, t_ = wk[st_]
                    wkk = lambda j: ('wk', st_, j)
                    p.ts(DVE, g_[:, 0:n_], p.psb[bG][:, 0:n_], 7.0, None, ALU.min, r=[('ps', bG)], w=[wkk(0)])
                    p.act(sg_[:, 0:n_], g_[:, 0:n_], AF.Sigmoid, r=[wkk(0)], w=[wkk(1)], scale=1.702)
                    p.ts(DVE, u_[:, 0:n_], p.psb[bU][:, 0:n_], 7.0, -7.0, ALU.min, ALU.max, r=[('ps', bU)], w=[wkk(2)])
                    p.stt(DVE, t_[:, 0:n_], u_[:, 0:n_], 1.0, g_[:, 0:n_], ALU.add, ALU.mult, r=[wkk(2), wkk(0)], w=[wkk(3)])
                    p.tt(DVE, aT[:, fc, c0:c0 + n_], t_[:, 0:n_], sg_[:, 0:n_], ALU.mult, r=[wkk(3), wkk(1)], w=[('aT', fc)])
            alla = [('aT', fc) for fc in range(8)]
            for k in range(capt):
                b = TB[ex] + k
                yi = nd_ % 2
                nd_ += 1
                yb_ = ysb[yi]
                for hf_ in range(2):
                    hs = slice(hf_ * 512, (hf_ + 1) * 512)
                    pb_ = 5 if hf_ == 0 else 6
                    for fc in range(8):
                        p.mm(p.psb[pb_][:, :], aT[:, fc, k * 128:(k + 1) * 128], Wb[wbi][2][:, fc, hs], fc == 0, False, r=alla + [('W', wbi, 2)], w=[('ps', pb_)])
                    p.mm(p.psb[pb_][:, :], ohb[:, 0:128], Bd[:, hs], False, True, r=['ohb'], w=[('ps', pb_)])
                    p.cp(ACT, yb_[:, hs], p.psb[pb_][:, :], r=[('ps', pb_)], w=[('ysb', yi)])
                p.dma(SP, Y[b * 128:(b + 1) * 128, :], yb_[:], r=[('ysb', yi)], w=[('Y', b)])
        p.S.barrier()
        stX.close()
        yg = [p.sb("yg%d" % i, [128, D], F32, stE) for i in range(4)]
        acc = p.sb("acc", [128, D], F32, stE)
        x1ts = [p.sb("x1t%d" % i, [128, D], F32, stE) for i in range(2)]
        outb = [p.sb("outb%d" % i, [128, D], F32, stE) for i in range(2)]
        p.dma(SP, x1ts[0][:], p.X1[0:128, :], w=[('x1t', 0)])
        fs_ = p.sb("fs_", [128, 4], F32, stE)
        ng = 0
        for tt_ in range(16):
            x1t = x1ts[tt_ % 2]
            xk_ = ('x1t', tt_ % 2)
            ot = outb[tt_ % 2]
            ok_ = ('outb', tt_ % 2)
            if tt_ + 1 < 16:
                p.dma(SP, x1ts[(tt_ + 1) % 2][:], p.X1[(tt_ + 1) * 128:(tt_ + 2) * 128, :], w=[('x1t', (tt_ + 1) % 2)])
            for j in range(4):
                yb_ = yg[ng % 4]
                yk = ('yg', ng % 4)
                ng += 1
                p.S.op(POOL, lambda e, tt_=tt_, j=j, yb_=yb_: e.indirect_dma_start(
                    out=yb_[:, :], out_offset=None, in_=Y[:, :],
                    in_offset=bass.IndirectOffsetOnAxis(ap=desti[:, tt_, j:j + 1], axis=0)), [], [yk], dma=True)
                if j == 0:
                    p.ts(DVE, acc[:], yb_[:], wts[:, tt_, 0:1], None, ALU.mult, r=[yk], w=['acc'])
                else:
                    p.stt(DVE, acc[:], yb_[:], wts[:, tt_, j:j + 1], acc[:], ALU.mult, ALU.add, r=[yk, 'acc'], w=['acc'])
            p.tt(DVE, acc[:], acc[:], p.g2_b[:], ALU.mult, r=['acc'], w=['acc'])
            p.tt(DVE, acc[:], acc[:], x1t[:], ALU.add, r=['acc', xk_], w=['acc'])
            p.act(ot[:], acc[:], AF.Square, r=['acc'], w=[ok_, 'fs_'], accum_out=fs_[:, 0:1])
            p.act(fs_[:, 2:3], fs_[:, 0:1], AF.Ln, r=['fs_'], w=['fs_'], scale=1.0 / D, bias=p.eps_t[:, 0:1])
            p.act(fs_[:, 3:4], fs_[:, 2:3], AF.Exp, r=['fs_'], w=['fs_'], scale=-0.5)
            p.stt(DVE, ot[:], acc[:], fs_[:, 3:4], fnb[:], ALU.mult, ALU.mult, r=['acc', 'fs_', ok_], w=[ok_])
            p.dma(SP, yout[tt_ * 128:(tt_ + 1) * 128, :], ot[:], r=[ok_], w=[('yout', tt_)])
        p.S.barrier()
        stE.close()


def core_inputs(inputs, core, consts):
    b, hf = core // 2, core % 2
    rev = (hf == 0)
    m = {}
    x = np.asarray(inputs['x'][b], np.float32)
    m['x_seq'] = np.ascontiguousarray(x[::-1] if rev else x)
    cp = np.stack([inputs['c'][b], inputs['c_ctx']], 0).astype(np.float32)
    m['cT'] = np.ascontiguousarray(cp.reshape(2, 8, 128).transpose(2, 1, 0))
    m['bmod'] = fm_layout(inputs['b_mod'][0], 48)
    m['n1g'] = fm_layout(inputs['norm1_g'][0], 8)
    m['n2g'] = fm_layout(inputs['norm2_g'][0], 8)
    m['w_mod'] = np.ascontiguousarray(inputs['w_mod'][0], np.float32)
    cx = np.asarray(inputs['ctx'][b], np.float32)
    m['ctx_seq'] = np.ascontiguousarray(cx[::-1] if rev else cx)
    w_in = np.array(inputs['w_in'][0], np.float32)
    if rev:
        w2 = w_in.copy()
        w2[:, 2048:2056], w2[:, 2056:2064] = w_in[:, 2056:2064], w_in[:, 2048:2056]
        w2[:, 2064:2072], w2[:, 2072:2080] = w_in[:, 2072:2080], w_in[:, 2064:2072]
        w_in = w2
    m['w_in'] = np.ascontiguousarray(w_in)
    cwv = np.asarray(inputs['conv_w'][0], np.float32).reshape(9, 16, 128)
    if rev:
        cwv = cwv[::-1]
    cd = np.zeros((128, 16, 9, 128), np.float32)
    qi = np.arange(128)
    cd[qi, :, :, qi] = cwv.transpose(2, 1, 0)
    m['conv_diag'] = cd
    al = np.asarray(inputs['a_log'][0], np.float32)
    db = np.asarray(inputs['dt_bias'][0], np.float32)
    if rev:
        al, db = al[::-1], db[::-1]
    m['alog_t'] = np.ascontiguousarray(np.tile(al.reshape(1, 16), (NT + 2, 1)))
    m['dtb_t'] = np.ascontiguousarray(np.tile(db.reshape(1, 16), (NT + 2, 1)))
    pos = np.arange(L)[::-1] if rev else np.arange(L)
    own = pos[2048:]
    ang = (2.0 * np.pi / L) * ((pos[:, None].astype(np.int64) * own[None, :].astype(np.int64)) % L)
    tab = np.stack([np.cos(ang), np.sin(ang)], 0).astype(np.float32)
    tab = tab.reshape(2, 4, 8, 128, 4, 512).transpose(4, 1, 3, 0, 2, 5)
    m['dft_tab'] = bf(tab)
    cang = (2.0 * np.pi / 128) * ((np.arange(128)[:, None] * np.arange(128)[None, :]) % 128)
    m['cdft'] = bf(np.stack([np.cos(cang), -np.sin(cang)], 1))
    m['gng_t'] = np.ascontiguousarray(np.tile(np.asarray(inputs['gdn_norm_g'][0], np.float32), 8))
    m['w_fo'] = np.ascontiguousarray(inputs['w_fourier_out'][0], np.float32)
    m['w_go'] = np.ascontiguousarray(inputs['w_gdn_out'][0], np.float32)
    m['w_mo'] = np.ascontiguousarray(inputs['w_merge_out'][0], np.float32)
    m['w_router'] = np.ascontiguousarray(inputs['w_router'][0], np.float32)
    m['b_router'] = np.ascontiguousarray(inputs['b_router'][0], np.float32)
    for nm_, key in [('w_gate', 'w_gate'), ('w_up', 'w_up'), ('w_down', 'w_down'), ('b_gate', 'b_gate'), ('b_up', 'b_up'), ('b_down', 'b_down')]:
        m[nm_] = np.ascontiguousarray(inputs[key][0], np.float32)
    m['fng'] = np.ascontiguousarray(inputs['final_norm_g'], np.float32)
    for k, v in consts.items():
        m['c_' + k] = v
    return m


def build(debug=None):
    nc = bass.Bass("TRN2", target_bir_lowering=False)
    p = Builder(nc, debug)
    p.phase0()
    if debug == 'AD1':
        p.phaseA()
        p.debug = 'D1'
        p.phaseD()
        p.S.barrier(); p.S.emit(); p.st.close()
        return nc, p
    if debug == 'Gs':
        p.QKVs = p.inp("QKVs_in", [L + CTXL, QKV], BF16)
        p.ba = p.sb("ba", [128, NT + 2, 32], F32)
        p.dma(SP, p.ba[:], p.inp("ba_in", [128, NT + 2, 32], F32), w=['ba'])
        p.S.barrier()
        p.debug = 'G0'
    elif debug in ('Ds1', 'Ds'):
        p.XF = p.inp("XF_in", [L, 512], BF16)
        p.Od = [p.inp("Of_in", [2048, 1024], F32), p.inp("Ob_in", [2048, 1024], F32)]
        p.inp("w_in", [D, IN_COLS], F32)
        p.debug = 'D1' if debug == 'Ds1' else 'D'
    elif debug != '0':
        p.phaseA()
    if debug not in ('0', 'A', 'Ds', 'Ds1'):
        p.phaseG()
    if debug not in ('0', 'A', 'G', 'G0', 'Gs'):
        p.phaseD()
    if debug in (None, 'E'):
        p.phaseE()
    p.S.barrier()
    p.S.emit()
    p.st.close()
    return nc, p


def run(inputs, debug=None):
    nc, p = build(debug)
    maps = []
    for c in range(8):
        m = core_inputs(inputs, c, p.const_np)
        maps.append({k: m[k] for k in p.din})
    res = run_bass_kernel_spmd(nc, maps, core_ids=list(range(8)))
    return res.results


def kernel(**inputs):
    nc, p = build(None)
    maps = []
    for c in range(8):
        m = core_inputs(inputs, c, p.const_np)
        maps.append({k: m[k] for k in p.din})
    res = run_bass_kernel_spmd(nc, maps, core_ids=list(range(8)))
    out = np.zeros((4, L, D), np.float32)
    for c in range(8):
        b, hf = c // 2, c % 2
        y = np.asarray(res.results[c]['y'], np.float32)
        if hf == 1:
            out[b, 2048:] = y
        else:
            out[b, :2048] = y[::-1]
    return out
```

```python
import contextlib
import numpy as np
import ml_dtypes
import concourse.bass as bass
import concourse.mybir as mybir
from concourse.bass_utils import run_bass_kernel_spmd

F32 = mybir.dt.float32
BF16 = mybir.dt.bfloat16
I32 = mybir.dt.int32
U32 = mybir.dt.uint32
AF = mybir.ActivationFunctionType
ALU = mybir.AluOpType
AX = mybir.AxisListType

PE, ACT, DVE, POOL, SP = 'pe', 'act', 'dve', 'pool', 'sp'
ENGS = [PE, ACT, DVE, POOL, SP]
NDS = 8

D = 1024
L = 4096
NT = 32
OWN0 = 16
CTXL = 256
QKV = 2048
BETA_OFF = 2048
A_OFF = 2064
GDN_IN = 2080
Z_OFF = 2080
F_OFF = 3104
GA_OFF = 3616
GB_OFF = 4640
IN_COLS = 5664
NE = 32
EPS = 1e-6
MOE_CAPS = [8] + [6] * 3 + [5] * 4 + [4] * 8 + [3] * 16


class Sched:
    def __init__(s, nc):
        s.nc = nc
        s.ops = {e: [] for e in ENGS}
        s.res = {}
        s.ndma = {e: 0 for e in ENGS}
        s.epoch = 0

    def op(s, eng, fn, reads=(), writes=(), dma=False):
        idx = len(s.ops[eng])
        me = (eng, idx)
        deps = []
        for k in reads:
            r = s.res.get(k)
            if r and r[0] is not None:
                deps.append(r[0])
        for k in writes:
            r = s.res.get(k)
            if r:
                if r[0] is not None:
                    deps.append(r[0])
                deps.extend(r[1])
        waits = []
        best = {}
        for p in deps:
            pe, pi = p
            if s.ops[pe][pi]['dma']:
                waits.append(p)
                continue
            if pe == eng and eng == PE:
                continue
            if pi > best.get(pe, -1):
                best[pe] = pi
        for pe, pi in best.items():
            waits.append((pe, pi))
        o = dict(fn=fn, waits=waits, sig=False, dma=dma, dsem=None, dval=None, dn=0, ep=s.epoch)
        if dma:
            n = s.ndma[eng]
            s.ndma[eng] += 1
            o['dsem'] = n % NDS
            o['dval'] = 16 * (n // NDS + 1)
            o['dn'] = n
        s.ops[eng].append(o)
        for k in reads:
            s.res.setdefault(k, [None, []])[1].append(me)
        for k in writes:
            s.res[k] = [me, []]
        return me

    def barrier(s):
        for e in ENGS:
            pend = []
            for e2 in ENGS:
                n = len(s.ops[e2])
                if e2 != e:
                    for i in range(n - 1, -1, -1):
                        if s.ops[e2][i]['fn'] is not None and not s.ops[e2][i]['dma']:
                            pend.append((e2, i))
                            break
                cnt = 0
                for i in range(n - 1, -1, -1):
                    if s.ops[e2][i]['dma']:
                        pend.append((e2, i))
                        cnt += 1
                        if cnt >= NDS:
                            break
            s.ops[e].append(dict(fn=None, waits=pend, sig=False, dma=False, dsem=None, dval=None, dn=0, ep=s.epoch))
        s.res = {}
        s.epoch += 1

    def emit(s):
        nc = s.nc
        for e in ENGS:
            for o in s.ops[e]:
                for (pe, pi) in o['waits']:
                    po = s.ops[pe][pi]
                    if not po['dma']:
                        if po['fn'] is None:
                            raise RuntimeError("wait on barrier pseudo-op")
                        po['sig'] = True
        sigcount = {}
        used = set()
        for e in ENGS:
            c = {}
            for i, o in enumerate(s.ops[e]):
                if o['sig']:
                    c[o['ep']] = c.get(o['ep'], 0) + 1
                    used.add((e, o['ep']))
                sigcount[(e, i)] = c.get(o['ep'], 0)
        s.maxsig = max([0] + [sigcount[k] for k in sigcount])
        with contextlib.ExitStack() as st:
            esem = {k: st.enter_context(nc.semaphore("es_%s_%d" % k)) for k in sorted(used)}
            dsem = {e: [st.enter_context(nc.semaphore("ds_%s_%d" % (e, i))) for i in range(NDS)] for e in ENGS}
            block = st.enter_context(nc.Block())

            def run(e, eng):
                known = {}
                knownd = {}
                for o in s.ops[e]:
                    need = {}
                    needd = {}
                    if o['dma'] and o['dn'] >= NDS:
                        needd[(e, o['dsem'])] = o['dval'] - 16
                    for (pe, pi) in o['waits']:
                        po = s.ops[pe][pi]
                        if po['dma']:
                            k = (pe, po['dsem'])
                            needd[k] = max(needd.get(k, 0), po['dval'])
                        else:
                            k = (pe, po['ep'])
                            need[k] = max(need.get(k, 0), sigcount[(pe, pi)])
                    for k, v in need.items():
                        if known.get(k, 0) < v:
                            eng.wait_ge(esem[k], v)
                            known[k] = v
                    for k, v in needd.items():
                        if knownd.get(k, 0) < v:
                            eng.wait_ge(dsem[k[0]][k[1]], v)
                            knownd[k] = v
                    if o['fn'] is None:
                        continue
                    inst = o['fn'](eng)
                    if o['dma']:
                        inst.then_inc(dsem[e][o['dsem']], 16)
                    elif o['sig']:
                        inst.then_inc(esem[(e, o['ep'])], 1)

            block.tensor(lambda eng: run(PE, eng))
            block.scalar(lambda eng: run(ACT, eng))
            block.vector(lambda eng: run(DVE, eng))
            block.gpsimd(lambda eng: run(POOL, eng))
            block.sync(lambda eng: run(SP, eng))


class Prog:
    def __init__(p, nc):
        p.nc = nc
        p.S = Sched(nc)
        p.st = contextlib.ExitStack()
        p.din = {}
        p.dout = {}

    def inp(p, name, shape, dt):
        t = p.nc.dram_tensor(name, list(shape), dt, kind="ExternalInput").ap()
        p.din[name] = t
        return t

    def out(p, name, shape, dt):
        t = p.nc.dram_tensor(name, list(shape), dt, kind="ExternalOutput").ap()
        p.dout[name] = t
        return t

    def scratch(p, name, shape, dt):
        return p.nc.dram_tensor(name, list(shape), dt, kind="Internal").ap()

    def sb(p, name, shape, dt, st=None):
        return (st or p.st).enter_context(p.nc.sbuf_tensor(name, list(shape), dt))

    def psum(p, name, shape, dt):
        return p.st.enter_context(p.nc.psum_tensor(name, list(shape), dt))

    def dma(p, q, out, in_, r=(), w=(), **kw):
        return p.S.op(q, lambda e: e.dma_start(out=out, in_=in_, **kw), r, w, dma=True)

    def mm(p, out, lhsT, rhs, start, stop, r=(), w=()):
        return p.S.op(PE, lambda e: e.matmul(out, lhsT, rhs, start=start, stop=stop), r, w)

    def tr(p, out, in_, ident, r=(), w=()):
        return p.S.op(PE, lambda e: e.transpose(out, in_, ident), r, w)

    def act(p, out, in_, func, r=(), w=(), eng=ACT, **kw):
        return p.S.op(eng, lambda e: e.activation(out=out, in_=in_, func=func, **kw), r, w)

    def ts(p, eng, out, in0, s1, s2, op0, op1=None, r=(), w=(), **kw):
        if op1 is None:
            return p.S.op(eng, lambda e: e.tensor_scalar(out, in0, s1, s2, op0, **kw), r, w)
        return p.S.op(eng, lambda e: e.tensor_scalar(out, in0, s1, s2, op0, op1, **kw), r, w)

    def tt(p, eng, out, in0, in1, op, r=(), w=()):
        return p.S.op(eng, lambda e: e.tensor_tensor(out, in0, in1, op), r, w)

    def stt(p, eng, out, in0, scalar, in1, op0, op1, r=(), w=()):
        return p.S.op(eng, lambda e: e.scalar_tensor_tensor(out, in0, scalar, in1, op0, op1), r, w)

    def cp(p, eng, out, in_, r=(), w=()):
        if eng == ACT:
            return p.S.op(eng, lambda e: e.copy(out, in_), r, w)
        return p.S.op(eng, lambda e: e.tensor_copy(out, in_), r, w)

    def generic(p, eng, fn, r=(), w=()):
        return p.S.op(eng, fn, r, w)


def bf(a):
    return np.ascontiguousarray(a).astype(ml_dtypes.bfloat16)


def fm_layout(v, nchunk):
    return np.ascontiguousarray(np.asarray(v, np.float32).reshape(nchunk, 128).T)


def host_consts():
    c = {}
    c['ident_bf'] = bf(np.eye(128, dtype=np.float32))
    c['ident_f'] = np.eye(128, dtype=np.float32)
    c['ones_f'] = np.ones((128, 128), np.float32)
    t = np.arange(128)
    c['u_incl'] = (t[:, None] <= t[None, :]).astype(np.float32)
    c['u_strict'] = (t[:, None] < t[None, :]).astype(np.float32)
    c['l_incl'] = np.ascontiguousarray(c['u_incl'].T)
    c['l_strict'] = np.ascontiguousarray(c['u_strict'].T)
    rep4 = lambda m: np.ascontiguousarray(np.tile(m, (1, 4)))
    c['ms4_f'] = rep4(c['u_strict']); c['mi4_f'] = rep4(c['u_incl'])
    c['ms4_b'] = rep4(c['l_strict']); c['mi4_b'] = rep4(c['l_incl'])
    c['i4'] = bf(rep4(np.eye(128, dtype=np.float32)))
    lv = np.zeros((2, 7, 128, 512), np.float32)
    for li in range(7):
        sz = 1 << li
        j = t[:, None]; i = t[None, :]
        m = ((j // (2 * sz)) == (i // (2 * sz))) & ((j % (2 * sz)) < sz) & ((i % (2 * sz)) >= sz)
        lv[0, li] = rep4(m.astype(np.float32))
        lv[1, li] = rep4(m.T.astype(np.float32))
    c['lvl'] = bf(lv.transpose(2, 0, 1, 3))
    c['ones_bf'] = bf(np.ones((128, 128), np.float32))
    c['ustrict_bf'] = bf(c['u_strict'])
    c['iota32'] = np.ascontiguousarray(np.tile(np.arange(32, dtype=np.float32)[None, :], (128, 1)))
    c['ecol'] = np.ascontiguousarray(np.arange(128, dtype=np.float32)[:, None])
    c['blk128'] = np.zeros((128, 1), np.float32)
    tb = np.concatenate([[0], np.cumsum(MOE_CAPS)[:-1]]).astype(np.float32) * 128.0
    c['basetab'] = np.ascontiguousarray(np.tile(tb[None, :], (128, 1)))
    c['captab'] = np.ascontiguousarray(np.tile((np.array(MOE_CAPS, np.float32) * 128.0)[None, :], (128, 1)))
    c['rowoff'] = np.ascontiguousarray((np.arange(8)[None, :] * 128 + np.arange(128)[:, None]).astype(np.float32))
    c['tokid'] = np.ascontiguousarray((np.arange(16)[None, :] * 128 + np.arange(128)[:, None]).astype(np.int32))
    return c


class Builder(Prog):
    def __init__(p, nc, debug=None):
        super().__init__(nc)
        p.debug = debug
        p.const_np = host_consts()
        p.psb = [p.psum("ps%d" % i, [128, 512], F32) for i in range(8)]

    def load_const(p, name, dt):
        a = p.const_np[name]
        d = p.inp("c_" + name, a.shape, dt)
        t = p.sb("k_" + name, a.shape, dt)
        p.dma(SP, t[:], d, w=[('k', name)])
        return t

    def phase0(p):
        nc = p.nc
        p.ident_bf = p.load_const('ident_bf', BF16)
        p.ident_f = p.load_const('ident_f', F32)
        p.ones_f = p.load_const('ones_f', F32)
        cT = p.inp("cT", [128, 8, 2], F32)
        bmod = p.inp("bmod", [128, 48], F32)
        n1g = p.inp("n1g", [128, 8], F32)
        n2g = p.inp("n2g", [128, 8], F32)
        wmod = p.inp("w_mod", [D, 6 * D], F32)
        p.eps_t = p.sb("eps_t", [128, 1], F32)
        p.S.op(DVE, lambda e: e.memset(p.eps_t[:], EPS), [], ['eps_t'])
        p.modsb = p.sb("modsb", [128, 48, 2], F32)
        p.vecs = p.sb("vecs", [128, 8, 8], F32)
        st0 = contextlib.ExitStack()
        scT = p.sb("scT", [128, 8, 2], F32, st0)
        bm = p.sb("bm", [128, 48], F32, st0)
        g1t = p.sb("n1g_sb", [128, 8], F32, st0)
        g2t = p.sb("n2g_sb", [128, 8], F32, st0)
        wb = [p.sb("wmodb%d" % i, [128, 8, 512], F32, st0) for i in range(2)]
        p.dma(SP, scT[:], cT, w=['scT'])
        p.dma(SP, bm[:], bmod, w=['bm'])
        p.dma(SP, g1t[:], n1g, w=['n1g'])
        p.dma(SP, g2t[:], n2g, w=['n2g'])
        p.act(scT[:], scT[:], AF.Silu, r=['scT'], w=['scT'])
        wv = wmod.rearrange("(kc q) n -> q kc n", q=128)
        psM = p.psb[0]
        for blk in range(12):
            b = wb[blk % 2]
            p.dma(SP, b[:], wv[:, :, blk * 512:(blk + 1) * 512], w=[('wmodb', blk % 2)])
            for fc in range(4):
                j = blk * 4 + fc
                for kc in range(8):
                    p.mm(psM[:, 2 * j:2 * j + 2], b[:, kc, fc * 128:(fc + 1) * 128], scT[:, kc, :],
                         kc == 0, kc == 7, r=[('wmodb', blk % 2), 'scT'], w=[('ps', 0)])
        pv = psM[:, 0:96].rearrange("q (j m) -> q j m", m=2)
        for m in range(2):
            p.tt(DVE, p.modsb[:, :, m], pv[:, :, m], bm[:], ALU.add, r=[('ps', 0), 'bm'], w=['modsb'])
        p.stt(DVE, p.vecs[:, 0, :], p.modsb[:, 8:16, 0], 1.0, g1t[:], ALU.add, ALU.mult, r=['modsb', 'n1g'], w=['vecs'])
        p.stt(DVE, p.vecs[:, 1, :], p.modsb[:, 8:16, 1], 1.0, g1t[:], ALU.add, ALU.mult, r=['modsb', 'n1g'], w=['vecs'])
        p.stt(DVE, p.vecs[:, 2, :], p.modsb[:, 32:40, 0], 1.0, g2t[:], ALU.add, ALU.mult, r=['modsb', 'n2g'], w=['vecs'])
        p.S.barrier()
        st0.close()
        if p.debug == '0':
            o = p.out("dbg_mod", [128, 48, 2], F32)
            p.dma(SP, o, p.modsb[:], r=['modsb'])
            o2 = p.out("dbg_vecs", [128, 8, 8], F32)
            p.dma(SP, o2, p.vecs[:], r=['vecs'])

    def gs1(p, c): return p.vecs[:, 0, c:c + 1]
    def sh1(p, c): return p.modsb[:, c, 0:1]
    def cgs1(p, c): return p.vecs[:, 1, c:c + 1]
    def csh1(p, c): return p.modsb[:, c, 1:2]

    def norm_T(p, xsrc, gs, sh, uT_dst, bufs, tag):
        xt, xk = bufs['x']
        p.dma(SP, xt[:], xsrc, w=[xk])
        junk, jk = bufs['junk']
        ss, sk = bufs['ss']
        p.act(junk[:], xt[:], AF.Square, r=[xk], w=[jk, sk], accum_out=ss[:, 0:1])
        p.act(ss[:, 2:3], ss[:, 0:1], AF.Ln, r=[sk], w=[sk], scale=1.0 / D, bias=p.eps_t[:, 0:1])
        p.act(ss[:, 3:4], ss[:, 2:3], AF.Exp, r=[sk], w=[sk], scale=-0.5)
        xn, nk = bufs['xn']
        p.ts(DVE, xn[:], xt[:], ss[:, 3:4], None, ALU.mult, r=[xk, sk], w=[nk])
        pb, pk = bufs['ps']
        pbv = pb[:].bitcast(BF16)
        for c in range(8):
            p.tr(pbv[:, c * 128:(c + 1) * 128], xn[:, c * 128:(c + 1) * 128], p.ident_bf[:], r=[nk, ('k', 'ident_bf')], w=[pk])
        if tag == 'split':
            return
        p.norm_T_b(gs, sh, uT_dst, bufs)

    def norm_T_b(p, gs, sh, uT_dst, bufs):
        pb, pk = bufs['ps']
        pbv = pb[:].bitcast(BF16)
        for c in range(8):
            dst, dk = uT_dst(c)
            if c % 2 == 0:
                p.act(dst, pbv[:, c * 128:(c + 1) * 128], AF.Identity, r=[pk, 'modsb', 'vecs'], w=[dk],
                      scale=gs(c), bias=sh(c))
            else:
                p.ts(DVE, dst, pbv[:, c * 128:(c + 1) * 128], gs(c), sh(c), ALU.mult, ALU.add, r=[pk, 'modsb', 'vecs'], w=[dk])

    def phaseA(p):
        x = p.inp("x_seq", [L, D], F32)
        ctx = p.inp("ctx_seq", [CTXL, D], F32)
        w_in = p.inp("w_in", [D, IN_COLS], F32)
        cw = p.inp("conv_diag", [128, 16, 9, 128], F32)
        p.XF = p.scratch("XF", [L, 512], BF16)
        p.QKVs = p.scratch("QKVs", [L + CTXL, QKV], BF16)
        p.ba = p.sb("ba", [128, NT + 2, 32], F32)
        stA = contextlib.ExitStack()
        wA = p.sb("wA", [128, 8, 2592], BF16, stA)
        wv = w_in.rearrange("(kc q) n -> q kc n", q=128)
        for kc in range(8):
            p.dma(POOL, wA[:, kc, 0:2080], wv[:, kc, 0:2080], w=[('wA', kc)])
            p.dma(POOL, wA[:, kc, 2080:2592], wv[:, kc, F_OFF:F_OFF + 512], w=[('wA', kc)])
        cwts = [p.sb("cwt%d" % i, [128, 2, 9, 128], BF16, stA) for i in range(2)]
        NTT = NT + 2
        uT = p.sb("uT", [128, 8, NTT * 128], BF16, stA)
        stA1 = contextlib.ExitStack()
        NB1 = 4
        bufs = [{
            'x': (p.sb("xt%d" % i, [128, D], F32, stA1), ('xt', i)),
            'junk': (p.sb("junk%d" % i, [128, D], BF16, stA1), ('junk', i)),
            'ss': (p.sb("ss%d" % i, [128, 4], F32, stA1), ('ss', i)),
            'xn': (p.sb("xn%d" % i, [128, D], BF16, stA1), ('xn', i)),
            'ps': (p.psb[[0, 1, 6, 7][i]], ('ps', [0, 1, 6, 7][i])),
        } for i in range(NB1)]
        xfb = [p.sb("xfb%d" % i, [128, 512], BF16, stA1) for i in range(2)]
        def a1_args(t):
            if t < NT:
                return x[t * 128:(t + 1) * 128, :], p.gs1, p.sh1
            return ctx[(t - NT) * 128:(t - NT + 1) * 128, :], p.cgs1, p.csh1

        def a1_front(t):
            src, gs, sh = a1_args(t)
            p.norm_T(src, gs, sh, None, bufs[t % NB1], 'split')

        DEPTH = 2
        for t0 in range(DEPTH):
            a1_front(t0)
        for t in range(NTT):
            b = bufs[t % NB1]
            if t + DEPTH < NTT:
                a1_front(t + DEPTH)
            src, gs, sh = a1_args(t)
            p.norm_T_b(gs, sh, lambda c, t=t: (uT[:, c, t * 128:(t + 1) * 128], ('uT', t)), b)
            ps = p.psb[2 + t % 2]
            for kc in range(8):
                p.mm(ps[:, 0:32], uT[:, kc, t * 128:(t + 1) * 128], wA[:, kc, 2048:2080], kc == 0, kc == 7,
                     r=[('wA', kc), ('uT', t)], w=[('ps', 2 + t % 2)])
            p.cp(DVE, p.ba[:, t, :], ps[:, 0:32], r=[('ps', 2 + t % 2)], w=[('ba', t)])
            if t < NT:
                ps = p.psb[4 + t % 2]
                for kc in range(8):
                    p.mm(ps[:, :], uT[:, kc, t * 128:(t + 1) * 128], wA[:, kc, 2080:2592], kc == 0, kc == 7,
                         r=[('wA', kc), ('uT', t)], w=[('ps', 4 + t % 2)])
                xb = xfb[t % 2]
                p.cp(DVE, xb[:], ps[:, :], r=[('ps', 4 + t % 2)], w=[('xfb', t % 2)])
                p.dma(POOL, p.XF[t * 128:(t + 1) * 128, :], xb[:], r=[('xfb', t % 2)], w=[('XF', t)])
        p.S.barrier()
        stA1.close()
        LD = 72
        C0 = LD + L + 64
        CBN = C0 + 258
        cb = [p.sb("cb%d" % i, [128, 3, CBN], BF16, stA) for i in range(2)]
        for i in range(2):
            p.S.op(DVE, (lambda e, t_=cb[i]: e.memset(t_[:], 0.0)), [], [('cb', i)])
        qt = [p.sb("qt%d" % i, [128, 512], BF16, stA) for i in range(2)]
        for ch in range(16):
            cbuf = cb[ch % 2]
            ck = ('cb', ch % 2)
            if ch % 2 == 0:
                cwt = cwts[(ch // 2) % 2]
                cwk = ('cwt', (ch // 2) % 2)
                p.dma(POOL, cwt[:], cw[:, ch:ch + 2, :, :], w=[cwk])
            for st_ in range(9):
                ntok = 512 if st_ < 8 else 256
                t0 = st_ * 512
                ps = p.psb[st_ % 4]
                pk = ('ps', st_ % 4)
                for kc in range(8):
                    p.mm(ps[:, 0:ntok], wA[:, kc, ch * 128:(ch + 1) * 128], uT[:, kc, t0:t0 + ntok], kc == 0, kc == 7,
                         r=[('wA', kc)] + [('uT', t0 // 128 + q) for q in range(ntok // 128)], w=[pk])
                if st_ < 8:
                    o0 = LD + t0
                    p.cp(ACT, cbuf[:, 0, o0:o0 + 512], ps[:, 0:512], r=[pk], w=[ck])
                    sv = ps[:, 0:512].rearrange("q (r c) -> q r c", c=64)
                    p.cp(DVE, cbuf[:, 1, o0:o0 + 512].rearrange("q (r c) -> q r c", c=64)[:, :, 0:63], sv[:, :, 0:63], r=[pk], w=[ck])
                    p.cp(DVE, cbuf[:, 2, o0:o0 + 512].rearrange("q (r c) -> q r c", c=64)[:, :, 1:64], sv[:, :, 1:64], r=[pk], w=[ck])
                else:
                    p.cp(ACT, cbuf[:, 0, C0 + 1:C0 + 257], ps[:, 0:256], r=[pk], w=[ck])
            for t in range(NTT):
                if t % 4 == 0:
                    ps = p.psb[4 + (t // 4) % 4]
                    pk = ('ps', 4 + (t // 4) % 4)
                if t < NT:
                    taps = [(dy, dx) for dy in (-1, 0, 1) for dx in (-1, 0, 1)]
                else:
                    taps = [(0, dx) for dx in (-1, 0, 1)]
                for ti, (dy, dx) in enumerate(taps):
                    if t < NT:
                        base = LD + 128 * t + 64 * dy + dx
                        lhsT = cbuf[:, {-1: 1, 0: 0, 1: 2}[dx], base:base + 128]
                    else:
                        base = C0 + 1 + (t - NT) * 128 + dx
                        lhsT = cbuf[:, 0, base:base + 128]
                    tap = (dy + 1) * 3 + (dx + 1)
                    p.mm(ps[:, (t % 4) * 128:(t % 4 + 1) * 128], lhsT, cwt[:, ch % 2, tap, :], ti == 0, ti == len(taps) - 1,
                         r=[ck, cwk], w=[pk])
                if t % 4 == 3 or t == NTT - 1:
                    nt_ = t % 4 + 1
                    tb = t - t % 4
                    qi_ = (tb // 4) % 2
                    q_ = qt[qi_]
                    p.act(q_[:, 0:nt_ * 128], ps[:, 0:nt_ * 128], AF.Silu, r=[pk], w=[('qt', qi_)])
                    row = tb * 128 if tb < NT else L
                    dstv = p.QKVs[row:row + nt_ * 128, ch * 128:(ch + 1) * 128].rearrange("(a q) c -> q a c", q=128)
                    p.dma(SP, dstv, q_[:, 0:nt_ * 128].rearrange("q (a c) -> q a c", c=128), r=[('qt', qi_)], w=[('QKVs', tb, ch)])
        p.S.barrier()
        stA.close()
        if p.debug == 'A':
            o = p.out("dbg_qkv", [L + CTXL, QKV], BF16)
            p.dma(SP, o, p.QKVs, r=[])
            o = p.out("dbg_ba", [128, NT + 2, 32], F32)
            p.dma(SP, o, p.ba[:], r=[])
            o = p.out("dbg_xf", [L, 512], BF16)
            p.dma(SP, o, p.XF, r=[])


    def phaseG(p):
        DKS = 128 ** -0.5
        alog = p.inp("alog_t", [NT + 2, 16], F32)
        dtb = p.inp("dtb_t", [NT + 2, 16], F32)
        NTT = NT + 2
        p.Od = [p.scratch("O_f", [16 * 128, 1024], F32), p.scratch("O_b", [16 * 128, 1024], F32)]
        stG = contextlib.ExitStack()
        K = {}
        for nm, dt_ in [('u_incl', F32), ('l_incl', F32), ('u_strict', F32), ('l_strict', F32),
                        ('ms4_f', F32), ('mi4_f', F32), ('ms4_b', F32), ('mi4_b', F32), ('i4', BF16), ('lvl', BF16)]:
            a = p.const_np[nm]
            d = p.inp("c_" + nm, a.shape, dt_)
            K[nm] = p.sb("k_" + nm, a.shape, dt_, stG)
            p.dma(SP, K[nm][:], d, w=[('k', nm)])
        kr = lambda *n: [('k', x) for x in n]
        p.S.barrier()
        lgs = p.sb("lgs", [128, NTT, 16], F32, stG)
        bts = p.sb("bts", [128, NTT, 16], F32, stG)
        nbt = p.sb("nbts", [128, NTT, 16], F32, stG)
        prm = p.sb("prm", [128, 2, NTT, 16], F32, stG)
        p.dma(SP, prm[:, 0], alog.partition_broadcast(128), w=['prm'])
        p.dma(SP, prm[:, 1], dtb.partition_broadcast(128), w=['prm'])
        p.act(prm[:, 0], prm[:, 0], AF.Exp, r=['prm'], w=['prm'])
        p.tt(DVE, lgs[:], p.ba[:, :, 16:32], prm[:, 1], ALU.add, r=['prm'], w=['lgs'])
        p.act(lgs[:], lgs[:], AF.Exp, r=['lgs'], w=['lgs'])
        p.ts(DVE, lgs[:], lgs[:], 1.0, None, ALU.add, r=['lgs'], w=['lgs'])
        p.act(lgs[:], lgs[:], AF.Ln, r=['lgs'], w=['lgs'])
        p.stt(DVE, lgs[:], lgs[:], -1.0, prm[:, 0], ALU.mult, ALU.mult, r=['lgs', 'prm'], w=['lgs'])
        p.act(bts[:], p.ba[:, :, 0:16], AF.Exp, r=[], w=['bts'], scale=-1.0)
        p.ts(DVE, bts[:], bts[:], 1.0, None, ALU.add, r=['bts'], w=['bts'])
        p.S.op(DVE, lambda e: e.reciprocal(bts[:], bts[:]), ['bts'], ['bts'])
        p.ts(DVE, nbt[:], bts[:], -1.0, None, ALU.mult, r=['bts'], w=['nbt'])
        Sf = p.sb("S_f32", [128, 16, 128], F32, stG)
        Sb = [p.sb("S_bf%d" % i, [128, 16, 128], BF16, stG) for i in range(2)]
        p.S.op(DVE, lambda e: e.memset(Sf[:], 0.0), [], [('Sf', h) for h in range(16)])
        p.S.op(DVE, lambda e: e.memset(Sb[0][:], 0.0), [], [('Sb', 0, h) for h in range(16)])
        sbi = [0] * 16
        qkvb = [p.sb("qkvb%d" % i, [128, QKV], BF16, stG) for i in range(2)]
        sqs = [p.sb("sq%d" % i, [128, 1024], F32, stG) for i in range(2)]
        nrms = [p.sb("nrm%d" % i, [128, 4, 8], F32, stG) for i in range(2)]
        qkn = [p.sb("qkn%d" % i, [128, 8, 128], BF16, stG) for i in range(2)]
        kT = [p.sb("kT%d" % i, [128, 4, 128], BF16, stG) for i in range(2)]
        qT = [p.sb("qT%d" % i, [128, 4, 128], BF16, stG) for i in range(2)]
        gsc = [p.sb("gsc%d" % i, [128, 6, 16], F32, stG) for i in range(2)]
        CT = []
        for ci in range(2):
            c_ = dict(i=ci, ba=3 * ci)
            for nm_, shp, dt_ in [('LW', [128, 4, 128], F32), ('Eb', [128, 512], F32), ('Ems', [128, 512], F32), ('Emi', [128, 512], F32),
                                  ('Nb', [128, 512], BF16), ('NTb', [128, 512], BF16), ('NTl', [128, 6, 512], BF16),
                                  ('X0', [128, 512], BF16), ('X1', [128, 512], BF16), ('XT0', [128, 512], BF16), ('XT1', [128, 512], BF16),
                                  ('Yb', [128, 512], BF16), ('tmpb', [128, 512], BF16), ('tmpf', [128, 512], F32), ('qkp', [128, 512], BF16),
                                  ('kdec', [128, 512], BF16), ('rb', [128, 512], BF16), ('ub', [128, 512], BF16), ('o1', [128, 512], F32)]:
                c_[nm_] = p.sb("%s_%d" % (nm_, ci), shp, dt_, stG)
            CT.append(c_)
        ob = [p.sb("ob%d" % i, [128, 1024], F32, stG) for i in range(2)]
        PS = lambda i: p.psb[i]
        PK = lambda i: ('ps', i)
        sched = [(32, 0, False), (33, 0, False), (33, 1, False), (32, 1, False)]
        fw = [(t, 0, t >= OWN0) for t in range(NT)]
        bw = [(t, 1, True) for t in range(NT - 1, OWN0 - 1, -1)]
        while fw or bw:
            if fw:
                sched.append(fw.pop(0))
            if bw and (len(fw) < 2 * len(bw) + 1):
                sched.append(bw.pop(0))
        if p.debug == 'G0':
            sched = sched[:4]
        loaded = {}
        step = 0
        nload = 0
        real_op = p.S.op
        recs = []

        def rec_into(lst):
            p.S.op = lambda eng, fn, reads=(), writes=(), dma=False: lst.append((eng, fn, list(reads), list(writes), dma))

        for (t, d, need_out) in sched:
            prep_l, stage_ll, tail_l = [], [], []
            recs.append((prep_l, stage_ll, tail_l))
            rec_into(prep_l)
            bi = nload % 2
            nload += 1
            qb = qkvb[bi]
            sq = sqs[bi]
            nrm = nrms[bi]
            sqk = ('sq', bi)
            nrk = ('nrm', bi)
            row = t * 128 if t < NT else L + (t - NT) * 128
            p.dma(SP, qb[:], p.QKVs[row:row + 128, :], w=[('qkvb', bi)])
            p.tt(DVE, sq[:], qb[:, 0:1024], qb[:, 0:1024], ALU.mult, r=[('qkvb', bi)], w=[sqk])
            p.S.op(DVE, lambda e, nrm=nrm, sq=sq: e.tensor_reduce(nrm[:, 0, :], sq[:].rearrange("q (h c) -> q h c", c=128), AX.X, ALU.add), [sqk], [nrk])
            p.ts(DVE, nrm[:, 1, :], nrm[:, 0, :], EPS, None, ALU.add, r=[nrk], w=[nrk])
            p.act(nrm[:, 2, :], nrm[:, 1, :], AF.Ln, r=[nrk], w=[nrk])
            p.act(nrm[:, 3, :], nrm[:, 2, :], AF.Exp, r=[nrk], w=[nrk], scale=-0.5)
            p.ts(DVE, nrm[:, 3, 0:4], nrm[:, 3, 0:4], DKS, None, ALU.mult, r=[nrk], w=[nrk])
            qn = qkn[bi]
            p.tt(DVE, qn[:], qb[:, 0:1024].rearrange("q (h c) -> q h c", c=128), nrm[:, 3, :].unsqueeze(2).to_broadcast([128, 8, 128]), ALU.mult,
                 r=[('qkvb', bi), nrk], w=[('qkn', bi)])
            kTb, qTb = kT[bi], qT[bi]
            pv = PS(6)[:].bitcast(BF16)
            for h in range(4):
                p.tr(pv[:, h * 128:(h + 1) * 128], qn[:, 4 + h, :], p.ident_bf[:], r=[('qkn', bi)], w=[PK(6)])
            p.cp(ACT, kTb[:].rearrange("q h c -> q (h c)"), pv[:, 0:512], r=[PK(6)], w=[('kT', bi)])
            if need_out:
                pv5 = PS(6)[:].bitcast(BF16)[:, 512:1024]
                for h in range(4):
                    p.tr(pv5[:, h * 128:(h + 1) * 128], qn[:, h, :], p.ident_bf[:], r=[('qkn', bi)], w=[PK(6)])
                p.cp(ACT, qTb[:].rearrange("q h c -> q (h c)"), pv5[:, 0:512], r=[PK(6)], w=[('qT', bi)])
            g = gsc[bi]
            gk = ('gsc', bi)
            lg = lgs[:, t, :]
            ps6 = PS(7)
            p.mm(ps6[:, 0:8], K['u_incl'][:], lgs[:, t, 0:8], True, True, r=kr('u_incl') + ['lgs'], w=[PK(7)])
            p.mm(ps6[:, 8:16], K['l_incl'][:], lgs[:, t, 8:16], True, True, r=kr('l_incl') + ['lgs'], w=[PK(7)])
            p.mm(ps6[:, 16:32], p.ones_f[:], lgs[:, t, :], True, True, r=['lgs'], w=[PK(7)])
            p.cp(DVE, g[:, 0:2, :].rearrange("q a c -> q (a c)"), ps6[:, 0:32], r=[PK(7)], w=[gk])
            p.act(g[:, 2, :], g[:, 0, :], AF.Exp, r=[gk], w=[gk])
            p.ts(DVE, g[:, 3, :], g[:, 2, :], -1.0, None, ALU.mult, r=[gk], w=[gk])
            p.tt(DVE, g[:, 4, :], g[:, 1, :], g[:, 0, :], ALU.subtract, r=[gk], w=[gk])
            p.act(g[:, 4, :], g[:, 4, :], AF.Exp, r=[gk], w=[gk])
            p.act(g[:, 5, :], g[:, 1, :], AF.Exp, r=[gk], w=[gk])
            ms_lhs = K['l_strict'] if d == 0 else K['u_strict']
            mi_rhs = K['u_incl'] if d == 0 else K['l_incl']
            MS4 = K['ms4_f'] if d == 0 else K['ms4_b']
            MI4 = K['mi4_f'] if d == 0 else K['mi4_b']
            H4 = lambda ap: ap.rearrange("q (h c) -> q h c", c=128)
            bci = lambda ap: ap.unsqueeze(2).to_broadcast([128, 4, 128])
            bcm = lambda ap: ap.unsqueeze(1).to_broadcast([128, 4, 128])
            obuf = ob[step % 2]

            def key(c_, n):
                return (n, c_['i'])

            def st_D(c_, grp):
                hd0 = d * 8 + grp * 4
                p.tt(DVE, c_['LW'][:], bcm(ms_lhs[:]), bci(lgs[:, t, hd0:hd0 + 4]), ALU.mult, r=['lgs'], w=[key(c_, 'LW')])
                for j4 in range(4):
                    p.mm(PS(c_['ba'])[:, j4 * 128:(j4 + 1) * 128], c_['LW'][:, j4, :], mi_rhs[:], True, True, r=[key(c_, 'LW')], w=[PK(c_['ba'])])
                for j4 in range(4):
                    hq = (grp * 4 + j4) // 2
                    p.mm(PS(c_['ba'] + 1)[:, j4 * 128:(j4 + 1) * 128], kTb[:, hq, :], kTb[:, hq, :], True, True, r=[('kT', bi)], w=[PK(c_['ba'] + 1)])
                if need_out:
                    for j4 in range(4):
                        hq = (grp * 4 + j4) // 2
                        p.mm(PS(c_['ba'] + 2)[:, j4 * 128:(j4 + 1) * 128], kTb[:, hq, :], qTb[:, hq, :], True, True,
                             r=[('kT', bi), ('qT', bi)], w=[PK(c_['ba'] + 2)])

            def st_E(c_, grp):
                hd0 = d * 8 + grp * 4
                p.act(c_['Eb'][:], PS(c_['ba'])[:, :], AF.Exp, r=[PK(c_['ba'])], w=[key(c_, 'Eb')])
                p.tt(DVE, c_['Ems'][:], c_['Eb'][:], MS4[:], ALU.mult, r=[key(c_, 'Eb')], w=[key(c_, 'Ems')])
                p.tt(DVE, H4(c_['Ems'][:]), H4(c_['Ems'][:]), bci(nbt[:, t, hd0:hd0 + 4]), ALU.mult, r=[key(c_, 'Ems'), 'nbt'], w=[key(c_, 'Ems')])
                p.tt(DVE, c_['Nb'][:], PS(c_['ba'] + 1)[:, :], c_['Ems'][:], ALU.mult, r=[PK(c_['ba'] + 1), key(c_, 'Ems')], w=[key(c_, 'Nb')])
                if need_out:
                    p.tt(DVE, c_['Emi'][:], c_['Eb'][:], MI4[:], ALU.mult, r=[key(c_, 'Eb')], w=[key(c_, 'Emi')])
                    p.tt(DVE, c_['qkp'][:], PS(c_['ba'] + 2)[:, :], c_['Emi'][:], ALU.mult, r=[PK(c_['ba'] + 2), key(c_, 'Emi')], w=[key(c_, 'qkp')])

            def st_NT(c_, grp):
                pv3 = PS(c_['ba'])[:].bitcast(BF16)
                for j4 in range(4):
                    p.tr(pv3[:, j4 * 128:(j4 + 1) * 128], c_['Nb'][:, j4 * 128:(j4 + 1) * 128], p.ident_bf[:], r=[key(c_, 'Nb')], w=[PK(c_['ba'])])
                p.cp(ACT, c_['NTb'][:], pv3[:, 0:512], r=[PK(c_['ba'])], w=[key(c_, 'NTb')])
                p.tt(DVE, c_['NTl'][:], K['lvl'][:, 1 - d, 1:7, :], c_['NTb'][:].unsqueeze(1).to_broadcast([128, 6, 512]), ALU.mult,
                     r=[key(c_, 'NTb')], w=[key(c_, 'NTl')])
                p.tt(DVE, c_['tmpb'][:], c_['Nb'][:], K['lvl'][:, d, 0, :], ALU.mult, r=[key(c_, 'Nb')], w=[key(c_, 'tmpb')])
                p.tt(DVE, c_['X0'][:], c_['tmpb'][:], K['i4'][:], ALU.add, r=[key(c_, 'tmpb')], w=[key(c_, 'X0')])
                p.tt(DVE, c_['tmpb'][:], c_['NTb'][:], K['lvl'][:, 1 - d, 0, :], ALU.mult, r=[key(c_, 'NTb'), key(c_, 'tmpb')], w=[key(c_, 'tmpb')])
                p.tt(DVE, c_['XT0'][:], c_['tmpb'][:], K['i4'][:], ALU.add, r=[key(c_, 'tmpb')], w=[key(c_, 'XT0')])

            def st_L1(c_, grp, li):
                xs = (li - 1) % 2
                X, XT = c_['X%d' % xs], c_['XT%d' % xs]
                for j4 in range(4):
                    sl = slice(j4 * 128, (j4 + 1) * 128)
                    p.mm(PS(c_['ba'])[:, sl], c_['NTl'][:, li - 1, sl], X[:, sl], True, True, r=[key(c_, 'NTl'), key(c_, 'X%d' % xs)], w=[PK(c_['ba'])])
                p.cp(ACT, c_['Yb'][:], PS(c_['ba'])[:, :], r=[PK(c_['ba'])], w=[key(c_, 'Yb')])

            def st_L2(c_, grp, li):
                xs = (li - 1) % 2
                X, XT = c_['X%d' % xs], c_['XT%d' % xs]
                Xn, XTn = c_['X%d' % (1 - xs)], c_['XT%d' % (1 - xs)]
                for j4 in range(4):
                    sl = slice(j4 * 128, (j4 + 1) * 128)
                    p.mm(PS(c_['ba'] + 1)[:, sl], XT[:, sl], c_['Yb'][:, sl], True, False, r=[key(c_, 'XT%d' % xs), key(c_, 'Yb')], w=[PK(c_['ba'] + 1)])
                    p.mm(PS(c_['ba'] + 1)[:, sl], p.ident_bf[:], X[:, sl], False, True, r=[key(c_, 'X%d' % xs)], w=[PK(c_['ba'] + 1)])
                if li < 6:
                    for j4 in range(4):
                        sl = slice(j4 * 128, (j4 + 1) * 128)
                        p.mm(PS(c_['ba'] + 2)[:, sl], c_['Yb'][:, sl], XT[:, sl], True, False, r=[key(c_, 'XT%d' % xs), key(c_, 'Yb')], w=[PK(c_['ba'] + 2)])
                        p.mm(PS(c_['ba'] + 2)[:, sl], p.ident_bf[:], XT[:, sl], False, True, r=[key(c_, 'XT%d' % xs)], w=[PK(c_['ba'] + 2)])
                p.cp(DVE if li % 2 == 0 else ACT, Xn[:], PS(c_['ba'] + 1)[:, :], r=[PK(c_['ba'] + 1)], w=[key(c_, 'X%d' % (1 - xs))])
                if li < 6:
                    p.cp(ACT if li % 2 == 0 else DVE, XTn[:], PS(c_['ba'] + 2)[:, :], r=[PK(c_['ba'] + 2)], w=[key(c_, 'XT%d' % (1 - xs))])

            def st_S1(c_, grp):
                hd0 = d * 8 + grp * 4
                hq0 = 4 + grp * 2
                p.tt(DVE, c_['kdec'][:].rearrange("q (a b c) -> q a b c", b=2, c=128),
                     qn[:, hq0:hq0 + 2, :].unsqueeze(2).to_broadcast([128, 2, 2, 128]),
                     g[:, 4, hd0:hd0 + 4].rearrange("q (a b) -> q a b", b=2).unsqueeze(3).to_broadcast([128, 2, 2, 128]), ALU.mult,
                     r=[('qkn', bi), gk], w=[key(c_, 'kdec')])
                for j4 in range(4):
                    hd = hd0 + j4
                    hv = grp * 4 + j4
                    sl = slice(j4 * 128, (j4 + 1) * 128)
                    So = Sb[sbi[hd]]
                    p.mm(PS(c_['ba'])[:, sl], kTb[:, hv // 2, :], So[:, hd, :], True, True, r=[('kT', bi), ('Sb', sbi[hd], hd)], w=[PK(c_['ba'])])
                p.tt(DVE, H4(c_['tmpf'][:]), H4(PS(c_['ba'])[:, :]), bci(g[:, 3, hd0:hd0 + 4]), ALU.mult, r=[PK(c_['ba']), gk], w=[key(c_, 'tmpf')])
                v0 = 1024 + grp * 512
                p.tt(DVE, c_['rb'][:], c_['tmpf'][:], qb[:, v0:v0 + 512], ALU.add, r=[key(c_, 'tmpf'), ('qkvb', bi)], w=[key(c_, 'rb')])

            def st_S2(c_, grp):
                hd0 = d * 8 + grp * 4
                X = c_['X0']
                for j4 in range(4):
                    sl = slice(j4 * 128, (j4 + 1) * 128)
                    p.mm(PS(c_['ba'] + 1)[:, sl], X[:, sl], c_['rb'][:, sl], True, True, r=[key(c_, 'X0'), key(c_, 'rb')], w=[PK(c_['ba'] + 1)])
                for j4 in range(4):
                    hd = hd0 + j4
                    sl = slice(j4 * 128, (j4 + 1) * 128)
                    p.act(c_['ub'][:, sl], PS(c_['ba'] + 1)[:, sl], AF.Copy, r=[PK(c_['ba'] + 1), 'bts'], w=[key(c_, 'ub')], scale=bts[:, t, hd:hd + 1])

            def st_S3(c_, grp):
                hd0 = d * 8 + grp * 4
                for j4 in range(4):
                    sl = slice(j4 * 128, (j4 + 1) * 128)
                    p.mm(PS(c_['ba'])[:, sl], c_['kdec'][:, sl], c_['ub'][:, sl], True, True, r=[key(c_, 'kdec'), key(c_, 'ub')], w=[PK(c_['ba'])])
                if need_out:
                    for j4 in range(4):
                        hd = hd0 + j4
                        hv = grp * 4 + j4
                        sl = slice(j4 * 128, (j4 + 1) * 128)
                        So = Sb[sbi[hd]]
                        p.mm(PS(c_['ba'] + 1)[:, sl], qTb[:, hv // 2, :], So[:, hd, :], True, True, r=[('qT', bi), ('Sb', sbi[hd], hd)], w=[PK(c_['ba'] + 1)])
                    for j4 in range(4):
                        hd = hd0 + j4
                        sl = slice(j4 * 128, (j4 + 1) * 128)
                        p.act(c_['o1'][:, sl], PS(c_['ba'] + 1)[:, sl], AF.Copy, r=[PK(c_['ba'] + 1), gk], w=[key(c_, 'o1')], scale=g[:, 2, hd:hd + 1])
                    for j4 in range(4):
                        sl = slice(j4 * 128, (j4 + 1) * 128)
                        p.mm(PS(c_['ba'] + 2)[:, sl], c_['qkp'][:, sl], c_['ub'][:, sl], True, True, r=[key(c_, 'qkp'), key(c_, 'ub')], w=[PK(c_['ba'] + 2)])
                    p.tt(DVE, obuf[:, grp * 512:(grp + 1) * 512], PS(c_['ba'] + 2)[:, :], c_['o1'][:], ALU.add, r=[PK(c_['ba'] + 2), key(c_, 'o1')],
                         w=[('ob', step % 2, grp)])
                hk = [('Sf', hd0 + j) for j in range(4)]
                p.tt(DVE, Sf[:, hd0:hd0 + 4, :], Sf[:, hd0:hd0 + 4, :], bci(g[:, 5, hd0:hd0 + 4]), ALU.mult, r=hk + [gk], w=hk)
                p.tt(DVE, Sf[:, hd0:hd0 + 4, :], Sf[:, hd0:hd0 + 4, :], H4(PS(c_['ba'])[:, :]), ALU.add, r=hk + [PK(c_['ba'])], w=hk)
                nb_ = 1 - sbi[hd0]
                p.cp(ACT, Sb[nb_][:, hd0:hd0 + 4, :], Sf[:, hd0:hd0 + 4, :], r=hk, w=[('Sb', nb_, hd0 + j) for j in range(4)])
                for j in range(4):
                    sbi[hd0 + j] = nb_

            stages = [st_D, st_E, st_NT]
            for li in range(1, 7):
                stages.append(lambda c_, grp, li=li: st_L1(c_, grp, li))
                stages.append(lambda c_, grp, li=li: st_L2(c_, grp, li))
            stages += [st_S1, st_S2, st_S3]
            for stg in stages:
                sl_ = []
                stage_ll.append(sl_)
                rec_into(sl_)
                for grp in range(2):
                    stg(CT[grp], grp)
            rec_into(tail_l)
            if need_out:
                p.dma(SP, p.Od[d][(t - OWN0) * 128:(t - OWN0 + 1) * 128, :], obuf[:], r=[('ob', step % 2, 0), ('ob', step % 2, 1)],
                      w=[('Od', d, t)])
            step += 1
            if p.debug in ('G0', 'G') and (t, d) == (32, 1):
                o = p.out("dbg_S", [128, 16, 128], F32)
                p.dma(SP, o, Sf[:], r=[('Sf', h) for h in range(16)])
        p.S.op = real_op
        for o_ in recs[0][0]:
            real_op(*o_)
        for i_, (prep_l, stage_ll, tail_l) in enumerate(recs):
            nxt = list(recs[i_ + 1][0]) if i_ + 1 < len(recs) else []
            per = -(-len(nxt) // max(1, len(stage_ll) - 2)) if nxt else 0
            for sl_ in stage_ll:
                for o_ in sl_:
                    real_op(*o_)
                for _ in range(per):
                    if nxt:
                        real_op(*nxt.pop(0))
            for o_ in nxt:
                real_op(*o_)
            for o_ in tail_l:
                real_op(*o_)
        p.S.barrier()
        stG.close()
        if p.debug == 'G':
            for d in range(2):
                o = p.out("dbg_O%d" % d, [16 * 128, 1024], F32)
                p.dma(SP, o, p.Od[d], r=[])


    def bcast_rows(p, dst, vec, key):
        dg = p.bc_dg
        for c in range(8):
            p.ts(DVE, dg[:, c * 128:(c + 1) * 128], p.ident_f[:], vec(c), None, ALU.mult, r=['modsb', 'vecs'], w=['bc_dg'])
        for hf_ in range(2):
            p.mm(p.psb[7][:, :], p.ones_f[:], dg[:, hf_ * 512:(hf_ + 1) * 512], True, True, r=['bc_dg'], w=[('ps', 7)])
            p.cp(ACT, dst[:, hf_ * 512:(hf_ + 1) * 512], p.psb[7][:, :], r=[('ps', 7)], w=[key])

    def phaseD(p):
        x = p.inp("x_seq", [L, D], F32) if "x_seq" not in p.din else p.din["x_seq"]
        w_in = p.din["w_in"]
        tabs = p.inp("dft_tab", [4, 4, 128, 2, 8, 512], BF16)
        cdft = p.inp("cdft", [128, 2, 128], BF16)
        gng = p.inp("gng_t", [1024], F32)
        wfo = p.inp("w_fo", [512, D], F32)
        wgo = p.inp("w_go", [D, D], F32)
        wmo = p.inp("w_mo", [D, D], F32)
        wr = p.inp("w_router", [D, NE], F32)
        br = p.inp("b_router", [NE], F32)
        p.X1 = p.scratch("X1", [2048, D], F32)
        p.H = p.scratch("H", [2049, D], BF16)
        p.logits = p.sb("logits", [128, 16, NE], F32)
        p.g2_b = p.sb("g2_b", [128, 1024], F32)
        stD = contextlib.ExitStack()
        p.bc_dg = p.sb("bc_dg", [128, 1024], F32, stD)
        p.bcast_rows(p.g2_b, lambda c: p.modsb[:, 40 + c, 0:1], 'g2_b')
        fmT = p.sb("fmT", [128, 4, 2048], BF16, stD)
        wv = w_in.rearrange("(kc q) n -> q kc n", q=128)
        Wzg = p.sb("Wzg", [128, 8, 3072], BF16, stD)
        for kc in range(8):
            p.dma(POOL, Wzg[:, kc, 0:1024], wv[:, kc, Z_OFF:Z_OFF + 1024], w=['Wzg'])
            p.dma(POOL, Wzg[:, kc, 1024:3072], wv[:, kc, GA_OFF:GA_OFF + 2048], w=['Wzg'])
        Wfo = p.sb("Wfo", [128, 4, D], BF16, stD)
        p.dma(POOL, Wfo[:], wfo.rearrange("(kc q) n -> q kc n", q=128), w=['Wfo'])
        Wgo = p.sb("Wgo", [128, 8, D], BF16, stD)
        p.dma(POOL, Wgo[:], wgo.rearrange("(kc q) n -> q kc n", q=128), w=['Wgo'])
        Wmo = p.sb("Wmo", [128, 8, D], BF16, stD)
        p.dma(POOL, Wmo[:], wmo.rearrange("(kc q) n -> q kc n", q=128), w=['Wmo'])
        Wr = p.sb("Wr", [128, 8, NE], F32, stD)
        p.dma(SP, Wr[:], wr.rearrange("(kc q) n -> q kc n", q=128), w=['Wr'])
        brb = p.sb("brb", [128, NE], F32, stD)
        p.dma(SP, brb[:], br.partition_broadcast(128), w=['brb'])
        gnb = p.sb("gnb", [128, 1024], F32, stD)
        p.dma(SP, gnb[:], gng.partition_broadcast(128), w=['gnb'])
        st1 = contextlib.ExitStack()
        Xs = p.sb("Xs", [128, 32, 512], BF16, st1)
        for lc in range(32):
            p.dma(SP, Xs[:, lc, :], p.XF[lc * 128:(lc + 1) * 128, :], w=[('Xs', lc)])
        tb = [p.sb("tb%d" % i, [128, 2, 8, 512], BF16, st1) for i in range(2)]
        cd = p.sb("cd", [128, 2, 128], BF16, st1)
        p.dma(SP, cd[:], cdft, w=['cd'])
        AB = p.sb("AB", [128, 8, 512], BF16, st1)
        SC = float(1.0 / np.sqrt(4096.0 * 128.0))
        nl = 0
        for kt in range(4):
            for q4 in range(4):
                tbuf = tb[nl % 2]
                tk = ('tb', nl % 2)
                nl += 1
                p.dma(SP, tbuf[:], tabs[kt, q4], w=[tk])
                for lc in range(8):
                    la = q4 * 8 + lc
                    for g_ in range(4):
                        for ab in range(2):
                            p.mm(p.psb[g_ * 2 + ab][:, :], Xs[:, la, g_ * 128:(g_ + 1) * 128], tbuf[:, ab, lc, :], la == 0, la == 31,
                                 r=[('Xs', la), tk], w=[('ps', g_ * 2 + ab)])
            for i8 in range(8):
                p.cp(ACT if i8 % 2 == 0 else DVE, AB[:, i8, :], p.psb[i8][:, :], r=[('ps', i8)], w=[('AB', i8)])
            for g_ in range(4):
                p.mm(p.psb[g_][:, :], cd[:, 0, :], AB[:, g_ * 2, :], True, False, r=['cd', ('AB', g_ * 2)], w=[('ps', g_)])
                p.mm(p.psb[g_][:, :], cd[:, 1, :], AB[:, g_ * 2 + 1, :], False, True, r=['cd', ('AB', g_ * 2 + 1)], w=[('ps', g_)])
                p.act(fmT[:, g_, kt * 512:(kt + 1) * 512], p.psb[g_][:, :], AF.Copy, r=[('ps', g_)], w=[('fmT', g_, kt)], scale=SC)
        p.S.barrier()
        st1.close()
        if p.debug == 'D1':
            o = p.out("dbg_fmT", [128, 4, 2048], BF16)
            p.dma(SP, o, fmT[:], r=[])
            p.S.barrier(); stD.close()
            return
        st2 = stD
        g1_b = p.sb("g1_b", [128, 1024], F32, st2)
        gs2_b = p.sb("gs2_b", [128, 1024], F32, st2)
        sh2_b = p.sb("sh2_b", [128, 1024], F32, st2)
        p.bcast_rows(g1_b, lambda c: p.modsb[:, 16 + c, 0:1], 'g1_b')
        p.bcast_rows(gs2_b, lambda c: p.vecs[:, 2, c:c + 1], 'gs2_b')
        p.bcast_rows(sh2_b, lambda c: p.modsb[:, 24 + c, 0:1], 'sh2_b')
        zrow = p.sb("zrow", [1, D], BF16, st2)
        p.S.op(DVE, lambda e: e.memset(zrow[:], 0.0), [], ['zrow'])
        p.dma(SP, p.H[2048:2049, :], zrow[:], r=['zrow'], w=[('H', 'z')])
        p.S.barrier()
        uT = p.sb("uTd", [128, 8, 512], BF16, st2)
        ybT = p.sb("ybT", [128, 8, 512], BF16, st2)
        mT = p.sb("mT", [128, 8, 512], BF16, st2)
        bufs = {
            'x': (p.sb("xtd", [128, D], F32, st2), 'xtd'),
            'junk': (p.sb("junkd", [128, D], BF16, st2), 'junkd'),
            'ss': (p.sb("ssd", [128, 4], F32, st2), 'ssd'),
            'xn': (p.sb("xnd", [128, D], BF16, st2), 'xnd'),
            'ps': (p.psb[0], ('ps', 0)),
        }
        zs = p.sb("zs", [128, D], F32, st2)
        of_ = p.sb("of_", [128, D], F32, st2)
        ob_ = p.sb("ob_", [128, D], F32, st2)
        on8 = p.sb("on8", [128, 4, 8], F32, st2)
        ybin = p.sb("ybin", [128, D], BF16, st2)
        gw = [p.sb("gw%d" % i, [128, 512], BF16, st2) for i in range(4)]
        x1 = p.sb("x1", [128, D], F32, st2)
        xt2 = p.sb("xt2d", [128, D], F32, st2)
        h2 = p.sb("h2", [128, D], F32, st2)
        h2b = p.sb("h2b", [128, D], BF16, st2)
        h2T = p.sb("h2T", [128, 8, 128], F32, st2)
        s2 = p.sb("s2", [128, 4], F32, st2)
        real_op = p.S.op
        recD = []

        def rec_into(lst):
            p.S.op = lambda eng, fn, reads=(), writes=(), dma=False: lst.append((eng, fn, list(reads), list(writes), dma))

        for sti in range(4):
            head_l, mid_l, tail_l = [], [], []
            recD.append((head_l, mid_l, tail_l))
            rec_into(head_l)
            for ti in range(4):
                tt_ = sti * 4 + ti
                t = OWN0 + tt_
                p.norm_T(x[t * 128:(t + 1) * 128, :], p.gs1, p.sh1, lambda c: (uT[:, c, ti * 128:(ti + 1) * 128], ('uTd', ti)), bufs, 'd')
                for hf_ in range(2):
                    ps = p.psb[1 + hf_]
                    for kc in range(8):
                        p.mm(ps[:, :], uT[:, kc, ti * 128:(ti + 1) * 128], Wzg[:, kc, hf_ * 512:(hf_ + 1) * 512], kc == 0, kc == 7,
                             r=[('uTd', ti), 'Wzg'], w=[('ps', 1 + hf_)])
                    sl = slice(hf_ * 512, (hf_ + 1) * 512)
                    p.act(zs[:, sl], ps[:, :], AF.Silu, r=[('ps', 1 + hf_)], w=[('zs', hf_)])
                p.dma(SP, of_[:], p.Od[0][tt_ * 128:(tt_ + 1) * 128, :], w=['of_'])
                p.dma(SP, ob_[:], p.Od[1][tt_ * 128:(tt_ + 1) * 128, :], w=['ob_'])
                p.tt(DVE, of_[:], of_[:], ob_[:], ALU.add, r=['of_', 'ob_'], w=['of_'])
                p.tt(DVE, ob_[:], of_[:], of_[:], ALU.mult, r=['of_', 'ob_'], w=['ob_'])
                p.S.op(DVE, lambda e: e.tensor_reduce(on8[:, 0, :], ob_[:].rearrange("q (h c) -> q h c", c=128), AX.X, ALU.add), ['ob_'], ['on8'])
                p.ts(DVE, on8[:, 1, :], on8[:, 0, :], 1.0 / 128, EPS, ALU.mult, ALU.add, r=['on8'], w=['on8'])
                p.act(on8[:, 2, :], on8[:, 1, :], AF.Ln, r=['on8'], w=['on8'])
                p.act(on8[:, 3, :], on8[:, 2, :], AF.Exp, r=['on8'], w=['on8'], scale=-0.5)
                for h in range(8):
                    hs = slice(h * 128, (h + 1) * 128)
                    p.stt(DVE, of_[:, hs], of_[:, hs], on8[:, 3, h:h + 1], gnb[:, hs], ALU.mult, ALU.mult, r=['of_', 'on8', 'gnb'], w=['of_'])
                p.tt(DVE, ybin[:], of_[:], zs[:], ALU.mult, r=['of_', ('zs', 0), ('zs', 1)], w=['ybin'])
                pv = p.psb[3][:].bitcast(BF16)
                for c in range(8):
                    p.tr(pv[:, c * 128:(c + 1) * 128], ybin[:, c * 128:(c + 1) * 128], p.ident_bf[:], r=['ybin'], w=[('ps', 3)])
                p.cp(ACT, ybT[:, :, ti * 128:(ti + 1) * 128], pv[:, :].rearrange("q (c k) -> q c k", k=128), r=[('ps', 3)], w=[('ybT', ti)])
            rec_into(mid_l)
            allu = [('uTd', i) for i in range(4)]
            ally = [('ybT', i) for i in range(4)]
            k0 = sti * 512
            for dc in range(8):
                dsl = slice(dc * 128, (dc + 1) * 128)
                b0 = 4 * (dc % 2)
                for fc in range(4):
                    p.mm(p.psb[b0][:, :], Wfo[:, fc, dsl], fmT[:, fc, k0:k0 + 512], fc == 0, fc == 3, r=['Wfo'], w=[('ps', b0)])
                for fc in range(8):
                    p.mm(p.psb[b0 + 1][:, :], Wgo[:, fc, dsl], ybT[:, fc, :], fc == 0, fc == 7, r=['Wgo'] + ally, w=[('ps', b0 + 1)])
                for kc in range(8):
                    p.mm(p.psb[b0 + 2][:, :], Wzg[:, kc, 1024 + dc * 128:1024 + (dc + 1) * 128], uT[:, kc, :], kc == 0, kc == 7, r=['Wzg'] + allu, w=[('ps', b0 + 2)])
                for kc in range(8):
                    p.mm(p.psb[b0 + 3][:, :], Wzg[:, kc, 2048 + dc * 128:2048 + (dc + 1) * 128], uT[:, kc, :], kc == 0, kc == 7, r=['Wzg'] + allu, w=[('ps', b0 + 3)])
                for br_, (pg, py) in enumerate([(b0 + 2, b0), (b0 + 3, b0 + 1)]):
                    gb_ = gw[(dc % 2) * 2 + br_]
                    gk_ = ('gw', (dc % 2) * 2 + br_)
                    p.act(gb_[:], p.psb[pg][:, :], AF.Sigmoid, r=[('ps', pg)], w=[gk_])
                    p.tt(DVE, gb_[:], gb_[:], p.psb[py][:, :], ALU.mult, r=[gk_, ('ps', py)], w=[gk_])
                p.tt(DVE, mT[:, dc, :], gw[(dc % 2) * 2][:], gw[(dc % 2) * 2 + 1][:], ALU.add, r=[('gw', (dc % 2) * 2), ('gw', (dc % 2) * 2 + 1)], w=[('mT', dc)])
            allm = [('mT', i) for i in range(8)]
            rec_into(tail_l)
            for ti in range(4):
                tt_ = sti * 4 + ti
                t = OWN0 + tt_
                xt, xk = xt2, 'xt2'
                p.dma(SP, xt[:], x[t * 128:(t + 1) * 128, :], w=[xk])
                for hf_ in range(2):
                    ps = p.psb[4 + hf_]
                    sl = slice(hf_ * 512, (hf_ + 1) * 512)
                    for dc in range(8):
                        p.mm(ps[:, :], mT[:, dc, ti * 128:(ti + 1) * 128], Wmo[:, dc, sl], dc == 0, dc == 7, r=allm + ['Wmo'], w=[('ps', 4 + hf_)])
                    p.tt(DVE, x1[:, sl], ps[:, :], g1_b[:, sl], ALU.mult, r=[('ps', 4 + hf_)], w=['x1'])
                p.tt(DVE, x1[:], x1[:], xt[:], ALU.add, r=['x1', xk], w=['x1'])
                p.dma(POOL, p.X1[tt_ * 128:(tt_ + 1) * 128, :], x1[:], r=['x1'], w=[('X1', tt_)])
                p.act(h2[:], x1[:], AF.Square, r=['x1'], w=['h2', 's2'], accum_out=s2[:, 0:1])
                p.act(s2[:, 2:3], s2[:, 0:1], AF.Ln, r=['s2'], w=['s2'], scale=1.0 / D, bias=p.eps_t[:, 0:1])
                p.act(s2[:, 3:4], s2[:, 2:3], AF.Exp, r=['s2'], w=['s2'], scale=-0.5)
                p.stt(DVE, h2[:], x1[:], s2[:, 3:4], gs2_b[:], ALU.mult, ALU.mult, r=['x1', 's2', 'h2'], w=['h2'])
                p.tt(DVE, h2[:], h2[:], sh2_b[:], ALU.add, r=['h2'], w=['h2'])
                p.cp(ACT, h2b[:], h2[:], r=['h2'], w=['h2b'])
                p.dma(POOL, p.H[tt_ * 128:(tt_ + 1) * 128, :], h2b[:], r=['h2b'], w=[('H', tt_)])
                for hf_ in range(2):
                    for c in range(4):
                        p.tr(p.psb[6][:, c * 128:(c + 1) * 128], h2[:, (hf_ * 4 + c) * 128:(hf_ * 4 + c + 1) * 128], p.ident_f[:], r=['h2'], w=[('ps', 6)])
                    p.cp(ACT, h2T[:, hf_ * 4:(hf_ + 1) * 4, :], p.psb[6][:, :].rearrange("q (c k) -> q c k", k=128), r=[('ps', 6)], w=['h2T'])
                for kc in range(8):
                    p.mm(p.psb[7][:, 0:NE], h2T[:, kc, :], Wr[:, kc, :], kc == 0, kc == 7, r=['h2T', 'Wr'], w=[('ps', 7)])
                p.tt(DVE, p.logits[:, tt_, :], p.psb[7][:, 0:NE], brb[:], ALU.add, r=[('ps', 7), 'brb'], w=[('logits', tt_)])
        p.S.op = real_op
        for o_ in recD[0][0]:
            real_op(*o_)
        for i_, (head_l, mid_l, tail_l) in enumerate(recD):
            for o_ in mid_l:
                real_op(*o_)
            nxt = list(recD[i_ + 1][0]) if i_ + 1 < len(recD) else []
            tl = list(tail_l)
            CH = 6
            while tl or nxt:
                for _ in range(CH):
                    if tl:
                        real_op(*tl.pop(0))
                for _ in range(CH):
                    if nxt:
                        real_op(*nxt.pop(0))
        p.S.barrier()
        stD.close()
        if p.debug == 'D':
            o = p.out("dbg_x1", [2048, D], F32)
            p.dma(SP, o, p.X1, r=[])
            o = p.out("dbg_logits", [128, 16, NE], F32)
            p.dma(SP, o, p.logits[:], r=[])
            o = p.out("dbg_H", [2049, D], BF16)
            p.dma(SP, o, p.H, r=[])


    def phaseE(p):
        CAPS = MOE_CAPS
        TB = [sum(CAPS[:i]) for i in range(NE)]
        NB = sum(CAPS)
        DUMMY = NB * 128
        CAPMAX = max(CAPS) * 128
        wg = p.inp("w_gate", [NE, D, D], F32)
        wu = p.inp("w_up", [NE, D, D], F32)
        wd = p.inp("w_down", [NE, D, D], F32)
        bg = p.inp("b_gate", [NE, D], F32)
        bu = p.inp("b_up", [NE, D], F32)
        bd = p.inp("b_down", [NE, D], F32)
        fng = p.inp("fng", [D], F32)
        yout = p.out("y", [2048, D], F32)
        Y = p.scratch("Yslots", [NB * 128 + 128, D], F32)
        IDX = p.scratch("IDX", [NB * 128 + 128, 1], I32)
        stE = contextlib.ExitStack()
        K = {}
        for nm, dt_ in [('ones_bf', BF16), ('ustrict_bf', BF16), ('iota32', F32), ('ecol', F32), ('blk128', F32),
                        ('tokid', I32), ('l_strict', F32), ('basetab', F32), ('captab', F32), ('rowoff', F32)]:
            a = p.const_np[nm]
            d = p.inp("c_" + nm, a.shape, dt_) if ("c_" + nm) not in p.din else p.din["c_" + nm]
            K[nm] = p.sb("ke_" + nm, a.shape, dt_, stE)
            p.dma(SP, K[nm][:], d, w=[('k', nm)])
        Bg = p.sb("Bg", [32, D], BF16, stE)
        Bu = p.sb("Bu", [32, D], BF16, stE)
        Bd = p.sb("Bd", [32, D], BF16, stE)
        p.dma(POOL, Bg[:], bg, w=['Bg'])
        p.dma(POOL, Bu[:], bu, w=['Bu'])
        p.dma(POOL, Bd[:], bd, w=['Bd'])
        fnb = p.sb("fnb", [128, D], F32, stE)
        p.dma(SP, fnb[:], fng.partition_broadcast(128), w=['fnb'])
        i2048 = p.sb("i2048", [128, NB], I32, stE)
        p.S.op(DVE, lambda e: e.memset(i2048[:], 2048), [], ['i2048'])
        p.dma(SP, IDX[0:NB * 128, :].rearrange("(q b) o -> q (b o)", q=128), i2048[:], r=['i2048'], w=['IDX'])
        zy = p.sb("zy", [128, D], F32, stE)
        p.S.op(DVE, lambda e: e.memset(zy[:], 0.0), [], ['zy'])
        p.dma(SP, Y[DUMMY:DUMMY + 128, :], zy[:], r=['zy'], w=['Yz'])
        p.S.barrier()
        mx = p.sb("mx", [128, 16, 8], F32, stE)
        mi = p.sb("mi", [128, 16, 8], U32, stE)
        idf = p.sb("idf", [128, 16, 4], F32, stE)
        wts = p.sb("wts", [128, 16, 4], F32, stE)
        nm0 = p.sb("nm0", [128, 16], F32, stE)
        ssum = p.sb("ssum", [128, 16], F32, stE)
        Mf = p.sb("Mf", [128, 16, NE], F32, stE)
        Mb = p.sb("Mb", [128, 16, NE], BF16, stE)
        for tt_ in range(16):
            p.S.op(DVE, lambda e, tt_=tt_: e.max(mx[:, tt_, :], p.logits[:, tt_, :]), [], [('mx', tt_)])
            p.S.op(DVE, lambda e, tt_=tt_: e.max_index(mi[:, tt_, :], mx[:, tt_, :], p.logits[:, tt_, :]), [('mx', tt_)], [('mi', tt_)])
            p.cp(DVE, idf[:, tt_, :], mi[:, tt_, 0:4], r=[('mi', tt_)], w=[('idf', tt_)])
            p.ts(DVE, nm0[:, tt_:tt_ + 1], mx[:, tt_, 0:1], -1.0, None, ALU.mult, r=[('mx', tt_)], w=[('nm0', tt_)])
            p.act(wts[:, tt_, :], mx[:, tt_, 0:4], AF.Exp, r=[('mx', tt_), ('nm0', tt_)], w=[('wts', tt_)], bias=nm0[:, tt_:tt_ + 1], scale=1.0)
            p.S.op(DVE, lambda e, tt_=tt_: e.tensor_reduce(ssum[:, tt_:tt_ + 1], wts[:, tt_, :], AX.X, ALU.add), [('wts', tt_)], [('ssum', tt_)])
            p.S.op(DVE, lambda e, tt_=tt_: e.reciprocal(ssum[:, tt_:tt_ + 1], ssum[:, tt_:tt_ + 1]), [('ssum', tt_)], [('ssum', tt_)])
            p.ts(DVE, wts[:, tt_, :], wts[:, tt_, :], ssum[:, tt_:tt_ + 1], None, ALU.mult, r=[('wts', tt_), ('ssum', tt_)], w=[('wts', tt_)])
            p.ts(DVE, Mf[:, tt_, :], K['iota32'][:], idf[:, tt_, 0:1], None, ALU.is_equal, r=[('idf', tt_)], w=[('Mf', tt_)])
            for j in range(1, 4):
                p.stt(DVE, Mf[:, tt_, :], K['iota32'][:], idf[:, tt_, j:j + 1], Mf[:, tt_, :], ALU.is_equal, ALU.add, r=[('idf', tt_), ('Mf', tt_)], w=[('Mf', tt_)])
            p.cp(DVE, Mb[:, tt_, :], Mf[:, tt_, :], r=[('Mf', tt_)], w=[('Mb', tt_)])
        allM = [('Mb', i) for i in range(16)]
        for tt_ in range(16):
            p.mm(p.psb[0][:, 0:NE], K['ones_bf'][:], Mb[:, tt_, :], tt_ == 0, tt_ == 15, r=allM, w=[('ps', 0)])
        for tt_ in range(16):
            p.mm(p.psb[1][0:32, 0:1], Mb[:, tt_, :], K['ones_bf'][:, 0:1], tt_ == 0, tt_ == 15, r=allM, w=[('ps', 1)])
        cntb = p.sb("cntb", [128, NE], F32, stE)
        cc = p.sb("cc", [32, 8], F32, stE)
        sq32 = p.sb("sq32", [32, 3, NE], F32, stE)
        Pm = p.sb("Pm", [32, NE], F32, stE)
        p.cp(DVE, cntb[:], p.psb[0][:, 0:NE], r=[('ps', 0)], w=['cntb'])
        p.cp(DVE, cc[:, 0:1], p.psb[1][0:32, 0:1], r=[('ps', 1)], w=['cc'])
        p.ts(DVE, sq32[:, 0, :], cntb[0:32, :], cc[:, 0:1], None, ALU.is_gt, r=['cntb', 'cc'], w=['sq32'])
        p.ts(DVE, sq32[:, 1, :], cntb[0:32, :], cc[:, 0:1], None, ALU.is_equal, r=['cntb', 'cc'], w=['sq32'])
        p.tt(DVE, sq32[:, 1, :], sq32[:, 1, :], K['l_strict'][0:32, 0:32], ALU.mult, r=['sq32'], w=['sq32'])
        p.tt(DVE, sq32[:, 0, :], sq32[:, 0, :], sq32[:, 1, :], ALU.add, r=['sq32'], w=['sq32'])
        p.S.op(DVE, lambda e: e.tensor_reduce(cc[:, 1:2], sq32[:, 0, :], AX.X, ALU.add), ['sq32'], ['cc'])
        p.ts(DVE, Pm[:], K['iota32'][0:32, :], cc[:, 1:2], None, ALU.is_equal, r=['cc'], w=['Pm'])
        p.tt(DVE, sq32[:, 0, :], Pm[:], K['basetab'][0:32, :], ALU.mult, r=['Pm', 'sq32'], w=['sq32'])
        p.S.op(DVE, lambda e: e.tensor_reduce(cc[:, 2:3], sq32[:, 0, :], AX.X, ALU.add), ['sq32'], ['cc'])
        p.tt(DVE, sq32[:, 1, :], Pm[:], K['captab'][0:32, :], ALU.mult, r=['Pm', 'sq32'], w=['sq32'])
        p.S.op(DVE, lambda e: e.tensor_reduce(cc[:, 3:4], sq32[:, 1, :], AX.X, ALU.add), ['sq32'], ['cc'])
        lb = p.sb("lb", [32, 3, 128], F32, stE)
        one32f = p.sb("one32f", [32, 128], F32, stE)
        p.S.op(DVE, lambda e: e.memset(one32f[:], 1.0), [], ['one32f'])
        p.ts(DVE, lb[:, 0, :], one32f[:], cc[:, 2:3], None, ALU.mult, r=['one32f', 'cc'], w=['lb'])
        p.ts(DVE, lb[:, 1, :], one32f[:], cc[:, 3:4], None, ALU.mult, r=['one32f', 'cc'], w=['lb'])
        p.ts(DVE, lb[:, 2, :], one32f[:], K['ecol'][0:32, 0:1], None, ALU.mult, r=['one32f'], w=['lb'])
        p.mm(p.psb[0][:, 0:NE], lb[:, 0, :], p.ident_f[0:32, 0:32], True, True, r=['lb'], w=[('ps', 0)])
        p.mm(p.psb[0][:, 32:64], lb[:, 1, :], p.ident_f[0:32, 0:32], True, True, r=['lb'], w=[('ps', 0)])
        p.mm(p.psb[0][:, 64:96], lb[:, 2, :], Pm[:], True, True, r=['lb', 'Pm'], w=[('ps', 0)])
        bcb = p.sb("bcb", [128, 3, NE], F32, stE)
        p.ts(DVE, bcb[:, 0, :], p.psb[0][:, 0:NE], -float(DUMMY), None, ALU.add, r=[('ps', 0)], w=['bcb'])
        p.cp(DVE, bcb[:, 1, :], p.psb[0][:, 32:64], r=[('ps', 0)], w=['bcb'])
        p.ts(DVE, bcb[:, 2, :], p.psb[0][:, 64:96], 128.0, None, ALU.mult, r=[('ps', 0)], w=['bcb'])
        idwf = p.sb("idwf", [128, NE], F32, stE)
        idw = p.sb("idw", [128, NE], I32, stE)
        p.ts(DVE, idwf[:], bcb[:, 2, :], K['rowoff'][:, 0:1], None, ALU.add, r=['bcb'], w=['idwf'])
        p.cp(DVE, idw[:], idwf[:], r=['idwf'], w=['idw'])
        rk = p.sb("rk", [128, NE], F32, stE)
        sel = p.sb("sel", [128, NE], F32, stE)
        destf = p.sb("destf", [128, 16, 4], F32, stE)
        desti = p.sb("desti", [128, 16, 4], I32, stE)
        for tt_ in range(16):
            ps = p.psb[2 + tt_ % 2]
            pk = ('ps', 2 + tt_ % 2)
            for t2 in range(tt_):
                p.mm(ps[:, 0:NE], K['ones_bf'][:], Mb[:, t2, :], t2 == 0, False, r=allM, w=[pk])
            p.mm(ps[:, 0:NE], K['ustrict_bf'][:], Mb[:, tt_, :], tt_ == 0, True, r=allM, w=[pk])
            p.tt(DVE, sel[:], ps[:, 0:NE], bcb[:, 1, :], ALU.is_lt, r=[pk, 'bcb'], w=['sel'])
            p.tt(DVE, rk[:], ps[:, 0:NE], bcb[:, 0, :], ALU.add, r=[pk, 'bcb'], w=['rk'])
            p.tt(DVE, rk[:], rk[:], sel[:], ALU.mult, r=['rk', 'sel'], w=['rk'])
            p.ts(DVE, rk[:], rk[:], float(DUMMY), None, ALU.add, r=['rk'], w=['rk'])
            for j in range(4):
                p.ts(DVE, sel[:], K['iota32'][:], idf[:, tt_, j:j + 1], None, ALU.is_equal, r=[('idf', tt_)], w=['sel'])
                p.tt(DVE, sel[:], sel[:], rk[:], ALU.mult, r=['sel', 'rk'], w=['sel'])
                p.S.op(DVE, lambda e, tt_=tt_, j=j: e.tensor_reduce(destf[:, tt_, j:j + 1], sel[:], AX.X, ALU.add), ['sel'], [('destf', tt_)])
            p.cp(DVE, desti[:, tt_, :], destf[:, tt_, :], r=[('destf', tt_)], w=[('desti', tt_)])
            for j in range(4):
                p.S.op(POOL, lambda e, tt_=tt_, j=j: e.indirect_dma_start(
                    out=IDX[:, :], out_offset=bass.IndirectOffsetOnAxis(ap=desti[:, tt_, j:j + 1], axis=0),
                    in_=K['tokid'][:, tt_:tt_ + 1], in_offset=None), [('desti', tt_), 'IDX0'], [('IDXs', tt_, j)], dma=True)
        p.S.barrier()
        idx_sb = p.sb("idx_sb", [128, NB], I32, stE)
        p.dma(SP, idx_sb[:], IDX[0:NB * 128, :].rearrange("(b q) o -> q (b o)", q=128), w=['idx_sb'], allow_slow_non_contiguous=True)
        p.S.barrier()
        stX = contextlib.ExitStack()
        Wb = [[p.sb("W%s%d" % (n_, i), [128, 8, D], BF16, stX) for n_ in "gud"] for i in range(2)]
        xg = [p.sb("xg%d" % i, [128, D], BF16, stX) for i in range(2)]
        xT = p.sb("xTe", [128, 8, CAPMAX], BF16, stX)
        aT = p.sb("aT", [128, 8, CAPMAX], BF16, stX)
        ohb = p.sb("ohb", [32, 512], BF16, stX)
        ones32 = p.sb("ones32", [32, 512], BF16, stX)
        p.S.op(DVE, lambda e: e.memset(ones32[:], 1.0), [], ['ones32'])
        wk = [[p.sb("wk%d_%d" % (i, j), [128, 512], BF16, stX) for j in range(4)] for i in range(2)]
        ysb = [p.sb("ysb%d" % i, [128, D], F32, stX) for i in range(2)]
        w2d = [w_.rearrange("e (q j) n -> (e q) (j n)", j=8) for w_ in (wg, wu, wd)]
        ng_ = 0
        nd_ = 0
        nch = 0
        for ex in range(NE):
            wbi = ex % 2
            capt = CAPS[ex]
            for wi_ in range(3):
                p.S.op(POOL, lambda e, wbi=wbi, wi_=wi_, ex=ex: e.indirect_dma_start(
                    out=Wb[wbi][wi_][:].rearrange("q j n -> q (j n)"), out_offset=None, in_=w2d[wi_][:, :],
                    in_offset=bass.IndirectOffsetOnAxis(ap=idw[:, ex:ex + 1], axis=0)), ['idw'], [('W', wbi, wi_)], dma=True)
            p.ts(DVE, ohb[:], ones32[:], Pm[:, ex:ex + 1], None, ALU.mult, r=['ones32', 'Pm'], w=['ohb'])
            for k in range(capt):
                b = TB[ex] + k
                gi = ng_ % 2
                ng_ += 1
                p.S.op(POOL, lambda e, b=b, gi=gi: e.indirect_dma_start(
                    out=xg[gi][:, :], out_offset=None, in_=p.H[:, :],
                    in_offset=bass.IndirectOffsetOnAxis(ap=idx_sb[:, b:b + 1], axis=0)), ['idx_sb'], [('xg', gi)], dma=True)
                pb_ = 0 if k % 2 == 0 else 7
                pv = p.psb[pb_][:].bitcast(BF16)
                xgv = xg[gi][:].rearrange("s (q j) -> s j q", j=8)
                for c in range(8):
                    p.tr(pv[:, c * 128:(c + 1) * 128], xgv[:, c, :], p.ident_bf[:], r=[('xg', gi)], w=[('ps', pb_)])
                p.cp(ACT if k % 2 == 0 else DVE, xT[:, :, k * 128:(k + 1) * 128], pv[:, :].rearrange("q (c k) -> q c k", k=128),
                     r=[('ps', pb_)], w=[('xTe', k)])
            allx = [('xTe', k) for k in range(capt)]
            nsl = capt * 128
            chunks = [(c0, min(512, nsl - c0)) for c0 in range(0, nsl, 512)]
            for fc in range(8):
                fs = slice(fc * 128, (fc + 1) * 128)
                for (c0, n_) in chunks:
                    st_ = nch % 2
                    nch += 1
                    bG, bU = 1 + 2 * st_, 2 + 2 * st_
                    for (wi_, Bt, bk) in ((0, Bg, bG), (1, Bu, bU)):
                        for kc in range(8):
                            p.mm(p.psb[bk][:, 0:n_], Wb[wbi][wi_][:, kc, :].rearrange("q (f j) -> q j f", j=8)[:, fc, :], xT[:, kc, c0:c0 + n_],
                                 kc == 0, False, r=[('W', wbi, wi_)] + allx, w=[('ps', bk)])
                        p.mm(p.psb[bk][:, 0:n_], Bt[:].rearrange("e (f j) -> e j f", j=8)[:, fc, :], ohb[:, 0:n_], False, True, r=['ohb'], w=[('ps', bk)])
                    g_, sg_, u_, t_ = wk[st_]
                    wkk = lambda j: ('wk', st_, j)
                    p.ts(DVE, g_[:, 0:n_], p.psb[bG][:, 0:n_], 7.0, None, ALU.min, r=[('ps', bG)], w=[wkk(0)])
                    p.act(sg_[:, 0:n_], g_[:, 0:n_], AF.Sigmoid, r=[wkk(0)], w=[wkk(1)], scale=1.702)
                    p.ts(DVE, u_[:, 0:n_], p.psb[bU][:, 0:n_], 7.0, -7.0, ALU.min, ALU.max, r=[('ps', bU)], w=[wkk(2)])
                    p.stt(DVE, t_[:, 0:n_], u_[:, 0:n_], 1.0, g_[:, 0:n_], ALU.add, ALU.mult, r=[wkk(2), wkk(0)], w=[wkk(3)])
                    p.tt(DVE, aT[:, fc, c0:c0 + n_], t_[:, 0:n_], sg_[:, 0:n_], ALU.mult, r=[wkk(3), wkk(1)], w=[('aT', fc)])
            alla = [('aT', fc) for fc in range(8)]
            for k in range(capt):
                b = TB[ex] + k
                yi = nd_ % 2
                nd_ += 1
                yb_ = ysb[yi]
                for hf_ in range(2):
                    hs = slice(hf_ * 512, (hf_ + 1) * 512)
                    pb_ = 5 if hf_ == 0 else 6
                    for fc in range(8):
                        p.mm(p.psb[pb_][:, :], aT[:, fc, k * 128:(k + 1) * 128], Wb[wbi][2][:, fc, hs], fc == 0, False, r=alla + [('W', wbi, 2)], w=[('ps', pb_)])
                    p.mm(p.psb[pb_][:, :], ohb[:, 0:128], Bd[:, hs], False, True, r=['ohb'], w=[('ps', pb_)])
                    p.cp(ACT, yb_[:, hs], p.psb[pb_][:, :], r=[('ps', pb_)], w=[('ysb', yi)])
                p.dma(SP, Y[b * 128:(b + 1) * 128, :], yb_[:], r=[('ysb', yi)], w=[('Y', b)])
        p.S.barrier()
        stX.close()
        yg = [p.sb("yg%d" % i, [128, D], F32, stE) for i in range(4)]
        acc = p.sb("acc", [128, D], F32, stE)
        x1ts = [p.sb("x1t%d" % i, [128, D], F32, stE) for i in range(2)]
        outb = [p.sb("outb%d" % i, [128, D], F32, stE) for i in range(2)]
        p.dma(SP, x1ts[0][:], p.X1[0:128, :], w=[('x1t', 0)])
        fs_ = p.sb("fs_", [128, 4], F32, stE)
        ng = 0
        for tt_ in range(16):
            x1t = x1ts[tt_ % 2]
            xk_ = ('x1t', tt_ % 2)
            ot = outb[tt_ % 2]
            ok_ = ('outb', tt_ % 2)
            if tt_ + 1 < 16:
                p.dma(SP, x1ts[(tt_ + 1) % 2][:], p.X1[(tt_ + 1) * 128:(tt_ + 2) * 128, :], w=[('x1t', (tt_ + 1) % 2)])
            for j in range(4):
                yb_ = yg[ng % 4]
                yk = ('yg', ng % 4)
                ng += 1
                p.S.op(POOL, lambda e, tt_=tt_, j=j, yb_=yb_: e.indirect_dma_start(
                    out=yb_[:, :], out_offset=None, in_=Y[:, :],
                    in_offset=bass.IndirectOffsetOnAxis(ap=desti[:, tt_, j:j + 1], axis=0)), [], [yk], dma=True)
                if j == 0:
                    p.ts(DVE, acc[:], yb_[:], wts[:, tt_, 0:1], None, ALU.mult, r=[yk], w=['acc'])
                else:
                    p.stt(DVE, acc[:], yb_[:], wts[:, tt_, j:j + 1], acc[:], ALU.mult, ALU.add, r=[yk, 'acc'], w=['acc'])
            p.tt(DVE, acc[:], acc[:], p.g2_b[:], ALU.mult, r=['acc'], w=['acc'])
            p.tt(DVE, acc[:], acc[:], x1t[:], ALU.add, r=['acc', xk_], w=['acc'])
            p.act(ot[:], acc[:], AF.Square, r=['acc'], w=[ok_, 'fs_'], accum_out=fs_[:, 0:1])
            p.act(fs_[:, 2:3], fs_[:, 0:1], AF.Ln, r=['fs_'], w=['fs_'], scale=1.0 / D, bias=p.eps_t[:, 0:1])
            p.act(fs_[:, 3:4], fs_[:, 2:3], AF.Exp, r=['fs_'], w=['fs_'], scale=-0.5)
            p.stt(DVE, ot[:], acc[:], fs_[:, 3:4], fnb[:], ALU.mult, ALU.mult, r=['acc', 'fs_', ok_], w=[ok_])
            p.dma(SP, yout[tt_ * 128:(tt_ + 1) * 128, :], ot[:], r=[ok_], w=[('yout', tt_)])
        p.S.barrier()
        stE.close()


def core_inputs(inputs, core, consts):
    b, hf = core // 2, core % 2
    rev = (hf == 0)
    m = {}
    x = np.asarray(inputs['x'][b], np.float32)
    m['x_seq'] = np.ascontiguousarray(x[::-1] if rev else x)
    cp = np.stack([inputs['c'][b], inputs['c_ctx']], 0).astype(np.float32)
    m['cT'] = np.ascontiguousarray(cp.reshape(2, 8, 128).transpose(2, 1, 0))
    m['bmod'] = fm_layout(inputs['b_mod'][0], 48)
    m['n1g'] = fm_layout(inputs['norm1_g'][0], 8)
    m['n2g'] = fm_layout(inputs['norm2_g'][0], 8)
    m['w_mod'] = np.ascontiguousarray(inputs['w_mod'][0], np.float32)
    cx = np.asarray(inputs['ctx'][b], np.float32)
    m['ctx_seq'] = np.ascontiguousarray(cx[::-1] if rev else cx)
    w_in = np.array(inputs['w_in'][0], np.float32)
    if rev:
        w2 = w_in.copy()
        w2[:, 2048:2056], w2[:, 2056:2064] = w_in[:, 2056:2064], w_in[:, 2048:2056]
        w2[:, 2064:2072], w2[:, 2072:2080] = w_in[:, 2072:2080], w_in[:, 2064:2072]
        w_in = w2
    m['w_in'] = np.ascontiguousarray(w_in)
    cwv = np.asarray(inputs['conv_w'][0], np.float32).reshape(9, 16, 128)
    if rev:
        cwv = cwv[::-1]
    cd = np.zeros((128, 16, 9, 128), np.float32)
    qi = np.arange(128)
    cd[qi, :, :, qi] = cwv.transpose(2, 1, 0)
    m['conv_diag'] = cd
    al = np.asarray(inputs['a_log'][0], np.float32)
    db = np.asarray(inputs['dt_bias'][0], np.float32)
    if rev:
        al, db = al[::-1], db[::-1]
    m['alog_t'] = np.ascontiguousarray(np.tile(al.reshape(1, 16), (NT + 2, 1)))
    m['dtb_t'] = np.ascontiguousarray(np.tile(db.reshape(1, 16), (NT + 2, 1)))
    pos = np.arange(L)[::-1] if rev else np.arange(L)
    own = pos[2048:]
    ang = (2.0 * np.pi / L) * ((pos[:, None].astype(np.int64) * own[None, :].astype(np.int64)) % L)
    tab = np.stack([np.cos(ang), np.sin(ang)], 0).astype(np.float32)
    tab = tab.reshape(2, 4, 8, 128, 4, 512).transpose(4, 1, 3, 0, 2, 5)
    m['dft_tab'] = bf(tab)
    cang = (2.0 * np.pi / 128) * ((np.arange(128)[:, None] * np.arange(128)[None, :]) % 128)
    m['cdft'] = bf(np.stack([np.cos(cang), -np.sin(cang)], 1))
    m['gng_t'] = np.ascontiguousarray(np.tile(np.asarray(inputs['gdn_norm_g'][0], np.float32), 8))
    m['w_fo'] = np.ascontiguousarray(inputs['w_fourier_out'][0], np.float32)
    m['w_go'] = np.ascontiguousarray(inputs['w_gdn_out'][0], np.float32)
    m['w_mo'] = np.ascontiguousarray(inputs['w_merge_out'][0], np.float32)
    m['w_router'] = np.ascontiguousarray(inputs['w_router'][0], np.float32)
    m['b_router'] = np.ascontiguousarray(inputs['b_router'][0], np.float32)
    for nm_, key in [('w_gate', 'w_gate'), ('w_up', 'w_up'), ('w_down', 'w_down'), ('b_gate', 'b_gate'), ('b_up', 'b_up'), ('b_down', 'b_down')]:
        m[nm_] = np.ascontiguousarray(inputs[key][0], np.float32)
    m['fng'] = np.ascontiguousarray(inputs['final_norm_g'], np.float32)
    for k, v in consts.items():
        m['c_' + k] = v
    return m


def build(debug=None):
    nc = bass.Bass("TRN2", target_bir_lowering=False)
    p = Builder(nc, debug)
    p.phase0()
    if debug == 'AD1':
        p.phaseA()
        p.debug = 'D1'
        p.phaseD()
        p.S.barrier(); p.S.emit(); p.st.close()
        return nc, p
    if debug == 'Gs':
        p.QKVs = p.inp("QKVs_in", [L + CTXL, QKV], BF16)
        p.ba = p.sb("ba", [128, NT + 2, 32], F32)
        p.dma(SP, p.ba[:], p.inp("ba_in", [128, NT + 2, 32], F32), w=['ba'])
        p.S.barrier()
        p.debug = 'G0'
    elif debug in ('Ds1', 'Ds'):
        p.XF = p.inp("XF_in", [L, 512], BF16)
        p.Od = [p.inp("Of_in", [2048, 1024], F32), p.inp("Ob_in", [2048, 1024], F32)]
        p.inp("w_in", [D, IN_COLS], F32)
        p.debug = 'D1' if debug == 'Ds1' else 'D'
    elif debug != '0':
        p.phaseA()
    if debug not in ('0', 'A', 'Ds', 'Ds1'):
        p.phaseG()
    if debug not in ('0', 'A', 'G', 'G0', 'Gs'):
        p.phaseD()
    if debug in (None, 'E'):
        p.phaseE()
    p.S.barrier()
    p.S.emit()
    p.st.close()
    return nc, p


def run(inputs, debug=None):
    nc, p = build(debug)
    maps = []
    for c in range(8):
        m = core_inputs(inputs, c, p.const_np)
        maps.append({k: m[k] for k in p.din})
    res = run_bass_kernel_spmd(nc, maps, core_ids=list(range(8)))
    return res.results


def kernel(**inputs):
    nc, p = build(None)
    maps = []
    for c in range(8):
        m = core_inputs(inputs, c, p.const_np)
        maps.append({k: m[k] for k in p.din})
    res = run_bass_kernel_spmd(nc, maps, core_ids=list(range(8)))
    out = np.zeros((4, L, D), np.float32)
    for c in range(8):
        b, hf = c // 2, c % 2
        y = np.asarray(res.results[c]['y'], np.float32)
        if hf == 1:
            out[b, 2048:] = y
        else:
            out[b, :2048] = y[::-1]
    return out
```

```python
import contextlib
import numpy as np
import ml_dtypes
import concourse.bass as bass
import concourse.mybir as mybir
from concourse.bass_utils import run_bass_kernel_spmd

F32 = mybir.dt.float32
BF16 = mybir.dt.bfloat16
I32 = mybir.dt.int32
U32 = mybir.dt.uint32
AF = mybir.ActivationFunctionType
ALU = mybir.AluOpType
AX = mybir.AxisListType

PE, ACT, DVE, POOL, SP = 'pe', 'act', 'dve', 'pool', 'sp'
ENGS = [PE, ACT, DVE, POOL, SP]
NDS = 8

D = 1024
L = 4096
NT = 32
OWN0 = 16
CTXL = 256
QKV = 2048
BETA_OFF = 2048
A_OFF = 2064
GDN_IN = 2080
Z_OFF = 2080
F_OFF = 3104
GA_OFF = 3616
GB_OFF = 4640
IN_COLS = 5664
NE = 32
EPS = 1e-6
MOE_CAPS = [8] + [6] * 3 + [5] * 4 + [4] * 8 + [3] * 16


class Sched:
    def __init__(s, nc):
        s.nc = nc
        s.ops = {e: [] for e in ENGS}
        s.res = {}
        s.ndma = {e: 0 for e in ENGS}
        s.epoch = 0

    def op(s, eng, fn, reads=(), writes=(), dma=False):
        idx = len(s.ops[eng])
        me = (eng, idx)
        deps = []
        for k in reads:
            r = s.res.get(k)
            if r and r[0] is not None:
                deps.append(r[0])
        for k in writes:
            r = s.res.get(k)
            if r:
                if r[0] is not None:
                    deps.append(r[0])
                deps.extend(r[1])
        waits = []
        best = {}
        for p in deps:
            pe, pi = p
            if s.ops[pe][pi]['dma']:
                waits.append(p)
                continue
            if pe == eng and eng == PE:
                continue
            if pi > best.get(pe, -1):
                best[pe] = pi
        for pe, pi in best.items():
            waits.append((pe, pi))
        o = dict(fn=fn, waits=waits, sig=False, dma=dma, dsem=None, dval=None, dn=0, ep=s.epoch)
        if dma:
            n = s.ndma[eng]
            s.ndma[eng] += 1
            o['dsem'] = n % NDS
            o['dval'] = 16 * (n // NDS + 1)
            o['dn'] = n
        s.ops[eng].append(o)
        for k in reads:
            s.res.setdefault(k, [None, []])[1].append(me)
        for k in writes:
            s.res[k] = [me, []]
        return me

    def barrier(s):
        for e in ENGS:
            pend = []
            for e2 in ENGS:
                n = len(s.ops[e2])
                if e2 != e:
                    for i in range(n - 1, -1, -1):
                        if s.ops[e2][i]['fn'] is not None and not s.ops[e2][i]['dma']:
                            pend.append((e2, i))
                            break
                cnt = 0
                for i in range(n - 1, -1, -1):
                    if s.ops[e2][i]['dma']:
                        pend.append((e2, i))
                        cnt += 1
                        if cnt >= NDS:
                            break
            s.ops[e].append(dict(fn=None, waits=pend, sig=False, dma=False, dsem=None, dval=None, dn=0, ep=s.epoch))
        s.res = {}
        s.epoch += 1

    def emit(s):
        nc = s.nc
        for e in ENGS:
            for o in s.ops[e]:
                for (pe, pi) in o['waits']:
                    po = s.ops[pe][pi]
                    if not po['dma']:
                        if po['fn'] is None:
                            raise RuntimeError("wait on barrier pseudo-op")
                        po['sig'] = True
        sigcount = {}
        used = set()
        for e in ENGS:
            c = {}
            for i, o in enumerate(s.ops[e]):
                if o['sig']:
                    c[o['ep']] = c.get(o['ep'], 0) + 1
                    used.add((e, o['ep']))
                sigcount[(e, i)] = c.get(o['ep'], 0)
        s.maxsig = max([0] + [sigcount[k] for k in sigcount])
        with contextlib.ExitStack() as st:
            esem = {k: st.enter_context(nc.semaphore("es_%s_%d" % k)) for k in sorted(used)}
            dsem = {e: [st.enter_context(nc.semaphore("ds_%s_%d" % (e, i))) for i in range(NDS)] for e in ENGS}
            block = st.enter_context(nc.Block())

            def run(e, eng):
                known = {}
                knownd = {}
                for o in s.ops[e]:
                    need = {}
                    needd = {}
                    if o['dma'] and o['dn'] >= NDS:
                        needd[(e, o['dsem'])] = o['dval'] - 16
                    for (pe, pi) in o['waits']:
                        po = s.ops[pe][pi]
                        if po['dma']:
                            k = (pe, po['dsem'])
                            needd[k] = max(needd.get(k, 0), po['dval'])
                        else:
                            k = (pe, po['ep'])
                            need[k] = max(need.get(k, 0), sigcount[(pe, pi)])
                    for k, v in need.items():
                        if known.get(k, 0) < v:
                            eng.wait_ge(esem[k], v)
                            known[k] = v
                    for k, v in needd.items():
                        if knownd.get(k, 0) < v:
                            eng.wait_ge(dsem[k[0]][k[1]], v)
                            knownd[k] = v
                    if o['fn'] is None:
                        continue
                    inst = o['fn'](eng)
                    if o['dma']:
                        inst.then_inc(dsem[e][o['dsem']], 16)
                    elif o['sig']:
                        inst.then_inc(esem[(e, o['ep'])], 1)

            block.tensor(lambda eng: run(PE, eng))
            block.scalar(lambda eng: run(ACT, eng))
            block.vector(lambda eng: run(DVE, eng))
            block.gpsimd(lambda eng: run(POOL, eng))
            block.sync(lambda eng: run(SP, eng))


class Prog:
    def __init__(p, nc):
        p.nc = nc
        p.S = Sched(nc)
        p.st = contextlib.ExitStack()
        p.din = {}
        p.dout = {}

    def inp(p, name, shape, dt):
        t = p.nc.dram_tensor(name, list(shape), dt, kind="ExternalInput").ap()
        p.din[name] = t
        return t

    def out(p, name, shape, dt):
        t = p.nc.dram_tensor(name, list(shape), dt, kind="ExternalOutput").ap()
        p.dout[name] = t
        return t

    def scratch(p, name, shape, dt):
        return p.nc.dram_tensor(name, list(shape), dt, kind="Internal").ap()

    def sb(p, name, shape, dt, st=None):
        return (st or p.st).enter_context(p.nc.sbuf_tensor(name, list(shape), dt))

    def psum(p, name, shape, dt):
        return p.st.enter_context(p.nc.psum_tensor(name, list(shape), dt))

    def dma(p, q, out, in_, r=(), w=(), **kw):
        return p.S.op(q, lambda e: e.dma_start(out=out, in_=in_, **kw), r, w, dma=True)

    def mm(p, out, lhsT, rhs, start, stop, r=(), w=()):
        return p.S.op(PE, lambda e: e.matmul(out, lhsT, rhs, start=start, stop=stop), r, w)

    def tr(p, out, in_, ident, r=(), w=()):
        return p.S.op(PE, lambda e: e.transpose(out, in_, ident), r, w)

    def act(p, out, in_, func, r=(), w=(), eng=ACT, **kw):
        return p.S.op(eng, lambda e: e.activation(out=out, in_=in_, func=func, **kw), r, w)

    def ts(p, eng, out, in0, s1, s2, op0, op1=None, r=(), w=(), **kw):
        if op1 is None:
            return p.S.op(eng, lambda e: e.tensor_scalar(out, in0, s1, s2, op0, **kw), r, w)
        return p.S.op(eng, lambda e: e.tensor_scalar(out, in0, s1, s2, op0, op1, **kw), r, w)

    def tt(p, eng, out, in0, in1, op, r=(), w=()):
        return p.S.op(eng, lambda e: e.tensor_tensor(out, in0, in1, op), r, w)

    def stt(p, eng, out, in0, scalar, in1, op0, op1, r=(), w=()):
        return p.S.op(eng, lambda e: e.scalar_tensor_tensor(out, in0, scalar, in1, op0, op1), r, w)

    def cp(p, eng, out, in_, r=(), w=()):
        if eng == ACT:
            return p.S.op(eng, lambda e: e.copy(out, in_), r, w)
        return p.S.op(eng, lambda e: e.tensor_copy(out, in_), r, w)

    def generic(p, eng, fn, r=(), w=()):
        return p.S.op(eng, fn, r, w)


def bf(a):
    return np.ascontiguousarray(a).astype(ml_dtypes.bfloat16)


def fm_layout(v, nchunk):
    return np.ascontiguousarray(np.asarray(v, np.float32).reshape(nchunk, 128).T)


def host_consts():
    c = {}
    c['ident_bf'] = bf(np.eye(128, dtype=np.float32))
    c['ident_f'] = np.eye(128, dtype=np.float32)
    c['ones_f'] = np.ones((128, 128), np.float32)
    t = np.arange(128)
    c['u_incl'] = (t[:, None] <= t[None, :]).astype(np.float32)
    c['u_strict'] = (t[:, None] < t[None, :]).astype(np.float32)
    c['l_incl'] = np.ascontiguousarray(c['u_incl'].T)
    c['l_strict'] = np.ascontiguousarray(c['u_strict'].T)
    rep4 = lambda m: np.ascontiguousarray(np.tile(m, (1, 4)))
    c['ms4_f'] = rep4(c['u_strict']); c['mi4_f'] = rep4(c['u_incl'])
    c['ms4_b'] = rep4(c['l_strict']); c['mi4_b'] = rep4(c['l_incl'])
    c['i4'] = bf(rep4(np.eye(128, dtype=np.float32)))
    lv = np.zeros((2, 7, 128, 512), np.float32)
    for li in range(7):
        sz = 1 << li
        j = t[:, None]; i = t[None, :]
        m = ((j // (2 * sz)) == (i // (2 * sz))) & ((j % (2 * sz)) < sz) & ((i % (2 * sz)) >= sz)
        lv[0, li] = rep4(m.astype(np.float32))
        lv[1, li] = rep4(m.T.astype(np.float32))
    c['lvl'] = bf(lv.transpose(2, 0, 1, 3))
    c['ones_bf'] = bf(np.ones((128, 128), np.float32))
    c['ustrict_bf'] = bf(c['u_strict'])
    c['iota32'] = np.ascontiguousarray(np.tile(np.arange(32, dtype=np.float32)[None, :], (128, 1)))
    c['ecol'] = np.ascontiguousarray(np.arange(128, dtype=np.float32)[:, None])
    c['blk128'] = np.zeros((128, 1), np.float32)
    tb = np.concatenate([[0], np.cumsum(MOE_CAPS)[:-1]]).astype(np.float32) * 128.0
    c['basetab'] = np.ascontiguousarray(np.tile(tb[None, :], (128, 1)))
    c['captab'] = np.ascontiguousarray(np.tile((np.array(MOE_CAPS, np.float32) * 128.0)[None, :], (128, 1)))
    c['rowoff'] = np.ascontiguousarray((np.arange(8)[None, :] * 128 + np.arange(128)[:, None]).astype(np.float32))
    c['tokid'] = np.ascontiguousarray((np.arange(16)[None, :] * 128 + np.arange(128)[:, None]).astype(np.int32))
    return c


class Builder(Prog):
    def __init__(p, nc, debug=None):
        super().__init__(nc)
        p.debug = debug
        p.const_np = host_consts()
        p.psb = [p.psum("ps%d" % i, [128, 512], F32) for i in range(8)]

    def load_const(p, name, dt):
        a = p.const_np[name]
        d = p.inp("c_" + name, a.shape, dt)
        t = p.sb("k_" + name, a.shape, dt)
        p.dma(SP, t[:], d, w=[('k', name)])
        return t

    def phase0(p):
        nc = p.nc
        p.ident_bf = p.load_const('ident_bf', BF16)
        p.ident_f = p.load_const('ident_f', F32)
        p.ones_f = p.load_const('ones_f', F32)
        cT = p.inp("cT", [128, 8, 2], F32)
        bmod = p.inp("bmod", [128, 48], F32)
        n1g = p.inp("n1g", [128, 8], F32)
        n2g = p.inp("n2g", [128, 8], F32)
        wmod = p.inp("w_mod", [D, 6 * D], F32)
        p.eps_t = p.sb("eps_t", [128, 1], F32)
        p.S.op(DVE, lambda e: e.memset(p.eps_t[:], EPS), [], ['eps_t'])
        p.modsb = p.sb("modsb", [128, 48, 2], F32)
        p.vecs = p.sb("vecs", [128, 8, 8], F32)
        st0 = contextlib.ExitStack()
        scT = p.sb("scT", [128, 8, 2], F32, st0)
        bm = p.sb("bm", [128, 48], F32, st0)
        g1t = p.sb("n1g_sb", [128, 8], F32, st0)
        g2t = p.sb("n2g_sb", [128, 8], F32, st0)
        wb = [p.sb("wmodb%d" % i, [128, 8, 512], F32, st0) for i in range(2)]
        p.dma(SP, scT[:], cT, w=['scT'])
        p.dma(SP, bm[:], bmod, w=['bm'])
        p.dma(SP, g1t[:], n1g, w=['n1g'])
        p.dma(SP, g2t[:], n2g, w=['n2g'])
        p.act(scT[:], scT[:], AF.Silu, r=['scT'], w=['scT'])
        wv = wmod.rearrange("(kc q) n -> q kc n", q=128)
        psM = p.psb[0]
        for blk in range(12):
            b = wb[blk % 2]
            p.dma(SP, b[:], wv[:, :, blk * 512:(blk + 1) * 512], w=[('wmodb', blk % 2)])
            for fc in range(4):
                j = blk * 4 + fc
                for kc in range(8):
                    p.mm(psM[:, 2 * j:2 * j + 2], b[:, kc, fc * 128:(fc + 1) * 128], scT[:, kc, :],
                         kc == 0, kc == 7, r=[('wmodb', blk % 2), 'scT'], w=[('ps', 0)])
        pv = psM[:, 0:96].rearrange("q (j m) -> q j m", m=2)
        for m in range(2):
            p.tt(DVE, p.modsb[:, :, m], pv[:, :, m], bm[:], ALU.add, r=[('ps', 0), 'bm'], w=['modsb'])
        p.stt(DVE, p.vecs[:, 0, :], p.modsb[:, 8:16, 0], 1.0, g1t[:], ALU.add, ALU.mult, r=['modsb', 'n1g'], w=['vecs'])
        p.stt(DVE, p.vecs[:, 1, :], p.modsb[:, 8:16, 1], 1.0, g1t[:], ALU.add, ALU.mult, r=['modsb', 'n1g'], w=['vecs'])
        p.stt(DVE, p.vecs[:, 2, :], p.modsb[:, 32:40, 0], 1.0, g2t[:], ALU.add, ALU.mult, r=['modsb', 'n2g'], w=['vecs'])
        p.S.barrier()
        st0.close()
        if p.debug == '0':
            o = p.out("dbg_mod", [128, 48, 2], F32)
            p.dma(SP, o, p.modsb[:], r=['modsb'])
            o2 = p.out("dbg_vecs", [128, 8, 8], F32)
            p.dma(SP, o2, p.vecs[:], r=['vecs'])

    def gs1(p, c): return p.vecs[:, 0, c:c + 1]
    def sh1(p, c): return p.modsb[:, c, 0:1]
    def cgs1(p, c): return p.vecs[:, 1, c:c + 1]
    def csh1(p, c): return p.modsb[:, c, 1:2]

    def norm_T(p, xsrc, gs, sh, uT_dst, bufs, tag):
        xt, xk = bufs['x']
        p.dma(SP, xt[:], xsrc, w=[xk])
        junk, jk = bufs['junk']
        ss, sk = bufs['ss']
        p.act(junk[:], xt[:], AF.Square, r=[xk], w=[jk, sk], accum_out=ss[:, 0:1])
        p.act(ss[:, 2:3], ss[:, 0:1], AF.Ln, r=[sk], w=[sk], scale=1.0 / D, bias=p.eps_t[:, 0:1])
        p.act(ss[:, 3:4], ss[:, 2:3], AF.Exp, r=[sk], w=[sk], scale=-0.5)
        xn, nk = bufs['xn']
        p.ts(DVE, xn[:], xt[:], ss[:, 3:4], None, ALU.mult, r=[xk, sk], w=[nk])
        pb, pk = bufs['ps']
        pbv = pb[:].bitcast(BF16)
        for c in range(8):
            p.tr(pbv[:, c * 128:(c + 1) * 128], xn[:, c * 128:(c + 1) * 128], p.ident_bf[:], r=[nk, ('k', 'ident_bf')], w=[pk])
        if tag == 'split':
            return
        p.norm_T_b(gs, sh, uT_dst, bufs)

    def norm_T_b(p, gs, sh, uT_dst, bufs):
        pb, pk = bufs['ps']
        pbv = pb[:].bitcast(BF16)
        for c in range(8):
            dst, dk = uT_dst(c)
            if c % 2 == 0:
                p.act(dst, pbv[:, c * 128:(c + 1) * 128], AF.Identity, r=[pk, 'modsb', 'vecs'], w=[dk],
                      scale=gs(c), bias=sh(c))
            else:
                p.ts(DVE, dst, pbv[:, c * 128:(c + 1) * 128], gs(c), sh(c), ALU.mult, ALU.add, r=[pk, 'modsb', 'vecs'], w=[dk])

    def phaseA(p):
        x = p.inp("x_seq", [L, D], F32)
        ctx = p.inp("ctx_seq", [CTXL, D], F32)
        w_in = p.inp("w_in", [D, IN_COLS], F32)
        cw = p.inp("conv_diag", [128, 16, 9, 128], F32)
        p.XF = p.scratch("XF", [L, 512], BF16)
        p.QKVs = p.scratch("QKVs", [L + CTXL, QKV], BF16)
        p.ba = p.sb("ba", [128, NT + 2, 32], F32)
        stA = contextlib.ExitStack()
        wA = p.sb("wA", [128, 8, 2592], BF16, stA)
        wv = w_in.rearrange("(kc q) n -> q kc n", q=128)
        for kc in range(8):
            p.dma(POOL, wA[:, kc, 0:2080], wv[:, kc, 0:2080], w=[('wA', kc)])
            p.dma(POOL, wA[:, kc, 2080:2592], wv[:, kc, F_OFF:F_OFF + 512], w=[('wA', kc)])
        cwts = [p.sb("cwt%d" % i, [128, 2, 9, 128], BF16, stA) for i in range(2)]
        NTT = NT + 2
        uT = p.sb("uT", [128, 8, NTT * 128], BF16, stA)
        stA1 = contextlib.ExitStack()
        NB1 = 4
        bufs = [{
            'x': (p.sb("xt%d" % i, [128, D], F32, stA1), ('xt', i)),
            'junk': (p.sb("junk%d" % i, [128, D], BF16, stA1), ('junk', i)),
            'ss': (p.sb("ss%d" % i, [128, 4], F32, stA1), ('ss', i)),
            'xn': (p.sb("xn%d" % i, [128, D], BF16, stA1), ('xn', i)),
            'ps': (p.psb[[0, 1, 6, 7][i]], ('ps', [0, 1, 6, 7][i])),
        } for i in range(NB1)]
        xfb = [p.sb("xfb%d" % i, [128, 512], BF16, stA1) for i in range(2)]
        def a1_args(t):
            if t < NT:
                return x[t * 128:(t + 1) * 128, :], p.gs1, p.sh1
            return ctx[(t - NT) * 128:(t - NT + 1) * 128, :], p.cgs1, p.csh1

        def a1_front(t):
            src, gs, sh = a1_args(t)
            p.norm_T(src, gs, sh, None, bufs[t % NB1], 'split')

        DEPTH = 2
        for t0 in range(DEPTH):
            a1_front(t0)
        for t in range(NTT):
            b = bufs[t % NB1]
            if t + DEPTH < NTT:
                a1_front(t + DEPTH)
            src, gs, sh = a1_args(t)
            p.norm_T_b(gs, sh, lambda c, t=t: (uT[:, c, t * 128:(t + 1) * 128], ('uT', t)), b)
            ps = p.psb[2 + t % 2]
            for kc in range(8):
                p.mm(ps[:, 0:32], uT[:, kc, t * 128:(t + 1) * 128], wA[:, kc, 2048:2080], kc == 0, kc == 7,
                     r=[('wA', kc), ('uT', t)], w=[('ps', 2 + t % 2)])
            p.cp(DVE, p.ba[:, t, :], ps[:, 0:32], r=[('ps', 2 + t % 2)], w=[('ba', t)])
            if t < NT:
                ps = p.psb[4 + t % 2]
                for kc in range(8):
                    p.mm(ps[:, :], uT[:, kc, t * 128:(t + 1) * 128], wA[:, kc, 2080:2592], kc == 0, kc == 7,
                         r=[('wA', kc), ('uT', t)], w=[('ps', 4 + t % 2)])
                xb = xfb[t % 2]
                p.cp(DVE, xb[:], ps[:, :], r=[('ps', 4 + t % 2)], w=[('xfb', t % 2)])
                p.dma(POOL, p.XF[t * 128:(t + 1) * 128, :], xb[:], r=[('xfb', t % 2)], w=[('XF', t)])
        p.S.barrier()
        stA1.close()
        LD = 72
        C0 = LD + L + 64
        CBN = C0 + 258
        cb = [p.sb("cb%d" % i, [128, 3, CBN], BF16, stA) for i in range(2)]
        for i in range(2):
            p.S.op(DVE, (lambda e, t_=cb[i]: e.memset(t_[:], 0.0)), [], [('cb', i)])
        qt = [p.sb("qt%d" % i, [128, 512], BF16, stA) for i in range(2)]
        for ch in range(16):
            cbuf = cb[ch % 2]
            ck = ('cb', ch % 2)
            if ch % 2 == 0:
                cwt = cwts[(ch // 2) % 2]
                cwk = ('cwt', (ch // 2) % 2)
                p.dma(POOL, cwt[:], cw[:, ch:ch + 2, :, :], w=[cwk])
            for st_ in range(9):
                ntok = 512 if st_ < 8 else 256
                t0 = st_ * 512
                ps = p.psb[st_ % 4]
                pk = ('ps', st_ % 4)
                for kc in range(8):
                    p.mm(ps[:, 0:ntok], wA[:, kc, ch * 128:(ch + 1) * 128], uT[:, kc, t0:t0 + ntok], kc == 0, kc == 7,
                         r=[('wA', kc)] + [('uT', t0 // 128 + q) for q in range(ntok // 128)], w=[pk])
                if st_ < 8:
                    o0 = LD + t0
                    p.cp(ACT, cbuf[:, 0, o0:o0 + 512], ps[:, 0:512], r=[pk], w=[ck])
                    sv = ps[:, 0:512].rearrange("q (r c) -> q r c", c=64)
                    p.cp(DVE, cbuf[:, 1, o0:o0 + 512].rearrange("q (r c) -> q r c", c=64)[:, :, 0:63], sv[:, :, 0:63], r=[pk], w=[ck])
                    p.cp(DVE, cbuf[:, 2, o0:o0 + 512].rearrange("q (r c) -> q r c", c=64)[:, :, 1:64], sv[:, :, 1:64], r=[pk], w=[ck])
                else:
                    p.cp(ACT, cbuf[:, 0, C0 + 1:C0 + 257], ps[:, 0:256], r=[pk], w=[ck])
            for t in range(NTT):
                if t % 4 == 0:
                    ps = p.psb[4 + (t // 4) % 4]
                    pk = ('ps', 4 + (t // 4) % 4)
                if t < NT:
                    taps = [(dy, dx) for dy in (-1, 0, 1) for dx in (-1, 0, 1)]
                else:
                    taps = [(0, dx) for dx in (-1, 0, 1)]
                for ti, (dy, dx) in enumerate(taps):
                    if t < NT:
                        base = LD + 128 * t + 64 * dy + dx
                        lhsT = cbuf[:, {-1: 1, 0: 0, 1: 2}[dx], base:base + 128]
                    else:
                        base = C0 + 1 + (t - NT) * 128 + dx
                        lhsT = cbuf[:, 0, base:base + 128]
                    tap = (dy + 1) * 3 + (dx + 1)
                    p.mm(ps[:, (t % 4) * 128:(t % 4 + 1) * 128], lhsT, cwt[:, ch % 2, tap, :], ti == 0, ti == len(taps) - 1,
                         r=[ck, cwk], w=[pk])
                if t % 4 == 3 or t == NTT - 1:
                    nt_ = t % 4 + 1
                    tb = t - t % 4
                    qi_ = (tb // 4) % 2
                    q_ = qt[qi_]
                    p.act(q_[:, 0:nt_ * 128], ps[:, 0:nt_ * 128], AF.Silu, r=[pk], w=[('qt', qi_)])
                    row = tb * 128 if tb < NT else L
                    dstv = p.QKVs[row:row + nt_ * 128, ch * 128:(ch + 1) * 128].rearrange("(a q) c -> q a c", q=128)
                    p.dma(SP, dstv, q_[:, 0:nt_ * 128].rearrange("q (a c) -> q a c", c=128), r=[('qt', qi_)], w=[('QKVs', tb, ch)])
        p.S.barrier()
        stA.close()
        if p.debug == 'A':
            o = p.out("dbg_qkv", [L + CTXL, QKV], BF16)
            p.dma(SP, o, p.QKVs, r=[])
            o = p.out("dbg_ba", [128, NT + 2, 32], F32)
            p.dma(SP, o, p.ba[:], r=[])
            o = p.out("dbg_xf", [L, 512], BF16)
            p.dma(SP, o, p.XF, r=[])


    def phaseG(p):
        DKS = 128 ** -0.5
        alog = p.inp("alog_t", [NT + 2, 16], F32)
        dtb = p.inp("dtb_t", [NT + 2, 16], F32)
        NTT = NT + 2
        p.Od = [p.scratch("O_f", [16 * 128, 1024], F32), p.scratch("O_b", [16 * 128, 1024], F32)]
        stG = contextlib.ExitStack()
        K = {}
        for nm, dt_ in [('u_incl', F32), ('l_incl', F32), ('u_strict', F32), ('l_strict', F32),
                        ('ms4_f', F32), ('mi4_f', F32), ('ms4_b', F32), ('mi4_b', F32), ('i4', BF16), ('lvl', BF16)]:
            a = p.const_np[nm]
            d = p.inp("c_" + nm, a.shape, dt_)
            K[nm] = p.sb("k_" + nm, a.shape, dt_, stG)
            p.dma(SP, K[nm][:], d, w=[('k', nm)])
        kr = lambda *n: [('k', x) for x in n]
        p.S.barrier()
        lgs = p.sb("lgs", [128, NTT, 16], F32, stG)
        bts = p.sb("bts", [128, NTT, 16], F32, stG)
        nbt = p.sb("nbts", [128, NTT, 16], F32, stG)
        prm = p.sb("prm", [128, 2, NTT, 16], F32, stG)
        p.dma(SP, prm[:, 0], alog.partition_broadcast(128), w=['prm'])
        p.dma(SP, prm[:, 1], dtb.partition_broadcast(128), w=['prm'])
        p.act(prm[:, 0], prm[:, 0], AF.Exp, r=['prm'], w=['prm'])
        p.tt(DVE, lgs[:], p.ba[:, :, 16:32], prm[:, 1], ALU.add, r=['prm'], w=['lgs'])
        p.act(lgs[:], lgs[:], AF.Exp, r=['lgs'], w=['lgs'])
        p.ts(DVE, lgs[:], lgs[:], 1.0, None, ALU.add, r=['lgs'], w=['lgs'])
        p.act(lgs[:], lgs[:], AF.Ln, r=['lgs'], w=['lgs'])
        p.stt(DVE, lgs[:], lgs[:], -1.0, prm[:, 0], ALU.mult, ALU.mult, r=['lgs', 'prm'], w=['lgs'])
        p.act(bts[:], p.ba[:, :, 0:16], AF.Exp, r=[], w=['bts'], scale=-1.0)
        p.ts(DVE, bts[:], bts[:], 1.0, None, ALU.add, r=['bts'], w=['bts'])
        p.S.op(DVE, lambda e: e.reciprocal(bts[:], bts[:]), ['bts'], ['bts'])
        p.ts(DVE, nbt[:], bts[:], -1.0, None, ALU.mult, r=['bts'], w=['nbt'])
        Sf = p.sb("S_f32", [128, 16, 128], F32, stG)
        Sb = [p.sb("S_bf%d" % i, [128, 16, 128], BF16, stG) for i in range(2)]
        p.S.op(DVE, lambda e: e.memset(Sf[:], 0.0), [], [('Sf', h) for h in range(16)])
        p.S.op(DVE, lambda e: e.memset(Sb[0][:], 0.0), [], [('Sb', 0, h) for h in range(16)])
        sbi = [0] * 16
        qkvb = [p.sb("qkvb%d" % i, [128, QKV], BF16, stG) for i in range(2)]
        sqs = [p.sb("sq%d" % i, [128, 1024], F32, stG) for i in range(2)]
        nrms = [p.sb("nrm%d" % i, [128, 4, 8], F32, stG) for i in range(2)]
        qkn = [p.sb("qkn%d" % i, [128, 8, 128], BF16, stG) for i in range(2)]
        kT = [p.sb("kT%d" % i, [128, 4, 128], BF16, stG) for i in range(2)]
        qT = [p.sb("qT%d" % i, [128, 4, 128], BF16, stG) for i in range(2)]
        gsc = [p.sb("gsc%d" % i, [128, 6, 16], F32, stG) for i in range(2)]
        CT = []
        for ci in range(2):
            c_ = dict(i=ci, ba=3 * ci)
            for nm_, shp, dt_ in [('LW', [128, 4, 128], F32), ('Eb', [128, 512], F32), ('Ems', [128, 512], F32), ('Emi', [128, 512], F32),
                                  ('Nb', [128, 512], BF16), ('NTb', [128, 512], BF16), ('NTl', [128, 6, 512], BF16),
                                  ('X0', [128, 512], BF16), ('X1', [128, 512], BF16), ('XT0', [128, 512], BF16), ('XT1', [128, 512], BF16),
                                  ('Yb', [128, 512], BF16), ('tmpb', [128, 512], BF16), ('tmpf', [128, 512], F32), ('qkp', [128, 512], BF16),
                                  ('kdec', [128, 512], BF16), ('rb', [128, 512], BF16), ('ub', [128, 512], BF16), ('o1', [128, 512], F32)]:
                c_[nm_] = p.sb("%s_%d" % (nm_, ci), shp, dt_, stG)
            CT.append(c_)
        ob = [p.sb("ob%d" % i, [128, 1024], F32, stG) for i in range(2)]
        PS = lambda i: p.psb[i]
        PK = lambda i: ('ps', i)
        sched = [(32, 0, False), (33, 0, False), (33, 1, False), (32, 1, False)]
        fw = [(t, 0, t >= OWN0) for t in range(NT)]
        bw = [(t, 1, True) for t in range(NT - 1, OWN0 - 1, -1)]
        while fw or bw:
            if fw:
                sched.append(fw.pop(0))
            if bw and (len(fw) < 2 * len(bw) + 1):
                sched.append(bw.pop(0))
        if p.debug == 'G0':
            sched = sched[:4]
        loaded = {}
        step = 0
        nload = 0
        real_op = p.S.op
        recs = []

        def rec_into(lst):
            p.S.op = lambda eng, fn, reads=(), writes=(), dma=False: lst.append((eng, fn, list(reads), list(writes), dma))

        for (t, d, need_out) in sched:
            prep_l, stage_ll, tail_l = [], [], []
            recs.append((prep_l, stage_ll, tail_l))
            rec_into(prep_l)
            bi = nload % 2
            nload += 1
            qb = qkvb[bi]
            sq = sqs[bi]
            nrm = nrms[bi]
            sqk = ('sq', bi)
            nrk = ('nrm', bi)
            row = t * 128 if t < NT else L + (t - NT) * 128
            p.dma(SP, qb[:], p.QKVs[row:row + 128, :], w=[('qkvb', bi)])
            p.tt(DVE, sq[:], qb[:, 0:1024], qb[:, 0:1024], ALU.mult, r=[('qkvb', bi)], w=[sqk])
            p.S.op(DVE, lambda e, nrm=nrm, sq=sq: e.tensor_reduce(nrm[:, 0, :], sq[:].rearrange("q (h c) -> q h c", c=128), AX.X, ALU.add), [sqk], [nrk])
            p.ts(DVE, nrm[:, 1, :], nrm[:, 0, :], EPS, None, ALU.add, r=[nrk], w=[nrk])
            p.act(nrm[:, 2, :], nrm[:, 1, :], AF.Ln, r=[nrk], w=[nrk])
            p.act(nrm[:, 3, :], nrm[:, 2, :], AF.Exp, r=[nrk], w=[nrk], scale=-0.5)
            p.ts(DVE, nrm[:, 3, 0:4], nrm[:, 3, 0:4], DKS, None, ALU.mult, r=[nrk], w=[nrk])
            qn = qkn[bi]
            p.tt(DVE, qn[:], qb[:, 0:1024].rearrange("q (h c) -> q h c", c=128), nrm[:, 3, :].unsqueeze(2).to_broadcast([128, 8, 128]), ALU.mult,
                 r=[('qkvb', bi), nrk], w=[('qkn', bi)])
            kTb, qTb = kT[bi], qT[bi]
            pv = PS(6)[:].bitcast(BF16)
            for h in range(4):
                p.tr(pv[:, h * 128:(h + 1) * 128], qn[:, 4 + h, :], p.ident_bf[:], r=[('qkn', bi)], w=[PK(6)])
            p.cp(ACT, kTb[:].rearrange("q h c -> q (h c)"), pv[:, 0:512], r=[PK(6)], w=[('kT', bi)])
            if need_out:
                pv5 = PS(6)[:].bitcast(BF16)[:, 512:1024]
                for h in range(4):
                    p.tr(pv5[:, h * 128:(h + 1) * 128], qn[:, h, :], p.ident_bf[:], r=[('qkn', bi)], w=[PK(6)])
                p.cp(ACT, qTb[:].rearrange("q h c -> q (h c)"), pv5[:, 0:512], r=[PK(6)], w=[('qT', bi)])
            g = gsc[bi]
            gk = ('gsc', bi)
            lg = lgs[:, t, :]
            ps6 = PS(7)
            p.mm(ps6[:, 0:8], K['u_incl'][:], lgs[:, t, 0:8], True, True, r=kr('u_incl') + ['lgs'], w=[PK(7)])
            p.mm(ps6[:, 8:16], K['l_incl'][:], lgs[:, t, 8:16], True, True, r=kr('l_incl') + ['lgs'], w=[PK(7)])
            p.mm(ps6[:, 16:32], p.ones_f[:], lgs[:, t, :], True, True, r=['lgs'], w=[PK(7)])
            p.cp(DVE, g[:, 0:2, :].rearrange("q a c -> q (a c)"), ps6[:, 0:32], r=[PK(7)], w=[gk])
            p.act(g[:, 2, :], g[:, 0, :], AF.Exp, r=[gk], w=[gk])
            p.ts(DVE, g[:, 3, :], g[:, 2, :], -1.0, None, ALU.mult, r=[gk], w=[gk])
            p.tt(DVE, g[:, 4, :], g[:, 1, :], g[:, 0, :], ALU.subtract, r=[gk], w=[gk])
            p.act(g[:, 4, :], g[:, 4, :], AF.Exp, r=[gk], w=[gk])
            p.act(g[:, 5, :], g[:, 1, :], AF.Exp, r=[gk], w=[gk])
            ms_lhs = K['l_strict'] if d == 0 else K['u_strict']
            mi_rhs = K['u_incl'] if d == 0 else K['l_incl']
            MS4 = K['ms4_f'] if d == 0 else K['ms4_b']
            MI4 = K['mi4_f'] if d == 0 else K['mi4_b']
            H4 = lambda ap: ap.rearrange("q (h c) -> q h c", c=128)
            bci = lambda ap: ap.unsqueeze(2).to_broadcast([128, 4, 128])
            bcm = lambda ap: ap.unsqueeze(1).to_broadcast([128, 4, 128])
            obuf = ob[step % 2]

            def key(c_, n):
                return (n, c_['i'])

            def st_D(c_, grp):
                hd0 = d * 8 + grp * 4
                p.tt(DVE, c_['LW'][:], bcm(ms_lhs[:]), bci(lgs[:, t, hd0:hd0 + 4]), ALU.mult, r=['lgs'], w=[key(c_, 'LW')])
                for j4 in range(4):
                    p.mm(PS(c_['ba'])[:, j4 * 128:(j4 + 1) * 128], c_['LW'][:, j4, :], mi_rhs[:], True, True, r=[key(c_, 'LW')], w=[PK(c_['ba'])])
                for j4 in range(4):
                    hq = (grp * 4 + j4) // 2
                    p.mm(PS(c_['ba'] + 1)[:, j4 * 128:(j4 + 1) * 128], kTb[:, hq, :], kTb[:, hq, :], True, True, r=[('kT', bi)], w=[PK(c_['ba'] + 1)])
                if need_out:
                    for j4 in range(4):
                        hq = (grp * 4 + j4) // 2
                        p.mm(PS(c_['ba'] + 2)[:, j4 * 128:(j4 + 1) * 128], kTb[:, hq, :], qTb[:, hq, :], True, True,
                             r=[('kT', bi), ('qT', bi)], w=[PK(c_['ba'] + 2)])

            def st_E(c_, grp):
                hd0 = d * 8 + grp * 4
                p.act(c_['Eb'][:], PS(c_['ba'])[:, :], AF.Exp, r=[PK(c_['ba'])], w=[key(c_, 'Eb')])
                p.tt(DVE, c_['Ems'][:], c_['Eb'][:], MS4[:], ALU.mult, r=[key(c_, 'Eb')], w=[key(c_, 'Ems')])
                p.tt(DVE, H4(c_['Ems'][:]), H4(c_['Ems'][:]), bci(nbt[:, t, hd0:hd0 + 4]), ALU.mult, r=[key(c_, 'Ems'), 'nbt'], w=[key(c_, 'Ems')])
                p.tt(DVE, c_['Nb'][:], PS(c_['ba'] + 1)[:, :], c_['Ems'][:], ALU.mult, r=[PK(c_['ba'] + 1), key(c_, 'Ems')], w=[key(c_, 'Nb')])
                if need_out:
                    p.tt(DVE, c_['Emi'][:], c_['Eb'][:], MI4[:], ALU.mult, r=[key(c_, 'Eb')], w=[key(c_, 'Emi')])
                    p.tt(DVE, c_['qkp'][:], PS(c_['ba'] + 2)[:, :], c_['Emi'][:], ALU.mult, r=[PK(c_['ba'] + 2), key(c_, 'Emi')], w=[key(c_, 'qkp')])

            def st_NT(c_, grp):
                pv3 = PS(c_['ba'])[:].bitcast(BF16)
                for j4 in range(4):
                    p.tr(pv3[:, j4 * 128:(j4 + 1) * 128], c_['Nb'][:, j4 * 128:(j4 + 1) * 128], p.ident_bf[:], r=[key(c_, 'Nb')], w=[PK(c_['ba'])])
                p.cp(ACT, c_['NTb'][:], pv3[:, 0:512], r=[PK(c_['ba'])], w=[key(c_, 'NTb')])
                p.tt(DVE, c_['NTl'][:], K['lvl'][:, 1 - d, 1:7, :], c_['NTb'][:].unsqueeze(1).to_broadcast([128, 6, 512]), ALU.mult,
                     r=[key(c_, 'NTb')], w=[key(c_, 'NTl')])
                p.tt(DVE, c_['tmpb'][:], c_['Nb'][:], K['lvl'][:, d, 0, :], ALU.mult, r=[key(c_, 'Nb')], w=[key(c_, 'tmpb')])
                p.tt(DVE, c_['X0'][:], c_['tmpb'][:], K['i4'][:], ALU.add, r=[key(c_, 'tmpb')], w=[key(c_, 'X0')])
                p.tt(DVE, c_['tmpb'][:], c_['NTb'][:], K['lvl'][:, 1 - d, 0, :], ALU.mult, r=[key(c_, 'NTb'), key(c_, 'tmpb')], w=[key(c_, 'tmpb')])
                p.tt(DVE, c_['XT0'][:], c_['tmpb'][:], K['i4'][:], ALU.add, r=[key(c_, 'tmpb')], w=[key(c_, 'XT0')])

            def st_L1(c_, grp, li):
                xs = (li - 1) % 2
                X, XT = c_['X%d' % xs], c_['XT%d' % xs]
                for j4 in range(4):
                    sl = slice(j4 * 128, (j4 + 1) * 128)
                    p.mm(PS(c_['ba'])[:, sl], c_['NTl'][:, li - 1, sl], X[:, sl], True, True, r=[key(c_, 'NTl'), key(c_, 'X%d' % xs)], w=[PK(c_['ba'])])
                p.cp(ACT, c_['Yb'][:], PS(c_['ba'])[:, :], r=[PK(c_['ba'])], w=[key(c_, 'Yb')])

            def st_L2(c_, grp, li):
                xs = (li - 1) % 2
                X, XT = c_['X%d' % xs], c_['XT%d' % xs]
                Xn, XTn = c_['X%d' % (1 - xs)], c_['XT%d' % (1 - xs)]
                for j4 in range(4):
                    sl = slice(j4 * 128, (j4 + 1) * 128)
                    p.mm(PS(c_['ba'] + 1)[:, sl], XT[:, sl], c_['Yb'][:, sl], True, False, r=[key(c_, 'XT%d' % xs), key(c_, 'Yb')], w=[PK(c_['ba'] + 1)])
                    p.mm(PS(c_['ba'] + 1)[:, sl], p.ident_bf[:], X[:, sl], False, True, r=[key(c_, 'X%d' % xs)], w=[PK(c_['ba'] + 1)])
                if li < 6:
                    for j4 in range(4):
                        sl = slice(j4 * 128, (j4 + 1) * 128)
                        p.mm(PS(c_['ba'] + 2)[:, sl], c_['Yb'][:, sl], XT[:, sl], True, False, r=[key(c_, 'XT%d' % xs), key(c_, 'Yb')], w=[PK(c_['ba'] + 2)])
                        p.mm(PS(c_['ba'] + 2)[:, sl], p.ident_bf[:], XT[:, sl], False, True, r=[key(c_, 'XT%d' % xs)], w=[PK(c_['ba'] + 2)])
                p.cp(ACT, Xn[:], PS(c_['ba'] + 1)[:, :], r=[PK(c_['ba'] + 1)], w=[key(c_, 'X%d' % (1 - xs))])
                if li < 6:
                    p.cp(ACT if li % 2 == 0 else DVE, XTn[:], PS(c_['ba'] + 2)[:, :], r=[PK(c_['ba'] + 2)], w=[key(c_, 'XT%d' % (1 - xs))])

            def st_S1(c_, grp):
                hd0 = d * 8 + grp * 4
                hq0 = 4 + grp * 2
                p.tt(DVE, c_['kdec'][:].rearrange("q (a b c) -> q a b c", b=2, c=128),
                     qn[:, hq0:hq0 + 2, :].unsqueeze(2).to_broadcast([128, 2, 2, 128]),
                     g[:, 4, hd0:hd0 + 4].rearrange("q (a b) -> q a b", b=2).unsqueeze(3).to_broadcast([128, 2, 2, 128]), ALU.mult,
                     r=[('qkn', bi), gk], w=[key(c_, 'kdec')])
                for j4 in range(4):
                    hd = hd0 + j4
                    hv = grp * 4 + j4
                    sl = slice(j4 * 128, (j4 + 1) * 128)
                    So = Sb[sbi[hd]]
                    p.mm(PS(c_['ba'])[:, sl], kTb[:, hv // 2, :], So[:, hd, :], True, True, r=[('kT', bi), ('Sb', sbi[hd], hd)], w=[PK(c_['ba'])])
                p.tt(DVE, H4(c_['tmpf'][:]), H4(PS(c_['ba'])[:, :]), bci(g[:, 3, hd0:hd0 + 4]), ALU.mult, r=[PK(c_['ba']), gk], w=[key(c_, 'tmpf')])
                v0 = 1024 + grp * 512
                p.tt(DVE, c_['rb'][:], c_['tmpf'][:], qb[:, v0:v0 + 512], ALU.add, r=[key(c_, 'tmpf'), ('qkvb', bi)], w=[key(c_, 'rb')])

            def st_S2(c_, grp):
                hd0 = d * 8 + grp * 4
                X = c_['X0']
                for j4 in range(4):
                    sl = slice(j4 * 128, (j4 + 1) * 128)
                    p.mm(PS(c_['ba'] + 1)[:, sl], X[:, sl], c_['rb'][:, sl], True, True, r=[key(c_, 'X0'), key(c_, 'rb')], w=[PK(c_['ba'] + 1)])
                for j4 in range(4):
                    hd = hd0 + j4
                    sl = slice(j4 * 128, (j4 + 1) * 128)
                    p.act(c_['ub'][:, sl], PS(c_['ba'] + 1)[:, sl], AF.Copy, r=[PK(c_['ba'] + 1), 'bts'], w=[key(c_, 'ub')], scale=bts[:, t, hd:hd + 1])

            def st_S3(c_, grp):
                hd0 = d * 8 + grp * 4
                for j4 in range(4):
                    sl = slice(j4 * 128, (j4 + 1) * 128)
                    p.mm(PS(c_['ba'])[:, sl], c_['kdec'][:, sl], c_['ub'][:, sl], True, True, r=[key(c_, 'kdec'), key(c_, 'ub')], w=[PK(c_['ba'])])
                if need_out:
                    for j4 in range(4):
                        hd = hd0 + j4
                        hv = grp * 4 + j4
                        sl = slice(j4 * 128, (j4 + 1) * 128)
                        So = Sb[sbi[hd]]
                        p.mm(PS(c_['ba'] + 1)[:, sl], qTb[:, hv // 2, :], So[:, hd, :], True, True, r=[('qT', bi), ('Sb', sbi[hd], hd)], w=[PK(c_['ba'] + 1)])
                    for j4 in range(4):
                        hd = hd0 + j4
                        sl = slice(j4 * 128, (j4 + 1) * 128)
                        p.act(c_['o1'][:, sl], PS(c_['ba'] + 1)[:, sl], AF.Copy, r=[PK(c_['ba'] + 1), gk], w=[key(c_, 'o1')], scale=g[:, 2, hd:hd + 1])
                    for j4 in range(4):
                        sl = slice(j4 * 128, (j4 + 1) * 128)
                        p.mm(PS(c_['ba'] + 2)[:, sl], c_['qkp'][:, sl], c_['ub'][:, sl], True, True, r=[key(c_, 'qkp'), key(c_, 'ub')], w=[PK(c_['ba'] + 2)])
                    p.tt(DVE, obuf[:, grp * 512:(grp + 1) * 512], PS(c_['ba'] + 2)[:, :], c_['o1'][:], ALU.add, r=[PK(c_['ba'] + 2), key(c_, 'o1')],
                         w=[('ob', step % 2, grp)])
                hk = [('Sf', hd0 + j) for j in range(4)]
                p.tt(DVE, Sf[:, hd0:hd0 + 4, :], Sf[:, hd0:hd0 + 4, :], bci(g[:, 5, hd0:hd0 + 4]), ALU.mult, r=hk + [gk], w=hk)
                p.tt(DVE, Sf[:, hd0:hd0 + 4, :], Sf[:, hd0:hd0 + 4, :], H4(PS(c_['ba'])[:, :]), ALU.add, r=hk + [PK(c_['ba'])], w=hk)
                nb_ = 1 - sbi[hd0]
                p.cp(ACT, Sb[nb_][:, hd0:hd0 + 4, :], Sf[:, hd0:hd0 + 4, :], r=hk, w=[('Sb', nb_, hd0 + j) for j in range(4)])
                for j in range(4):
                    sbi[hd0 + j] = nb_

            stages = [st_D, st_E, st_NT]
            for li in range(1, 7):
                stages.append(lambda c_, grp, li=li: st_L1(c_, grp, li))
                stages.append(lambda c_, grp, li=li: st_L2(c_, grp, li))
            stages += [st_S1, st_S2, st_S3]
            for stg in stages:
                sl_ = []
                stage_ll.append(sl_)
                rec_into(sl_)
                for grp in range(2):
                    stg(CT[grp], grp)
            rec_into(tail_l)
            if need_out:
                p.dma(SP, p.Od[d][(t - OWN0) * 128:(t - OWN0 + 1) * 128, :], obuf[:], r=[('ob', step % 2, 0), ('ob', step % 2, 1)],
                      w=[('Od', d, t)])
            step += 1
            if p.debug in ('G0', 'G') and (t, d) == (32, 1):
                o = p.out("dbg_S", [128, 16, 128], F32)
                p.dma(SP, o, Sf[:], r=[('Sf', h) for h in range(16)])
        p.S.op = real_op
        for o_ in recs[0][0]:
            real_op(*o_)
        for i_, (prep_l, stage_ll, tail_l) in enumerate(recs):
            nxt = list(recs[i_ + 1][0]) if i_ + 1 < len(recs) else []
            per = -(-len(nxt) // max(1, len(stage_ll) - 2)) if nxt else 0
            for sl_ in stage_ll:
                for o_ in sl_:
                    real_op(*o_)
                for _ in range(per):
                    if nxt:
                        real_op(*nxt.pop(0))
            for o_ in nxt:
                real_op(*o_)
            for o_ in tail_l:
                real_op(*o_)
        p.S.barrier()
        stG.close()
        if p.debug == 'G':
            for d in range(2):
                o = p.out("dbg_O%d" % d, [16 * 128, 1024], F32)
                p.dma(SP, o, p.Od[d], r=[])


    def bcast_rows(p, dst, vec, key):
        dg = p.bc_dg
        for c in range(8):
            p.ts(DVE, dg[:, c * 128:(c + 1) * 128], p.ident_f[:], vec(c), None, ALU.mult, r=['modsb', 'vecs'], w=['bc_dg'])
        for hf_ in range(2):
            p.mm(p.psb[7][:, :], p.ones_f[:], dg[:, hf_ * 512:(hf_ + 1) * 512], True, True, r=['bc_dg'], w=[('ps', 7)])
            p.cp(ACT, dst[:, hf_ * 512:(hf_ + 1) * 512], p.psb[7][:, :], r=[('ps', 7)], w=[key])

    def phaseD(p):
        x = p.inp("x_seq", [L, D], F32) if "x_seq" not in p.din else p.din["x_seq"]
        w_in = p.din["w_in"]
        tabs = p.inp("dft_tab", [4, 4, 128, 2, 8, 512], BF16)
        cdft = p.inp("cdft", [128, 2, 128], BF16)
        gng = p.inp("gng_t", [1024], F32)
        wfo = p.inp("w_fo", [512, D], F32)
        wgo = p.inp("w_go", [D, D], F32)
        wmo = p.inp("w_mo", [D, D], F32)
        wr = p.inp("w_router", [D, NE], F32)
        br = p.inp("b_router", [NE], F32)
        p.X1 = p.scratch("X1", [2048, D], F32)
        p.H = p.scratch("H", [2049, D], BF16)
        p.logits = p.sb("logits", [128, 16, NE], F32)
        p.g2_b = p.sb("g2_b", [128, 1024], F32)
        stD = contextlib.ExitStack()
        p.bc_dg = p.sb("bc_dg", [128, 1024], F32, stD)
        p.bcast_rows(p.g2_b, lambda c: p.modsb[:, 40 + c, 0:1], 'g2_b')
        fmT = p.sb("fmT", [128, 4, 2048], BF16, stD)
        wv = w_in.rearrange("(kc q) n -> q kc n", q=128)
        Wzg = p.sb("Wzg", [128, 8, 3072], BF16, stD)
        for kc in range(8):
            p.dma(POOL, Wzg[:, kc, 0:1024], wv[:, kc, Z_OFF:Z_OFF + 1024], w=['Wzg'])
            p.dma(POOL, Wzg[:, kc, 1024:3072], wv[:, kc, GA_OFF:GA_OFF + 2048], w=['Wzg'])
        Wfo = p.sb("Wfo", [128, 4, D], BF16, stD)
        p.dma(POOL, Wfo[:], wfo.rearrange("(kc q) n -> q kc n", q=128), w=['Wfo'])
        Wgo = p.sb("Wgo", [128, 8, D], BF16, stD)
        p.dma(POOL, Wgo[:], wgo.rearrange("(kc q) n -> q kc n", q=128), w=['Wgo'])
        Wmo = p.sb("Wmo", [128, 8, D], BF16, stD)
        p.dma(POOL, Wmo[:], wmo.rearrange("(kc q) n -> q kc n", q=128), w=['Wmo'])
        Wr = p.sb("Wr", [128, 8, NE], F32, stD)
        p.dma(SP, Wr[:], wr.rearrange("(kc q) n -> q kc n", q=128), w=['Wr'])
        brb = p.sb("brb", [128, NE], F32, stD)
        p.dma(SP, brb[:], br.partition_broadcast(128), w=['brb'])
        gnb = p.sb("gnb", [128, 1024], F32, stD)
        p.dma(SP, gnb[:], gng.partition_broadcast(128), w=['gnb'])
        st1 = contextlib.ExitStack()
        Xs = p.sb("Xs", [128, 32, 512], BF16, st1)
        for lc in range(32):
            p.dma(SP, Xs[:, lc, :], p.XF[lc * 128:(lc + 1) * 128, :], w=[('Xs', lc)])
        tb = [p.sb("tb%d" % i, [128, 2, 8, 512], BF16, st1) for i in range(2)]
        cd = p.sb("cd", [128, 2, 128], BF16, st1)
        p.dma(SP, cd[:], cdft, w=['cd'])
        AB = p.sb("AB", [128, 8, 512], BF16, st1)
        SC = float(1.0 / np.sqrt(4096.0 * 128.0))
        nl = 0
        for kt in range(4):
            for q4 in range(4):
                tbuf = tb[nl % 2]
                tk = ('tb', nl % 2)
                nl += 1
                p.dma(SP, tbuf[:], tabs[kt, q4], w=[tk])
                for lc in range(8):
                    la = q4 * 8 + lc
                    for g_ in range(4):
                        for ab in range(2):
                            p.mm(p.psb[g_ * 2 + ab][:, :], Xs[:, la, g_ * 128:(g_ + 1) * 128], tbuf[:, ab, lc, :], la == 0, la == 31,
                                 r=[('Xs', la), tk], w=[('ps', g_ * 2 + ab)])
            for i8 in range(8):
                p.cp(ACT if i8 % 2 == 0 else DVE, AB[:, i8, :], p.psb[i8][:, :], r=[('ps', i8)], w=[('AB', i8)])
            for g_ in range(4):
                p.mm(p.psb[g_][:, :], cd[:, 0, :], AB[:, g_ * 2, :], True, False, r=['cd', ('AB', g_ * 2)], w=[('ps', g_)])
                p.mm(p.psb[g_][:, :], cd[:, 1, :], AB[:, g_ * 2 + 1, :], False, True, r=['cd', ('AB', g_ * 2 + 1)], w=[('ps', g_)])
                p.act(fmT[:, g_, kt * 512:(kt + 1) * 512], p.psb[g_][:, :], AF.Copy, r=[('ps', g_)], w=[('fmT', g_, kt)], scale=SC)
        p.S.barrier()
        st1.close()
        if p.debug == 'D1':
            o = p.out("dbg_fmT", [128, 4, 2048], BF16)
            p.dma(SP, o, fmT[:], r=[])
            p.S.barrier(); stD.close()
            return
        st2 = stD
        g1_b = p.sb("g1_b", [128, 1024], F32, st2)
        gs2_b = p.sb("gs2_b", [128, 1024], F32, st2)
        sh2_b = p.sb("sh2_b", [128, 1024], F32, st2)
        p.bcast_rows(g1_b, lambda c: p.modsb[:, 16 + c, 0:1], 'g1_b')
        p.bcast_rows(gs2_b, lambda c: p.vecs[:, 2, c:c + 1], 'gs2_b')
        p.bcast_rows(sh2_b, lambda c: p.modsb[:, 24 + c, 0:1], 'sh2_b')
        zrow = p.sb("zrow", [1, D], BF16, st2)
        p.S.op(DVE, lambda e: e.memset(zrow[:], 0.0), [], ['zrow'])
        p.dma(SP, p.H[2048:2049, :], zrow[:], r=['zrow'], w=[('H', 'z')])
        p.S.barrier()
        uT = p.sb("uTd", [128, 8, 512], BF16, st2)
        ybT = p.sb("ybT", [128, 8, 512], BF16, st2)
        mT = p.sb("mT", [128, 8, 512], BF16, st2)
        bufs = {
            'x': (p.sb("xtd", [128, D], F32, st2), 'xtd'),
            'junk': (p.sb("junkd", [128, D], BF16, st2), 'junkd'),
            'ss': (p.sb("ssd", [128, 4], F32, st2), 'ssd'),
            'xn': (p.sb("xnd", [128, D], BF16, st2), 'xnd'),
            'ps': (p.psb[0], ('ps', 0)),
        }
        zs = p.sb("zs", [128, D], F32, st2)
        of_ = p.sb("of_", [128, D], F32, st2)
        ob_ = p.sb("ob_", [128, D], F32, st2)
        on8 = p.sb("on8", [128, 4, 8], F32, st2)
        ybin = p.sb("ybin", [128, D], BF16, st2)
        gw = [p.sb("gw%d" % i, [128, 512], BF16, st2) for i in range(4)]
        x1 = p.sb("x1", [128, D], F32, st2)
        xt2 = p.sb("xt2d", [128, D], F32, st2)
        h2 = p.sb("h2", [128, D], F32, st2)
        h2b = p.sb("h2b", [128, D], BF16, st2)
        h2T = p.sb("h2T", [128, 8, 128], F32, st2)
        s2 = p.sb("s2", [128, 4], F32, st2)
        real_op = p.S.op
        recD = []

        def rec_into(lst):
            p.S.op = lambda eng, fn, reads=(), writes=(), dma=False: lst.append((eng, fn, list(reads), list(writes), dma))

        for sti in range(4):
            head_l, mid_l, tail_l = [], [], []
            recD.append((head_l, mid_l, tail_l))
            rec_into(head_l)
            for ti in range(4):
                tt_ = sti * 4 + ti
                t = OWN0 + tt_
                p.norm_T(x[t * 128:(t + 1) * 128, :], p.gs1, p.sh1, lambda c: (uT[:, c, ti * 128:(ti + 1) * 128], ('uTd', ti)), bufs, 'd')
                for hf_ in range(2):
                    ps = p.psb[1 + hf_]
                    for kc in range(8):
                        p.mm(ps[:, :], uT[:, kc, ti * 128:(ti + 1) * 128], Wzg[:, kc, hf_ * 512:(hf_ + 1) * 512], kc == 0, kc == 7,
                             r=[('uTd', ti), 'Wzg'], w=[('ps', 1 + hf_)])
                    sl = slice(hf_ * 512, (hf_ + 1) * 512)
                    p.act(zs[:, sl], ps[:, :], AF.Silu, r=[('ps', 1 + hf_)], w=[('zs', hf_)])
                p.dma(SP, of_[:], p.Od[0][tt_ * 128:(tt_ + 1) * 128, :], w=['of_'])
                p.dma(SP, ob_[:], p.Od[1][tt_ * 128:(tt_ + 1) * 128, :], w=['ob_'])
                p.tt(DVE, of_[:], of_[:], ob_[:], ALU.add, r=['of_', 'ob_'], w=['of_'])
                p.tt(DVE, ob_[:], of_[:], of_[:], ALU.mult, r=['of_', 'ob_'], w=['ob_'])
                p.S.op(DVE, lambda e: e.tensor_reduce(on8[:, 0, :], ob_[:].rearrange("q (h c) -> q h c", c=128), AX.X, ALU.add), ['ob_'], ['on8'])
                p.ts(DVE, on8[:, 1, :], on8[:, 0, :], 1.0 / 128, EPS, ALU.mult, ALU.add, r=['on8'], w=['on8'])
                p.act(on8[:, 2, :], on8[:, 1, :], AF.Ln, r=['on8'], w=['on8'])
                p.act(on8[:, 3, :], on8[:, 2, :], AF.Exp, r=['on8'], w=['on8'], scale=-0.5)
                for h in range(8):
                    hs = slice(h * 128, (h + 1) * 128)
                    p.stt(DVE, of_[:, hs], of_[:, hs], on8[:, 3, h:h + 1], gnb[:, hs], ALU.mult, ALU.mult, r=['of_', 'on8', 'gnb'], w=['of_'])
                p.tt(DVE, ybin[:], of_[:], zs[:], ALU.mult, r=['of_', ('zs', 0), ('zs', 1)], w=['ybin'])
                pv = p.psb[3][:].bitcast(BF16)
                for c in range(8):
                    p.tr(pv[:, c * 128:(c + 1) * 128], ybin[:, c * 128:(c + 1) * 128], p.ident_bf[:], r=['ybin'], w=[('ps', 3)])
                p.cp(ACT, ybT[:, :, ti * 128:(ti + 1) * 128], pv[:, :].rearrange("q (c k) -> q c k", k=128), r=[('ps', 3)], w=[('ybT', ti)])
            rec_into(mid_l)
            allu = [('uTd', i) for i in range(4)]
            ally = [('ybT', i) for i in range(4)]
            k0 = sti * 512
            for dc in range(8):
                dsl = slice(dc * 128, (dc + 1) * 128)
                b0 = 4 * (dc % 2)
                for fc in range(4):
                    p.mm(p.psb[b0][:, :], Wfo[:, fc, dsl], fmT[:, fc, k0:k0 + 512], fc == 0, fc == 3, r=['Wfo'], w=[('ps', b0)])
                for fc in range(8):
                    p.mm(p.psb[b0 + 1][:, :], Wgo[:, fc, dsl], ybT[:, fc, :], fc == 0, fc == 7, r=['Wgo'] + ally, w=[('ps', b0 + 1)])
                for kc in range(8):
                    p.mm(p.psb[b0 + 2][:, :], Wzg[:, kc, 1024 + dc * 128:1024 + (dc + 1) * 128], uT[:, kc, :], kc == 0, kc == 7, r=['Wzg'] + allu, w=[('ps', b0 + 2)])
                for kc in range(8):
                    p.mm(p.psb[b0 + 3][:, :], Wzg[:, kc, 2048 + dc * 128:2048 + (dc + 1) * 128], uT[:, kc, :], kc == 0, kc == 7, r=['Wzg'] + allu, w=[('ps', b0 + 3)])
                for br_, (pg, py) in enumerate([(b0 + 2, b0), (b0 + 3, b0 + 1)]):
                    gb_ = gw[(dc % 2) * 2 + br_]
                    gk_ = ('gw', (dc % 2) * 2 + br_)
                    p.act(gb_[:], p.psb[pg][:, :], AF.Sigmoid, r=[('ps', pg)], w=[gk_])
                    p.tt(DVE, gb_[:], gb_[:], p.psb[py][:, :], ALU.mult, r=[gk_, ('ps', py)], w=[gk_])
                p.tt(DVE, mT[:, dc, :], gw[(dc % 2) * 2][:], gw[(dc % 2) * 2 + 1][:], ALU.add, r=[('gw', (dc % 2) * 2), ('gw', (dc % 2) * 2 + 1)], w=[('mT', dc)])
            allm = [('mT', i) for i in range(8)]
            rec_into(tail_l)
            for ti in range(4):
                tt_ = sti * 4 + ti
                t = OWN0 + tt_
                xt, xk = xt2, 'xt2'
                p.dma(SP, xt[:], x[t * 128:(t + 1) * 128, :], w=[xk])
                for hf_ in range(2):
                    ps = p.psb[4 + hf_]
                    sl = slice(hf_ * 512, (hf_ + 1) * 512)
                    for dc in range(8):
                        p.mm(ps[:, :], mT[:, dc, ti * 128:(ti + 1) * 128], Wmo[:, dc, sl], dc == 0, dc == 7, r=allm + ['Wmo'], w=[('ps', 4 + hf_)])
                    p.tt(DVE, x1[:, sl], ps[:, :], g1_b[:, sl], ALU.mult, r=[('ps', 4 + hf_)], w=['x1'])
                p.tt(DVE, x1[:], x1[:], xt[:], ALU.add, r=['x1', xk], w=['x1'])
                p.dma(POOL, p.X1[tt_ * 128:(tt_ + 1) * 128, :], x1[:], r=['x1'], w=[('X1', tt_)])
                p.act(h2[:], x1[:], AF.Square, r=['x1'], w=['h2', 's2'], accum_out=s2[:, 0:1])
                p.act(s2[:, 2:3], s2[:, 0:1], AF.Ln, r=['s2'], w=['s2'], scale=1.0 / D, bias=p.eps_t[:, 0:1])
                p.act(s2[:, 3:4], s2[:, 2:3], AF.Exp, r=['s2'], w=['s2'], scale=-0.5)
                p.stt(DVE, h2[:], x1[:], s2[:, 3:4], gs2_b[:], ALU.mult, ALU.mult, r=['x1', 's2', 'h2'], w=['h2'])
                p.tt(DVE, h2[:], h2[:], sh2_b[:], ALU.add, r=['h2'], w=['h2'])
                p.cp(ACT, h2b[:], h2[:], r=['h2'], w=['h2b'])
                p.dma(POOL, p.H[tt_ * 128:(tt_ + 1) * 128, :], h2b[:], r=['h2b'], w=[('H', tt_)])
                for hf_ in range(2):
                    for c in range(4):
                        p.tr(p.psb[6][:, c * 128:(c + 1) * 128], h2[:, (hf_ * 4 + c) * 128:(hf_ * 4 + c + 1) * 128], p.ident_f[:], r=['h2'], w=[('ps', 6)])
                    p.cp(ACT, h2T[:, hf_ * 4:(hf_ + 1) * 4, :], p.psb[6][:, :].rearrange("q (c k) -> q c k", k=128), r=[('ps', 6)], w=['h2T'])
                for kc in range(8):
                    p.mm(p.psb[7][:, 0:NE], h2T[:, kc, :], Wr[:, kc, :], kc == 0, kc == 7, r=['h2T', 'Wr'], w=[('ps', 7)])
                p.tt(DVE, p.logits[:, tt_, :], p.psb[7][:, 0:NE], brb[:], ALU.add, r=[('ps', 7), 'brb'], w=[('logits', tt_)])
        p.S.op = real_op
        for o_ in recD[0][0]:
            real_op(*o_)
        for i_, (head_l, mid_l, tail_l) in enumerate(recD):
            for o_ in mid_l:
                real_op(*o_)
            nxt = list(recD[i_ + 1][0]) if i_ + 1 < len(recD) else []
            tl = list(tail_l)
            CH = 6
            while tl or nxt:
                for _ in range(CH):
                    if tl:
                        real_op(*tl.pop(0))
                for _ in range(CH):
                    if nxt:
                        real_op(*nxt.pop(0))
        p.S.barrier()
        stD.close()
        if p.debug == 'D':
            o = p.out("dbg_x1", [2048, D], F32)
            p.dma(SP, o, p.X1, r=[])
            o = p.out("dbg_logits", [128, 16, NE], F32)
            p.dma(SP, o, p.logits[:], r=[])
            o = p.out("dbg_H", [2049, D], BF16)
            p.dma(SP, o, p.H, r=[])


    def phaseE(p):
        CAPS = MOE_CAPS
        TB = [sum(CAPS[:i]) for i in range(NE)]
        NB = sum(CAPS)
        DUMMY = NB * 128
        CAPMAX = max(CAPS) * 128
        wg = p.inp("w_gate", [NE, D, D], F32)
        wu = p.inp("w_up", [NE, D, D], F32)
        wd = p.inp("w_down", [NE, D, D], F32)
        bg = p.inp("b_gate", [NE, D], F32)
        bu = p.inp("b_up", [NE, D], F32)
        bd = p.inp("b_down", [NE, D], F32)
        fng = p.inp("fng", [D], F32)
        yout = p.out("y", [2048, D], F32)
        Y = p.scratch("Yslots", [NB * 128 + 128, D], F32)
        IDX = p.scratch("IDX", [NB * 128 + 128, 1], I32)
        stE = contextlib.ExitStack()
        K = {}
        for nm, dt_ in [('ones_bf', BF16), ('ustrict_bf', BF16), ('iota32', F32), ('ecol', F32), ('blk128', F32),
                        ('tokid', I32), ('l_strict', F32), ('basetab', F32), ('captab', F32), ('rowoff', F32)]:
            a = p.const_np[nm]
            d = p.inp("c_" + nm, a.shape, dt_) if ("c_" + nm) not in p.din else p.din["c_" + nm]
            K[nm] = p.sb("ke_" + nm, a.shape, dt_, stE)
            p.dma(SP, K[nm][:], d, w=[('k', nm)])
        Bg = p.sb("Bg", [32, D], BF16, stE)
        Bu = p.sb("Bu", [32, D], BF16, stE)
        Bd = p.sb("Bd", [32, D], BF16, stE)
        p.dma(POOL, Bg[:], bg, w=['Bg'])
        p.dma(POOL, Bu[:], bu, w=['Bu'])
        p.dma(POOL, Bd[:], bd, w=['Bd'])
        fnb = p.sb("fnb", [128, D], F32, stE)
        p.dma(SP, fnb[:], fng.partition_broadcast(128), w=['fnb'])
        i2048 = p.sb("i2048", [128, NB], I32, stE)
        p.S.op(DVE, lambda e: e.memset(i2048[:], 2048), [], ['i2048'])
        p.dma(SP, IDX[0:NB * 128, :].rearrange("(q b) o -> q (b o)", q=128), i2048[:], r=['i2048'], w=['IDX'])
        zy = p.sb("zy", [128, D], F32, stE)
        p.S.op(DVE, lambda e: e.memset(zy[:], 0.0), [], ['zy'])
        p.dma(SP, Y[DUMMY:DUMMY + 128, :], zy[:], r=['zy'], w=['Yz'])
        p.S.barrier()
        mx = p.sb("mx", [128, 16, 8], F32, stE)
        mi = p.sb("mi", [128, 16, 8], U32, stE)
        idf = p.sb("idf", [128, 16, 4], F32, stE)
        wts = p.sb("wts", [128, 16, 4], F32, stE)
        nm0 = p.sb("nm0", [128, 16], F32, stE)
        ssum = p.sb("ssum", [128, 16], F32, stE)
        Mf = p.sb("Mf", [128, 16, NE], F32, stE)
        Mb = p.sb("Mb", [128, 16, NE], BF16, stE)
        for tt_ in range(16):
            p.S.op(DVE, lambda e, tt_=tt_: e.max(mx[:, tt_, :], p.logits[:, tt_, :]), [], [('mx', tt_)])
            p.S.op(DVE, lambda e, tt_=tt_: e.max_index(mi[:, tt_, :], mx[:, tt_, :], p.logits[:, tt_, :]), [('mx', tt_)], [('mi', tt_)])
            p.cp(DVE, idf[:, tt_, :], mi[:, tt_, 0:4], r=[('mi', tt_)], w=[('idf', tt_)])
            p.ts(DVE, nm0[:, tt_:tt_ + 1], mx[:, tt_, 0:1], -1.0, None, ALU.mult, r=[('mx', tt_)], w=[('nm0', tt_)])
            p.act(wts[:, tt_, :], mx[:, tt_, 0:4], AF.Exp, r=[('mx', tt_), ('nm0', tt_)], w=[('wts', tt_)], bias=nm0[:, tt_:tt_ + 1], scale=1.0)
            p.S.op(DVE, lambda e, tt_=tt_: e.tensor_reduce(ssum[:, tt_:tt_ + 1], wts[:, tt_, :], AX.X, ALU.add), [('wts', tt_)], [('ssum', tt_)])
            p.S.op(DVE, lambda e, tt_=tt_: e.reciprocal(ssum[:, tt_:tt_ + 1], ssum[:, tt_:tt_ + 1]), [('ssum', tt_)], [('ssum', tt_)])
            p.ts(DVE, wts[:, tt_, :], wts[:, tt_, :], ssum[:, tt_:tt_ + 1], None, ALU.mult, r=[('wts', tt_), ('ssum', tt_)], w=[('wts', tt_)])
            p.ts(DVE, Mf[:, tt_, :], K['iota32'][:], idf[:, tt_, 0:1], None, ALU.is_equal, r=[('idf', tt_)], w=[('Mf', tt_)])
            for j in range(1, 4):
                p.stt(DVE, Mf[:, tt_, :], K['iota32'][:], idf[:, tt_, j:j + 1], Mf[:, tt_, :], ALU.is_equal, ALU.add, r=[('idf', tt_), ('Mf', tt_)], w=[('Mf', tt_)])
            p.cp(DVE, Mb[:, tt_, :], Mf[:, tt_, :], r=[('Mf', tt_)], w=[('Mb', tt_)])
        allM = [('Mb', i) for i in range(16)]
        for tt_ in range(16):
            p.mm(p.psb[0][:, 0:NE], K['ones_bf'][:], Mb[:, tt_, :], tt_ == 0, tt_ == 15, r=allM, w=[('ps', 0)])
        for tt_ in range(16):
            p.mm(p.psb[1][0:32, 0:1], Mb[:, tt_, :], K['ones_bf'][:, 0:1], tt_ == 0, tt_ == 15, r=allM, w=[('ps', 1)])
        cntb = p.sb("cntb", [128, NE], F32, stE)
        cc = p.sb("cc", [32, 8], F32, stE)
        sq32 = p.sb("sq32", [32, 3, NE], F32, stE)
        Pm = p.sb("Pm", [32, NE], F32, stE)
        p.cp(DVE, cntb[:], p.psb[0][:, 0:NE], r=[('ps', 0)], w=['cntb'])
        p.cp(DVE, cc[:, 0:1], p.psb[1][0:32, 0:1], r=[('ps', 1)], w=['cc'])
        p.ts(DVE, sq32[:, 0, :], cntb[0:32, :], cc[:, 0:1], None, ALU.is_gt, r=['cntb', 'cc'], w=['sq32'])
        p.ts(DVE, sq32[:, 1, :], cntb[0:32, :], cc[:, 0:1], None, ALU.is_equal, r=['cntb', 'cc'], w=['sq32'])
        p.tt(DVE, sq32[:, 1, :], sq32[:, 1, :], K['l_strict'][0:32, 0:32], ALU.mult, r=['sq32'], w=['sq32'])
        p.tt(DVE, sq32[:, 0, :], sq32[:, 0, :], sq32[:, 1, :], ALU.add, r=['sq32'], w=['sq32'])
        p.S.op(DVE, lambda e: e.tensor_reduce(cc[:, 1:2], sq32[:, 0, :], AX.X, ALU.add), ['sq32'], ['cc'])
        p.ts(DVE, Pm[:], K['iota32'][0:32, :], cc[:, 1:2], None, ALU.is_equal, r=['cc'], w=['Pm'])
        p.tt(DVE, sq32[:, 0, :], Pm[:], K['basetab'][0:32, :], ALU.mult, r=['Pm', 'sq32'], w=['sq32'])
        p.S.op(DVE, lambda e: e.tensor_reduce(cc[:, 2:3], sq32[:, 0, :], AX.X, ALU.add), ['sq32'], ['cc'])
        p.tt(DVE, sq32[:, 1, :], Pm[:], K['captab'][0:32, :], ALU.mult, r=['Pm', 'sq32'], w=['sq32'])
        p.S.op(DVE, lambda e: e.tensor_reduce(cc[:, 3:4], sq32[:, 1, :], AX.X, ALU.add), ['sq32'], ['cc'])
        lb = p.sb("lb", [32, 3, 128], F32, stE)
        one32f = p.sb("one32f", [32, 128], F32, stE)
        p.S.op(DVE, lambda e: e.memset(one32f[:], 1.0), [], ['one32f'])
        p.ts(DVE, lb[:, 0, :], one32f[:], cc[:, 2:3], None, ALU.mult, r=['one32f', 'cc'], w=['lb'])
        p.ts(DVE, lb[:, 1, :], one32f[:], cc[:, 3:4], None, ALU.mult, r=['one32f', 'cc'], w=['lb'])
        p.ts(DVE, lb[:, 2, :], one32f[:], K['ecol'][0:32, 0:1], None, ALU.mult, r=['one32f'], w=['lb'])
        p.mm(p.psb[0][:, 0:NE], lb[:, 0, :], p.ident_f[0:32, 0:32], True, True, r=['lb'], w=[('ps', 0)])
        p.mm(p.psb[0][:, 32:64], lb[:, 1, :], p.ident_f[0:32, 0:32], True, True, r=['lb'], w=[('ps', 0)])
        p.mm(p.psb[0][:, 64:96], lb[:, 2, :], Pm[:], True, True, r=['lb', 'Pm'], w=[('ps', 0)])
        bcb = p.sb("bcb", [128, 3, NE], F32, stE)
        p.ts(DVE, bcb[:, 0, :], p.psb[0][:, 0:NE], -float(DUMMY), None, ALU.add, r=[('ps', 0)], w=['bcb'])
        p.cp(DVE, bcb[:, 1, :], p.psb[0][:, 32:64], r=[('ps', 0)], w=['bcb'])
        p.ts(DVE, bcb[:, 2, :], p.psb[0][:, 64:96], 128.0, None, ALU.mult, r=[('ps', 0)], w=['bcb'])
        idwf = p.sb("idwf", [128, NE], F32, stE)
        idw = p.sb("idw", [128, NE], I32, stE)
        p.ts(DVE, idwf[:], bcb[:, 2, :], K['rowoff'][:, 0:1], None, ALU.add, r=['bcb'], w=['idwf'])
        p.cp(DVE, idw[:], idwf[:], r=['idwf'], w=['idw'])
        rk = p.sb("rk", [128, NE], F32, stE)
        sel = p.sb("sel", [128, NE], F32, stE)
        destf = p.sb("destf", [128, 16, 4], F32, stE)
        desti = p.sb("desti", [128, 16, 4], I32, stE)
        for tt_ in range(16):
            ps = p.psb[2 + tt_ % 2]
            pk = ('ps', 2 + tt_ % 2)
            for t2 in range(tt_):
                p.mm(ps[:, 0:NE], K['ones_bf'][:], Mb[:, t2, :], t2 == 0, False, r=allM, w=[pk])
            p.mm(ps[:, 0:NE], K['ustrict_bf'][:], Mb[:, tt_, :], tt_ == 0, True, r=allM, w=[pk])
            p.tt(DVE, sel[:], ps[:, 0:NE], bcb[:, 1, :], ALU.is_lt, r=[pk, 'bcb'], w=['sel'])
            p.tt(DVE, rk[:], ps[:, 0:NE], bcb[:, 0, :], ALU.add, r=[pk, 'bcb'], w=['rk'])
            p.tt(DVE, rk[:], rk[:], sel[:], ALU.mult, r=['rk', 'sel'], w=['rk'])
            p.ts(DVE, rk[:], rk[:], float(DUMMY), None, ALU.add, r=['rk'], w=['rk'])
            for j in range(4):
                p.ts(DVE, sel[:], K['iota32'][:], idf[:, tt_, j:j + 1], None, ALU.is_equal, r=[('idf', tt_)], w=['sel'])
                p.tt(DVE, sel[:], sel[:], rk[:], ALU.mult, r=['sel', 'rk'], w=['sel'])
                p.S.op(DVE, lambda e, tt_=tt_, j=j: e.tensor_reduce(destf[:, tt_, j:j + 1], sel[:], AX.X, ALU.add), ['sel'], [('destf', tt_)])
            p.cp(DVE, desti[:, tt_, :], destf[:, tt_, :], r=[('destf', tt_)], w=[('desti', tt_)])
            for j in range(4):
                p.S.op(POOL, lambda e, tt_=tt_, j=j: e.indirect_dma_start(
                    out=IDX[:, :], out_offset=bass.IndirectOffsetOnAxis(ap=desti[:, tt_, j:j + 1], axis=0),
                    in_=K['tokid'][:, tt_:tt_ + 1], in_offset=None), [('desti', tt_), 'IDX0'], [('IDXs', tt_, j)], dma=True)
        p.S.barrier()
        idx_sb = p.sb("idx_sb", [128, NB], I32, stE)
        p.dma(SP, idx_sb[:], IDX[0:NB * 128, :].rearrange("(b q) o -> q (b o)", q=128), w=['idx_sb'], allow_slow_non_contiguous=True)
        p.S.barrier()
        stX = contextlib.ExitStack()
        Wb = [[p.sb("W%s%d" % (n_, i), [128, 8, D], BF16, stX) for n_ in "gud"] for i in range(2)]
        xg = [p.sb("xg%d" % i, [128, D], BF16, stX) for i in range(2)]
        xT = p.sb("xTe", [128, 8, CAPMAX], BF16, stX)
        aT = p.sb("aT", [128, 8, CAPMAX], BF16, stX)
        ohb = p.sb("ohb", [32, 512], BF16, stX)
        ones32 = p.sb("ones32", [32, 512], BF16, stX)
        p.S.op(DVE, lambda e: e.memset(ones32[:], 1.0), [], ['ones32'])
        wk = [[p.sb("wk%d_%d" % (i, j), [128, 512], BF16, stX) for j in range(4)] for i in range(2)]
        ysb = [p.sb("ysb%d" % i, [128, D], F32, stX) for i in range(2)]
        w2d = [w_.rearrange("e (q j) n -> (e q) (j n)", j=8) for w_ in (wg, wu, wd)]
        ng_ = 0
        nd_ = 0
        nch = 0
        for ex in range(NE):
            wbi = ex % 2
            capt = CAPS[ex]
            for wi_ in range(3):
                p.S.op(POOL, lambda e, wbi=wbi, wi_=wi_, ex=ex: e.indirect_dma_start(
                    out=Wb[wbi][wi_][:].rearrange("q j n -> q (j n)"), out_offset=None, in_=w2d[wi_][:, :],
                    in_offset=bass.IndirectOffsetOnAxis(ap=idw[:, ex:ex + 1], axis=0)), ['idw'], [('W', wbi, wi_)], dma=True)
            p.ts(DVE, ohb[:], ones32[:], Pm[:, ex:ex + 1], None, ALU.mult, r=['ones32', 'Pm'], w=['ohb'])
            for k in range(capt):
                b = TB[ex] + k
                gi = ng_ % 2
                ng_ += 1
                p.S.op(POOL, lambda e, b=b, gi=gi: e.indirect_dma_start(
                    out=xg[gi][:, :], out_offset=None, in_=p.H[:, :],
                    in_offset=bass.IndirectOffsetOnAxis(ap=idx_sb[:, b:b + 1], axis=0)), ['idx_sb'], [('xg', gi)], dma=True)
                pb_ = 0 if k % 2 == 0 else 7
                pv = p.psb[pb_][:].bitcast(BF16)
                xgv = xg[gi][:].rearrange("s (q j) -> s j q", j=8)
                for c in range(8):
                    p.tr(pv[:, c * 128:(c + 1) * 128], xgv[:, c, :], p.ident_bf[:], r=[('xg', gi)], w=[('ps', pb_)])
                p.cp(ACT if k % 2 == 0 else DVE, xT[:, :, k * 128:(k + 1) * 128], pv[:, :].rearrange("q (c k) -> q c k", k=128),
                     r=[('ps', pb_)], w=[('xTe', k)])
            allx = [('xTe', k) for k in range(capt)]
            nsl = capt * 128
            chunks = [(c0, min(512, nsl - c0)) for c0 in range(0, nsl, 512)]
            for fc in range(8):
                fs = slice(fc * 128, (fc + 1) * 128)
                for (c0, n_) in chunks:
                    st_ = nch % 2
                    nch += 1
                    bG, bU = 1 + 2 * st_, 2 + 2 * st_
                    for (wi_, Bt, bk) in ((0, Bg, bG), (1, Bu, bU)):
                        for kc in range(8):
                            p.mm(p.psb[bk][:, 0:n_], Wb[wbi][wi_][:, kc, :].rearrange("q (f j) -> q j f", j=8)[:, fc, :], xT[:, kc, c0:c0 + n_],
                                 kc == 0, False, r=[('W', wbi, wi_)] + allx, w=[('ps', bk)])
                        p.mm(p.psb[bk][:, 0:n_], Bt[:].rearrange("e (f j) -> e j f", j=8)[:, fc, :], ohb[:, 0:n_], False, True, r=['ohb'], w=[('ps', bk)])
                    g_, sg_, u_, t_ = wk[st_]
                    wkk = lambda j: ('wk', st_, j)
                    p.ts(DVE, g_[:, 0:n_], p.psb[bG][:, 0:n_], 7.0, None, ALU.min, r=[('ps', bG)], w=[wkk(0)])
                    p.act(sg_[:, 0:n_], g_[:, 0:n_], AF.Sigmoid, r=[wkk(0)], w=[wkk(1)], scale=1.702)
                    p.ts(DVE, u_[:, 0:n_], p.psb[bU][:, 0:n_], 7.0, -7.0, ALU.min, ALU.max, r=[('ps', bU)], w=[wkk(2)])
                    p.stt(DVE, t_[:, 0:n_], u_[:, 0:n_], 1.0, g_[:, 0:n_], ALU.add, ALU.mult, r=[wkk(2), wkk(0)], w=[wkk(3)])
                    p.tt(DVE, aT[:, fc, c0:c0 + n_], t_[:, 0:n_], sg_[:, 0:n_], ALU.mult, r=[wkk(3), wkk(1)], w=[('aT', fc)])
            alla = [('aT', fc) for fc in range(8)]
            for k in range(capt):
                b = TB[ex] + k
                yi = nd_ % 2
                nd_ += 1
                yb_ = ysb[yi]
                for hf_ in range(2):
                    hs = slice(hf_ * 512, (hf_ + 1) * 512)
                    pb_ = 5 if hf_ == 0 else 6
                    for fc in range(8):
                        p.mm(p.psb[pb_][:, :], aT[:, fc, k * 128:(k + 1) * 128], Wb[wbi][2][:, fc, hs], fc == 0, False, r=alla + [('W', wbi, 2)], w=[('ps', pb_)])
                    p.mm(p.psb[pb_][:, :], ohb[:, 0:128], Bd[:, hs], False, True, r=['ohb'], w=[('ps', pb_)])
                    p.cp(ACT, yb_[:, hs], p.psb[pb_][:, :], r=[('ps', pb_)], w=[('ysb', yi)])
                p.dma(SP, Y[b * 128:(b + 1) * 128, :], yb_[:], r=[('ysb', yi)], w=[('Y', b)])
        p.S.barrier()
        stX.close()
        yg = [p.sb("yg%d" % i, [128, D], F32, stE) for i in range(4)]
        acc = p.sb("acc", [128, D], F32, stE)
        x1ts = [p.sb("x1t%d" % i, [128, D], F32, stE) for i in range(2)]
        outb = [p.sb("outb%d" % i, [128, D], F32, stE) for i in range(2)]
        p.dma(SP, x1ts[0][:], p.X1[0:128, :], w=[('x1t', 0)])
        fs_ = p.sb("fs_", [128, 4], F32, stE)
        ng = 0
        for tt_ in range(16):
            x1t = x1ts[tt_ % 2]
            xk_ = ('x1t', tt_ % 2)
            ot = outb[tt_ % 2]
            ok_ = ('outb', tt_ % 2)
            if tt_ + 1 < 16:
                p.dma(SP, x1ts[(tt_ + 1) % 2][:], p.X1[(tt_ + 1) * 128:(tt_ + 2) * 128, :], w=[('x1t', (tt_ + 1) % 2)])
            for j in range(4):
                yb_ = yg[ng % 4]
                yk = ('yg', ng % 4)
                ng += 1
                p.S.op(POOL, lambda e, tt_=tt_, j=j, yb_=yb_: e.indirect_dma_start(
                    out=yb_[:, :], out_offset=None, in_=Y[:, :],
                    in_offset=bass.IndirectOffsetOnAxis(ap=desti[:, tt_, j:j + 1], axis=0)), [], [yk], dma=True)
                if j == 0:
                    p.ts(DVE, acc[:], yb_[:], wts[:, tt_, 0:1], None, ALU.mult, r=[yk], w=['acc'])
                else:
                    p.stt(DVE, acc[:], yb_[:], wts[:, tt_, j:j + 1], acc[:], ALU.mult, ALU.add, r=[yk, 'acc'], w=['acc'])
            p.tt(DVE, acc[:], acc[:], p.g2_b[:], ALU.mult, r=['acc'], w=['acc'])
            p.tt(DVE, acc[:], acc[:], x1t[:], ALU.add, r=['acc', xk_], w=['acc'])
            p.act(ot[:], acc[:], AF.Square, r=['acc'], w=[ok_, 'fs_'], accum_out=fs_[:, 0:1])
            p.act(fs_[:, 2:3], fs_[:, 0:1], AF.Ln, r=['fs_'], w=['fs_'], scale=1.0 / D, bias=p.eps_t[:, 0:1])
            p.act(fs_[:, 3:4], fs_[:, 2:3], AF.Exp, r=['fs_'], w=['fs_'], scale=-0.5)
            p.stt(DVE, ot[:], acc[:], fs_[:, 3:4], fnb[:], ALU.mult, ALU.mult, r=['acc', 'fs_', ok_], w=[ok_])
            p.dma(SP, yout[tt_ * 128:(tt_ + 1) * 128, :], ot[:], r=[ok_], w=[('yout', tt_)])
        p.S.barrier()
        stE.close()


def core_inputs(inputs, core, consts):
    b, hf = core // 2, core % 2
    rev = (hf == 0)
    m = {}
    x = np.asarray(inputs['x'][b], np.float32)
    m['x_seq'] = np.ascontiguousarray(x[::-1] if rev else x)
    cp = np.stack([inputs['c'][b], inputs['c_ctx']], 0).astype(np.float32)
    m['cT'] = np.ascontiguousarray(cp.reshape(2, 8, 128).transpose(2, 1, 0))
    m['bmod'] = fm_layout(inputs['b_mod'][0], 48)
    m['n1g'] = fm_layout(inputs['norm1_g'][0], 8)
    m['n2g'] = fm_layout(inputs['norm2_g'][0], 8)
    m['w_mod'] = np.ascontiguousarray(inputs['w_mod'][0], np.float32)
    cx = np.asarray(inputs['ctx'][b], np.float32)
    m['ctx_seq'] = np.ascontiguousarray(cx[::-1] if rev else cx)
    w_in = np.array(inputs['w_in'][0], np.float32)
    if rev:
        w2 = w_in.copy()
        w2[:, 2048:2056], w2[:, 2056:2064] = w_in[:, 2056:2064], w_in[:, 2048:2056]
        w2[:, 2064:2072], w2[:, 2072:2080] = w_in[:, 2072:2080], w_in[:, 2064:2072]
        w_in = w2
    m['w_in'] = np.ascontiguousarray(w_in)
    cwv = np.asarray(inputs['conv_w'][0], np.float32).reshape(9, 16, 128)
    if rev:
        cwv = cwv[::-1]
    cd = np.zeros((128, 16, 9, 128), np.float32)
    qi = np.arange(128)
    cd[qi, :, :, qi] = cwv.transpose(2, 1, 0)
    m['conv_diag'] = cd
    al = np.asarray(inputs['a_log'][0], np.float32)
    db = np.asarray(inputs['dt_bias'][0], np.float32)
    if rev:
        al, db = al[::-1], db[::-1]
    m['alog_t'] = np.ascontiguousarray(np.tile(al.reshape(1, 16), (NT + 2, 1)))
    m['dtb_t'] = np.ascontiguousarray(np.tile(db.reshape(1, 16), (NT + 2, 1)))
    pos = np.arange(L)[::-1] if rev else np.arange(L)
    own = pos[2048:]
    ang = (2.0 * np.pi / L) * ((pos[:, None].astype(np.int64) * own[None, :].astype(np.int64)) % L)
    tab = np.stack([np.cos(ang), np.sin(ang)], 0).astype(np.float32)
    tab = tab.reshape(2, 4, 8, 128, 4, 512).transpose(4, 1, 3, 0, 2, 5)
    m['dft_tab'] = bf(tab)
    cang = (2.0 * np.pi / 128) * ((np.arange(128)[:, None] * np.arange(128)[None, :]) % 128)
    m['cdft'] = bf(np.stack([np.cos(cang), -np.sin(cang)], 1))
    m['gng_t'] = np.ascontiguousarray(np.tile(np.asarray(inputs['gdn_norm_g'][0], np.float32), 8))
    m['w_fo'] = np.ascontiguousarray(inputs['w_fourier_out'][0], np.float32)
    m['w_go'] = np.ascontiguousarray(inputs['w_gdn_out'][0], np.float32)
    m['w_mo'] = np.ascontiguousarray(inputs['w_merge_out'][0], np.float32)
    m['w_router'] = np.ascontiguousarray(inputs['w_router'][0], np.float32)
    m['b_router'] = np.ascontiguousarray(inputs['b_router'][0], np.float32)
    for nm_, key in [('w_gate', 'w_gate'), ('w_up', 'w_up'), ('w_down', 'w_down'), ('b_gate', 'b_gate'), ('b_up', 'b_up'), ('b_down', 'b_down')]:
        m[nm_] = np.ascontiguousarray(inputs[key][0], np.float32)
    m['fng'] = np.ascontiguousarray(inputs['final_norm_g'], np.float32)
    for k, v in consts.items():
        m['c_' + k] = v
    return m


def build(debug=None):
    nc = bass.Bass("TRN2", target_bir_lowering=False)
    p = Builder(nc, debug)
    p.phase0()
    if debug == 'AD1':
        p.phaseA()
        p.debug = 'D1'
        p.phaseD()
        p.S.barrier(); p.S.emit(); p.st.close()
        return nc, p
    if debug == 'Gs':
        p.QKVs = p.inp("QKVs_in", [L + CTXL, QKV], BF16)
        p.ba = p.sb("ba", [128, NT + 2, 32], F32)
        p.dma(SP, p.ba[:], p.inp("ba_in", [128, NT + 2, 32], F32), w=['ba'])
        p.S.barrier()
        p.debug = 'G0'
    elif debug in ('Ds1', 'Ds'):
        p.XF = p.inp("XF_in", [L, 512], BF16)
        p.Od = [p.inp("Of_in", [2048, 1024], F32), p.inp("Ob_in", [2048, 1024], F32)]
        p.inp("w_in", [D, IN_COLS], F32)
        p.debug = 'D1' if debug == 'Ds1' else 'D'
    elif debug != '0':
        p.phaseA()
    if debug not in ('0', 'A', 'Ds', 'Ds1'):
        p.phaseG()
    if debug not in ('0', 'A', 'G', 'G0', 'Gs'):
        p.phaseD()
    if debug in (None, 'E'):
        p.phaseE()
    p.S.barrier()
    p.S.emit()
    p.st.close()
    return nc, p


def run(inputs, debug=None):
    nc, p = build(debug)
    maps = []
    for c in range(8):
        m = core_inputs(inputs, c, p.const_np)
        maps.append({k: m[k] for k in p.din})
    res = run_bass_kernel_spmd(nc, maps, core_ids=list(range(8)))
    return res.results


def kernel(**inputs):
    nc, p = build(None)
    maps = []
    for c in range(8):
        m = core_inputs(inputs, c, p.const_np)
        maps.append({k: m[k] for k in p.din})
    res = run_bass_kernel_spmd(nc, maps, core_ids=list(range(8)))
    out = np.zeros((4, L, D), np.float32)
    for c in range(8):
        b, hf = c // 2, c % 2
        y = np.asarray(res.results[c]['y'], np.float32)
        if hf == 1:
            out[b, 2048:] = y
        else:
            out[b, :2048] = y[::-1]
    return out
```
